# Optimizing a Trainium2 kernel written in Bass

```python
import math
import jax
import jax.numpy as jnp
from jax import lax
import numpy as np

D_MODEL = 1024
BATCH = 16
SEQ = 2048
DEPTH = 4

GRID_W = 64
CTX_LEN = 256

D_MIX = D_MODEL
GROUP_W = D_MIX // 4
HY_W = GROUP_W
SC_W = GROUP_W
GLA_HEADS = 4
GLA_DV = GROUP_W // GLA_HEADS
GLA_DK = GLA_DV // 2
GLA_QK = GLA_HEADS * GLA_DK
GLA_V = GLA_HEADS * GLA_DV
GLA_RANK = 16
GLA_TAU = 16.0
GDN_HEADS = 4
GDN_DK = GROUP_W // GDN_HEADS
GDN_DV = GDN_DK
GDN_KW = GDN_HEADS * GDN_DK
GDN_VW = GDN_HEADS * GDN_DV
HY_EMB = 33
HY_HID = 64
HY_DECAY_TARGET = 1e-2
HY_FAST_PCT = 0.3
HY_SLOW_PCT = 1.5
CHUNK = 64
N_EXPERTS = 32
TOP_K = 4
D_EXPERT = D_MODEL
SWIGLU_LIMIT = 7.0
SWIGLU_ALPHA = 1.702
LN_EPS = 1e-5
RMS_EPS = 1e-6
L2_EPS = 1e-6
DN_ALPHA = (2 * DEPTH) ** 0.25
DN_BETA = (8 * DEPTH) ** -0.25

HY_COLS = 3 * HY_W
SC_COLS = 3 * SC_W
GLA_COLS = 2 * GLA_QK + 2 * GLA_V + 2 * GLA_RANK
GDN_COLS = 2 * GDN_KW + 2 * GDN_VW + 4 * GDN_HEADS
D_IN = HY_COLS + SC_COLS + GLA_COLS + GDN_COLS

kernel_name = 'hybrid_hyena_conv_gla_gdn_moe_diffusion_trunk'

F32 = jnp.float32


def _split(x, sizes):
    return jnp.split(x, [int(s) for s in np.cumsum(sizes)[:-1]], axis=-1)


def layer_norm(x, g, b):
    xf = x.astype(F32)
    xc = xf - jnp.mean(xf, axis=-1, keepdims=True)
    var = jnp.mean(xc * xc, axis=-1, keepdims=True)
    return (xc * lax.rsqrt(var + LN_EPS) * g.astype(F32) + b.astype(F32)).astype(x.dtype)


def rms_norm(x, w):
    xf = x.astype(F32)
    return xf * lax.rsqrt(jnp.mean(xf * xf, axis=-1, keepdims=True) + RMS_EPS) * w.astype(F32)


def l2_normalize(x):
    return x * lax.rsqrt(jnp.sum(x * x, axis=-1, keepdims=True) + L2_EPS)


def short_conv3(x, w, n_rows):
    bsz, L, ch = x.shape
    xr = x.reshape(bsz, n_rows, L // n_rows, ch)
    xp = jnp.pad(xr, ((0, 0), (0, 0), (1, 1), (0, 0)))
    y = xp[:, :, :-2] * w[0] + xp[:, :, 1:-1] * w[1] + xp[:, :, 2:] * w[2]
    return y.reshape(bsz, L, ch)


def hyena_filters(L, w1, b1, w2, b2, w3, b3, w4, freq):
    t = jnp.linspace(0.0, 1.0, L, dtype=F32)[:, None]
    bands = (HY_EMB - 1) // 2
    ang = 2.0 * math.pi * jnp.arange(L, dtype=F32)[:, None] / L
    f = jnp.linspace(1e-4, bands - 1, bands, dtype=F32)[None, :]
    z = jnp.concatenate([t, jnp.cos(f * ang), -jnp.sin(f * ang)], axis=-1).astype(w1.dtype)
    h = jnp.sin(freq[0] * (z @ w1 + b1))
    h = jnp.sin(freq[1] * (h @ w2 + b2))
    h = jnp.sin(freq[2] * (h @ w3 + b3))
    k = (h @ w4).astype(F32)
    max_decay = math.log(HY_DECAY_TARGET) / HY_FAST_PCT
    min_decay = math.log(HY_DECAY_TARGET) / HY_SLOW_PCT
    deltas = jnp.abs(jnp.linspace(min_decay, max_decay, HY_W, dtype=F32))
    window = jnp.exp(-t * deltas[None, :])
    k_fwd = k[:, :HY_W] * window
    k_bwd = k[:, HY_W:] * window
    l1 = jnp.sum(jnp.abs(k_fwd), axis=0) + jnp.sum(jnp.abs(k_bwd[1:]), axis=0)
    return k_fwd / l1, k_bwd / l1


def hyena_mix(p, n_rows, conv_w, conv_b, k_fwd, k_bwd, d_bias):
    u = short_conv3(p, conv_w, n_rows) + conv_b
    x0, x1, v = jnp.split(u, 3, axis=-1)
    z = x1 * v
    L = z.shape[1]
    k_full = jnp.concatenate([k_fwd, jnp.zeros((1, HY_W), F32), k_bwd[:0:-1]], axis=0)
    zf = jnp.fft.rfft(z.astype(F32), n=2 * L, axis=1)
    kf = jnp.fft.rfft(k_full, axis=0)
    y = jnp.fft.irfft(zf * kf[None], n=2 * L, axis=1)[:, :L].astype(z.dtype)
    return (y + z * d_bias) * x0


def shortconv_mix(p, n_rows, conv_w):
    bg, cg, hs = jnp.split(p, 3, axis=-1)
    return bg * short_conv3(cg * hs, conv_w, n_rows)


def _to_chunks(t, n):
    bsz, L, H = t.shape[:3]
    t = t.reshape((bsz, n, CHUNK, H) + t.shape[3:])
    return jnp.moveaxis(t, (1, 3), (0, 2))


def _from_chunks(o):
    o = jnp.moveaxis(o, (0, 2), (1, 3))
    return o.reshape((o.shape[0], o.shape[1] * o.shape[2]) + o.shape[3:])


def gla_chunked(q, k, v, g, s0):
    n = q.shape[1] // CHUNK
    causal = jnp.tril(jnp.ones((CHUNK, CHUNK), dtype=bool))

    def step(S, inp):
        qc, kc, vc, gc = inp
        b = jnp.cumsum(gc, axis=2)
        b_mid = b[:, :, CHUNK // 2:CHUNK // 2 + 1]
        b_last = b[:, :, -1:]
        a = jnp.einsum('bhik,bhjk->bhij', qc * jnp.exp(b - b_mid), kc * jnp.exp(b_mid - b))
        a = jnp.where(causal, a, 0.0)
        o = jnp.einsum('bhij,bhjv->bhiv', a, vc) + jnp.einsum('bhik,bhkv->bhiv', qc * jnp.exp(b), S)
        S = jnp.exp(b_last[:, :, 0])[..., None] * S + jnp.einsum('bhjk,bhjv->bhkv', kc * jnp.exp(b_last - b), vc)
        return S, o

    S, o = lax.scan(step, s0, (_to_chunks(q, n), _to_chunks(k, n), _to_chunks(v, n), _to_chunks(g, n)))
    return _from_chunks(o), S


def gdn_chunked(q, k, v, beta, g, s0):
    n = q.shape[1] // CHUNK
    V = v.shape[-1]
    tril_incl = jnp.tril(jnp.ones((CHUNK, CHUNK), dtype=bool))
    tril_strict = jnp.tril(jnp.ones((CHUNK, CHUNK), dtype=bool), k=-1)
    eye = jnp.eye(CHUNK, dtype=q.dtype)

    def step(S, inp):
        qc, kc, vc, bc, gc = inp
        cum = jnp.cumsum(gc, axis=-1)
        decay = jnp.exp(jnp.where(tril_incl, cum[..., :, None] - cum[..., None, :], -jnp.inf))
        kb = kc * bc[..., None]
        a_strict = jnp.where(tril_strict, jnp.einsum('bhik,bhjk->bhij', kb, kc) * decay, 0.0)
        rhs = jnp.concatenate([vc * bc[..., None], kb * jnp.exp(cum)[..., None]], axis=-1)
        sol = lax.linalg.triangular_solve(a_strict + eye, rhs, left_side=True, lower=True, unit_diagonal=True)
        u, w = sol[..., :V], sol[..., V:]
        v_new = u - jnp.einsum('bhck,bhkv->bhcv', w, S)
        attn = jnp.einsum('bhik,bhjk->bhij', qc, kc) * decay
        o = jnp.einsum('bhik,bhkv->bhiv', qc * jnp.exp(cum)[..., None], S) + jnp.einsum('bhij,bhjv->bhiv', attn, v_new)
        S = jnp.exp(cum[..., -1])[..., None, None] * S + jnp.einsum(
            'bhjk,bhjv->bhkv', kc * jnp.exp(cum[..., -1:] - cum)[..., None], v_new)
        return S, o

    xs = (_to_chunks(q, n), _to_chunks(k, n), _to_chunks(v, n), _to_chunks(beta, n), _to_chunks(g, n))
    S, o = lax.scan(step, s0, xs)
    return _from_chunks(o), S


def bidirectional_prefix_scan(scan_fn, ctx_f, ctx_b, lat_f, lat_b, s0):
    flip = lambda args: tuple(a[:, ::-1] for a in args)
    oc_f, sc_f = scan_fn(*ctx_f, s0)
    ol_f, _ = scan_fn(*lat_f, sc_f)
    oc_b, sc_b = scan_fn(*flip(ctx_b), s0)
    ol_b, _ = scan_fn(*flip(lat_b), sc_b)
    return oc_f + oc_b[:, ::-1], ol_f + ol_b[:, ::-1]


def gla_mix(pc, pl, w_a, b_a, norm_w):
    def prep(p):
        bsz, L, _ = p.shape
        q, k, v, r, a_f, a_b = _split(p, [GLA_QK, GLA_QK, GLA_V, GLA_V, GLA_RANK, GLA_RANK])
        heads = lambda t, d: t.astype(F32).reshape(bsz, L, GLA_HEADS, d)
        log_decay = lambda a, i: jax.nn.log_sigmoid((a @ w_a[i] + b_a[i]).astype(F32)) / GLA_TAU
        q = heads(q, GLA_DK) * GLA_DK ** -0.5
        k = heads(k, GLA_DK)
        v = heads(v, GLA_DV)
        return (q, k, v, heads(log_decay(a_f, 0), GLA_DK)), (q, k, v, heads(log_decay(a_b, 1), GLA_DK)), r

    fwd_c, bwd_c, r_c = prep(pc)
    fwd_l, bwd_l, r_l = prep(pl)
    s0 = jnp.zeros((pc.shape[0], GLA_HEADS, GLA_DK, GLA_DV), F32)
    o_c, o_l = bidirectional_prefix_scan(gla_chunked, fwd_c, bwd_c, fwd_l, bwd_l, s0)

    def finish(o, r):
        bsz, L = r.shape[:2]
        o = rms_norm(o, norm_w).reshape(bsz, L, GLA_V)
        return (o * jax.nn.silu(r.astype(F32))).astype(r.dtype)

    return finish(o_c, r_c), finish(o_l, r_l)


def gdn_mix(pc, pl, lat_rows, conv_w, a_log, dt_bias, norm_w):
    def prep(p, n_rows):
        bsz, L, _ = p.shape
        qkv, z, a_f, a_b, b_f, b_b = _split(p, [2 * GDN_KW + GDN_VW, GDN_VW, GDN_HEADS, GDN_HEADS, GDN_HEADS, GDN_HEADS])
        qkv = jax.nn.silu(short_conv3(qkv, conv_w, n_rows)).astype(F32)
        q, k, v = _split(qkv, [GDN_KW, GDN_KW, GDN_VW])
        q = l2_normalize(q.reshape(bsz, L, GDN_HEADS, GDN_DK)) * GDN_DK ** -0.5
        k = l2_normalize(k.reshape(bsz, L, GDN_HEADS, GDN_DK))
        v = v.reshape(bsz, L, GDN_HEADS, GDN_DV)

        def gates(a, b, i):
            g = -jnp.exp(a_log[i].astype(F32)) * jax.nn.softplus(a.astype(F32) + dt_bias[i].astype(F32))
            return jax.nn.sigmoid(b.astype(F32)), g

        beta_f, g_f = gates(a_f, b_f, 0)
        beta_b, g_b = gates(a_b, b_b, 1)
        return (q, k, v, beta_f, g_f), (q, k, v, beta_b, g_b), z

    fwd_c, bwd_c, z_c = prep(pc, 1)
    fwd_l, bwd_l, z_l = prep(pl, lat_rows)
    s0 = jnp.zeros((pc.shape[0], GDN_HEADS, GDN_DK, GDN_DV), F32)
    o_c, o_l = bidirectional_prefix_scan(gdn_chunked, fwd_c, bwd_c, fwd_l, bwd_l, s0)

    def finish(o, z):
        bsz, L = z.shape[:2]
        o = rms_norm(o, norm_w) * jax.nn.silu(z.astype(F32).reshape(bsz, L, GDN_HEADS, GDN_DV))
        return o.reshape(bsz, L, GDN_VW).astype(z.dtype)

    return finish(o_c, z_c), finish(o_l, z_l)


def token_mixers(pc, pl, lat_rows, hy_conv_w, hy_conv_b, hy_w1, hy_b1, hy_w2, hy_b2, hy_w3, hy_b3, hy_w4,
                 hy_freq, hy_d, sc_conv_w, gla_w_a, gla_b_a, gla_norm_w, gdn_conv_w, gdn_a_log, gdn_dt_bias,
                 gdn_norm_w):
    hy_c, sc_c, gla_c, gdn_c = _split(pc, [HY_COLS, SC_COLS, GLA_COLS, GDN_COLS])
    hy_l, sc_l, gla_l, gdn_l = _split(pl, [HY_COLS, SC_COLS, GLA_COLS, GDN_COLS])
    kf_c, kb_c = hyena_filters(pc.shape[1], hy_w1, hy_b1, hy_w2, hy_b2, hy_w3, hy_b3, hy_w4, hy_freq)
    kf_l, kb_l = hyena_filters(pl.shape[1], hy_w1, hy_b1, hy_w2, hy_b2, hy_w3, hy_b3, hy_w4, hy_freq)
    ya_c = hyena_mix(hy_c, 1, hy_conv_w, hy_conv_b, kf_c, kb_c, hy_d)
    ya_l = hyena_mix(hy_l, lat_rows, hy_conv_w, hy_conv_b, kf_l, kb_l, hy_d)
    yb_c = shortconv_mix(sc_c, 1, sc_conv_w)
    yb_l = shortconv_mix(sc_l, lat_rows, sc_conv_w)
    yc_c, yc_l = gla_mix(gla_c, gla_l, gla_w_a, gla_b_a, gla_norm_w)
    yd_c, yd_l = gdn_mix(gdn_c, gdn_l, lat_rows, gdn_conv_w, gdn_a_log, gdn_dt_bias, gdn_norm_w)
    return (jnp.concatenate([ya_c, yb_c, yc_c, yd_c], axis=-1),
            jnp.concatenate([ya_l, yb_l, yc_l, yd_l], axis=-1))


def moe_ffn(h, w_router, b_router, w_gate, b_gate, w_up, b_up, w_down, b_down):
    shape = h.shape
    t = h.reshape(-1, shape[-1])
    logits = (t @ w_router + b_router).astype(F32)
    top_logit, top_idx = lax.top_k(logits, TOP_K)
    top_w = jax.nn.softmax(top_logit, axis=-1)
    combine = jnp.einsum('tk,tke->et', top_w, jax.nn.one_hot(top_idx, N_EXPERTS, dtype=F32)).astype(t.dtype)

    def expert(acc, prm):
        wg, bg, wu, bu, wd, bd, cw = prm
        gate = jnp.minimum(t @ wg + bg, SWIGLU_LIMIT)
        up = jnp.clip(t @ wu + bu, -SWIGLU_LIMIT, SWIGLU_LIMIT)
        y = ((up + 1.0) * gate * jax.nn.sigmoid(SWIGLU_ALPHA * gate)) @ wd + bd
        return acc + cw[:, None] * y, None

    out, _ = lax.scan(expert, jnp.zeros_like(t), (w_gate, b_gate, w_up, b_up, w_down, b_down, combine))
    return out.reshape(shape)


def setup_inputs(seed: int = 0) -> dict:
    key = jax.random.key(seed)
    ks = iter(jax.random.split(key, 48))

    def nrm(shape, scale=1.0):
        return jax.random.normal(next(ks), shape, F32) * scale

    L, E, F = DEPTH, N_EXPERTS, D_EXPERT
    dt = jnp.exp(jax.random.uniform(next(ks), (L, 2, GDN_HEADS), F32, math.log(1e-3), math.log(1e-1)))
    a_log = jnp.log(jax.random.uniform(next(ks), (L, 2, GDN_HEADS), F32, 1.0, 16.0))
    return {
        'x': nrm((BATCH, SEQ, D_MODEL)),
        'c': nrm((BATCH, D_MODEL)),
        'ctx': nrm((BATCH, CTX_LEN, D_MODEL)),
        'c_ctx': nrm((D_MODEL,)),
        'w_ada': nrm((L, D_MODEL, 6 * D_MODEL), D_MODEL ** -0.5),
        'b_ada': nrm((L, 6 * D_MODEL), 0.02),
        'w_in': nrm((L, D_MODEL, D_IN), D_MODEL ** -0.5),
        'hy_conv_w': nrm((L, 3, HY_COLS), 3 ** -0.5),
        'hy_conv_b': nrm((L, HY_COLS), 0.02),
        'hy_w1': nrm((L, HY_EMB, HY_HID), HY_EMB ** -0.5),
        'hy_b1': nrm((L, HY_HID), 0.02),
        'hy_w2': nrm((L, HY_HID, HY_HID), HY_HID ** -0.5),
        'hy_b2': nrm((L, HY_HID), 0.02),
        'hy_w3': nrm((L, HY_HID, HY_HID), HY_HID ** -0.5),
        'hy_b3': nrm((L, HY_HID), 0.02),
        'hy_w4': nrm((L, HY_HID, 2 * HY_W), HY_HID ** -0.5),
        'hy_freq': 1.0 + nrm((L, 3, HY_HID), 0.1),
        'hy_d': nrm((L, HY_W)),
        'sc_conv_w': nrm((L, 3, SC_W), 3 ** -0.5),
        'gla_w_a': nrm((L, 2, GLA_RANK, GLA_QK), GLA_RANK ** -0.5),
        'gla_b_a': nrm((L, 2, GLA_QK), 0.02),
        'gla_norm_w': 1.0 + nrm((L, GLA_DV), 0.02),
        'gdn_conv_w': nrm((L, 3, 2 * GDN_KW + GDN_VW), 3 ** -0.5),
        'gdn_a_log': a_log,
        'gdn_dt_bias': dt + jnp.log(-jnp.expm1(-dt)),
        'gdn_norm_w': 1.0 + nrm((L, GDN_DV), 0.02),
        'w_out': nrm((L, D_MIX, D_MODEL), D_MIX ** -0.5 * DN_BETA),
        'ln1_g': 1.0 + nrm((L, D_MODEL), 0.02),
        'ln1_b': nrm((L, D_MODEL), 0.02),
        'w_router': nrm((L, D_MODEL, E), D_MODEL ** -0.5),
        'b_router': nrm((L, E), 0.01),
        'w_gate': nrm((L, E, D_MODEL, F), D_MODEL ** -0.5),
        'b_gate': nrm((L, E, F), 0.02),
        'w_up': nrm((L, E, D_MODEL, F), D_MODEL ** -0.5),
        'b_up': nrm((L, E, F), 0.02),
        'w_down': nrm((L, E, F, D_MODEL), F ** -0.5 * DN_BETA),
        'b_down': nrm((L, E, D_MODEL), 0.02),
        'ln2_g': 1.0 + nrm((L, D_MODEL), 0.02),
        'ln2_b': nrm((L, D_MODEL), 0.02),
    }


def reference(x, c, ctx, c_ctx, w_ada, b_ada, w_in, hy_conv_w, hy_conv_b, hy_w1, hy_b1, hy_w2, hy_b2, hy_w3,
              hy_b3, hy_w4, hy_freq, hy_d, sc_conv_w, gla_w_a, gla_b_a, gla_norm_w, gdn_conv_w, gdn_a_log,
              gdn_dt_bias, gdn_norm_w, w_out, ln1_g, ln1_b, w_router, b_router, w_gate, b_gate, w_up, b_up,
              w_down, b_down, ln2_g, ln2_b):
    ROWS = x.shape[1] // GRID_W
    n_ctx = ctx.shape[1]
    silu_c = jax.nn.silu(c)
    silu_cc = jax.nn.silu(c_ctx)
    xl, xc = x, ctx
    for l in range(DEPTH):
        last = l == DEPTH - 1
        sh1_l, sc1_l, g1_l, sh2_l, sc2_l, g2_l = jnp.split((silu_c @ w_ada[l] + b_ada[l])[:, None, :], 6, axis=-1)
        sh1_c, sc1_c, g1_c, sh2_c, sc2_c, g2_c = jnp.split((silu_cc @ w_ada[l] + b_ada[l])[None, None, :], 6, axis=-1)
        pl = (xl * (1.0 + sc1_l) + sh1_l) @ w_in[l]
        pc = (xc * (1.0 + sc1_c) + sh1_c) @ w_in[l]
        yc, yl = token_mixers(pc, pl, ROWS, hy_conv_w[l], hy_conv_b[l], hy_w1[l], hy_b1[l], hy_w2[l], hy_b2[l],
                              hy_w3[l], hy_b3[l], hy_w4[l], hy_freq[l], hy_d[l], sc_conv_w[l], gla_w_a[l],
                              gla_b_a[l], gla_norm_w[l], gdn_conv_w[l], gdn_a_log[l], gdn_dt_bias[l], gdn_norm_w[l])
        xl = layer_norm(DN_ALPHA * xl + g1_l * (yl @ w_out[l]), ln1_g[l], ln1_b[l])
        hl = xl * (1.0 + sc2_l) + sh2_l
        if last:
            fl = moe_ffn(hl, w_router[l], b_router[l], w_gate[l], b_gate[l], w_up[l], b_up[l], w_down[l], b_down[l])
        else:
            xc = layer_norm(DN_ALPHA * xc + g1_c * (yc @ w_out[l]), ln1_g[l], ln1_b[l])
            hc = xc * (1.0 + sc2_c) + sh2_c
            f = moe_ffn(jnp.concatenate([hc, hl], axis=1), w_router[l], b_router[l], w_gate[l], b_gate[l],
                        w_up[l], b_up[l], w_down[l], b_down[l])
            fc, fl = f[:, :n_ctx], f[:, n_ctx:]
            xc = layer_norm(DN_ALPHA * xc + g2_c * fc, ln2_g[l], ln2_b[l])
        xl = layer_norm(DN_ALPHA * xl + g2_l * fl, ln2_g[l], ln2_b[l])
    return xl
```

```python
import contextlib
import math
import numpy as np
import ml_dtypes
import concourse.bass as bass
import concourse.mybir as mybir
from concourse.bass_utils import run_bass_kernel_spmd

F32 = mybir.dt.float32
BF16 = mybir.dt.bfloat16
AF = mybir.ActivationFunctionType
ALU = mybir.AluOpType
AX = mybir.AxisListType

D = 1024
DEPTH = 4
NB = 2
CTX = 256
SEQ = 2048
LS = CTX + SEQ
NT = LS // 128
TOK = NB * LS
D_IN = 3376
NE = 32
LN_EPS = 1e-5
DN_ALPHA = (2 * DEPTH) ** 0.25
HY0, SC0, GLA0, GDN0 = 0, 768, 1536, 2336

EPOCH = 12000
NSLOT = 12
COMPUTE = ("tensor", "vector", "scalar", "gpsimd")
QUEUES = ("sync", "gpsimd")
ALLENG = COMPUTE + ("sync",)


def _keys(aps):
    out = []
    for a in aps:
        if a is None:
            continue
        if isinstance(a, (str, tuple)):
            out.append(a)
        else:
            out.append(getattr(a, "tensor", a).name)
    return out


class Prog:
    def __init__(self, nc):
        self.nc = nc
        self.ops = []
        self.psum = set()

    def op(self, eng, fn, reads=(), writes=(), dma=False, barrier=False):
        r, w = _keys(reads), _keys(writes)
        w = w + [k for k in r if k in self.psum and k not in w]
        self.ops.append(dict(eng=eng, fn=fn, reads=r, writes=w, dma=dma, barrier=barrier))

    def barrier(self):
        self.ops.append(dict(eng=None, barrier=True))

    def dma(self, q, out, in_, reads=None, writes=None, **kw):
        r = [in_] if reads is None else reads
        w = [out] if writes is None else writes
        self.op(q, lambda e: e.dma_start(out=out, in_=in_, **kw), r, w, dma=True)

    def mm(self, out, lhsT, rhs, start=True, stop=True, reads=None, writes=None, **kw):
        r = [lhsT, rhs] if reads is None else reads
        w = [out] if writes is None else writes
        self.op("tensor", lambda e: e.matmul(out, lhsT, rhs, start=start, stop=stop, **kw), r, w)

    def tr(self, out, in_, ident, reads=None, writes=None):
        r = [in_, ident] if reads is None else reads
        w = [out] if writes is None else writes
        self.op("tensor", lambda e: e.transpose(out, in_, ident), r, w)

    def v(self, fn, reads, writes):
        self.op("vector", fn, reads, writes)

    def a(self, fn, reads, writes):
        self.op("scalar", fn, reads, writes)

    def g(self, fn, reads, writes):
        self.op("gpsimd", fn, reads, writes)

    def emit(self):
        nc = self.nc
        ops = [o for o in self.ops]
        seq = {e: 0 for e in ALLENG}
        dma_cnt = {q: 0 for q in QUEUES}
        last_w, readers = {}, {}
        last_tok = {}
        dma_since = []
        force = {e: set() for e in ALLENG}
        seen = {e: {} for e in ALLENG}
        waited = set()
        real = []
        for o in ops:
            if o.get("barrier") and o["eng"] is None:
                B = set(last_tok.values()) | set(dma_since)
                dma_since = []
                for e in ALLENG:
                    force[e] |= B
                continue
            e = o["eng"]
            deps = set(force[e])
            force[e] = set()
            for k in o["reads"]:
                if k in last_w:
                    deps.add(last_w[k])
            for k in o["writes"]:
                if k in last_w:
                    deps.add(last_w[k])
                deps.update(readers.get(k, ()))
            if o["dma"]:
                di = dma_cnt[e]
                dma_cnt[e] += 1
                tok = ("d", e, di)
                if di >= NSLOT:
                    deps.add(("d", e, di - NSLOT))
                seq[e] += 1
                dma_since.append(tok)
            else:
                tok = ("c", e, seq[e])
                seq[e] += 1
                last_tok[e] = tok
            need, best = [], {}
            for t in deps:
                if t == tok:
                    continue
                if t[0] == "c":
                    src = t[1]
                    if src == e and e == "tensor":
                        continue
                    if seen[e].get(src, -1) >= t[2]:
                        continue
                    if best.get(src, -1) < t[2]:
                        best[src] = t[2]
                else:
                    if t in seen[e]:
                        continue
                    need.append(t)
                    seen[e][t] = True
            for src, idx in best.items():
                need.append(("c", src, idx))
                seen[e][src] = idx
            waited.update(need)
            o["deps"], o["tok"] = need, tok
            for k in o["reads"]:
                readers.setdefault(k, []).append(tok)
            for k in o["writes"]:
                last_w[k] = tok
                readers[k] = []
            real.append(o)
        incno, cnt = {}, {e: 0 for e in COMPUTE}
        for o in real:
            t = o["tok"]
            if t[0] == "c" and t in waited:
                incno[t] = cnt[t[1]]
                cnt[t[1]] += 1
        stack = contextlib.ExitStack()
        sems = {e: [stack.enter_context(nc.semaphore(f"s_{e}_{j}"))
                    for j in range(max((cnt[e] + EPOCH - 1) // EPOCH, 1))] for e in COMPUTE}
        dsems = {q: [stack.enter_context(nc.semaphore(f"d_{q}_{j}")) for j in range(NSLOT)]
                 for q in QUEUES}

        def wait_args(t):
            if t[0] == "c":
                k = incno[t]
                return sems[t[1]][k // EPOCH], (k % EPOCH) + 1
            return dsems[t[1]][t[2] % NSLOT], 16 * (t[2] // NSLOT + 1)

        per_eng = {e: [] for e in ALLENG}
        for o in real:
            per_eng[o["eng"]].append(o)

        def run_engine(ename, eng):
            for o in per_eng[ename]:
                for t in o["deps"]:
                    s, val = wait_args(t)
                    eng.wait_ge(s, val)
                ins = o["fn"](eng)
                t = o["tok"]
                if t[0] == "d":
                    ins.then_inc(wait_args(t)[0], 16)
                elif t in incno:
                    ins.then_inc(sems[t[1]][incno[t] // EPOCH], 1)
            if ename in QUEUES:
                for di in range(max(0, dma_cnt[ename] - NSLOT), dma_cnt[ename]):
                    s, val = wait_args(("d", ename, di))
                    eng.wait_ge(s, val)

        with nc.Block() as block:
            block.tensor(lambda eng: run_engine("tensor", eng))
            block.vector(lambda eng: run_engine("vector", eng))
            block.scalar(lambda eng: run_engine("scalar", eng))
            block.gpsimd(lambda eng: run_engine("gpsimd", eng))
            block.sync(lambda eng: run_engine("sync", eng))
        stack.close()
        self.stats = dict(n_ops=len(real), incs=dict(cnt), dmas=dict(dma_cnt))


class KB:
    def __init__(self, nlayers=DEPTH, dbg=None, mixers=("hy", "sc", "gla", "gdn"), moe=True):
        self.nc = bass.Bass("TRN2", target_bir_lowering=False)
        self.P = Prog(self.nc)
        self.nl = nlayers
        self.dbg = dbg or {}
        self.mixers = mixers
        self.moe = moe
        self.gstack = contextlib.ExitStack()
        self.rr = 0
        self.names = {}

    def din(self, name, shape, dt=F32):
        return self.nc.dram_tensor(name, list(shape), dt, kind="ExternalInput").ap()

    def dout(self, name, shape, dt=F32):
        return self.nc.dram_tensor(name, list(shape), dt, kind="ExternalOutput").ap()

    def dscr(self, name, shape, dt=F32):
        if name in self.dbg:
            return self.nc.dram_tensor(name, list(shape), dt, kind="ExternalOutput").ap()
        return self.nc.dram_tensor(name, list(shape), dt, kind="Internal").ap()

    def _uniq(self, name):
        n = self.names.get(name, 0)
        self.names[name] = n + 1
        return name if n == 0 else f"{name}__{n}"

    def sb(self, st, name, shape, dt=F32):
        return st.enter_context(self.nc.sbuf_tensor(self._uniq(name), list(shape), dt))

    def ps(self, st, name, shape, dt=F32):
        nm = self._uniq(name)
        self.P.psum.add(nm)
        return st.enter_context(self.nc.psum_tensor(nm, list(shape), dt))

    def ew(self):
        self.rr += 1
        return "vector" if self.rr % 2 else "gpsimd"


class YTView:
    def __init__(self, ap3, k0):
        self.ap3, self.k0 = ap3, k0

    def __getitem__(self, idx):
        p, k, t = idx
        if isinstance(k, slice):
            k = slice(k.start - self.k0, k.stop - self.k0)
        else:
            k = k - self.k0
        return self.ap3[p, k, t]


def bcast_rows(ap_row, n):
    return ap_row.partition_broadcast(n)


def build(nlayers=DEPTH, dbg=None, mixers=("hy", "sc", "gla", "gdn"), moe=True):
    kb = KB(nlayers, dbg, mixers, moe)
    nc, P = kb.nc, kb.P
    L = nlayers
    xin = kb.din("xin", [TOK, D])
    c3T = kb.din("c3T", [128, 8, 3])
    w_ada = kb.din("w_ada", [DEPTH, D, 6 * D])
    b_ada = kb.din("b_ada", [DEPTH, 6 * D])
    w_in = kb.din("w_in", [DEPTH, D, D_IN])
    w_out = kb.din("w_out", [DEPTH, D, D])
    lnp = kb.din("lnp", [DEPTH, 4, D])
    w_router = kb.din("w_router", [DEPTH, D, NE])
    b_router = kb.din("b_router", [DEPTH, NE])
    w_gate = kb.din("w_gate", [DEPTH, NE, D, D]) if kb.moe else None
    w_up = kb.din("w_up", [DEPTH, NE, D, D]) if kb.moe else None
    w_down = kb.din("w_down", [DEPTH, NE, D, D]) if kb.moe else None
    bguT = kb.din("bguT", [DEPTH, 128, 2, NE, 8])
    b_down = kb.din("b_down", [DEPTH, NE, D])
    smallT = kb.din("smallT", [DEPTH, 128, 64])
    ident_d = kb.din("ident", [128, 128])
    sel_d = kb.din("sel3", [3, 3, 128])
    C = {}
    C["gla_w_a"] = kb.din("gla_w_a", [DEPTH, 2, 16, 128])
    C["gla_norm_w"] = kb.din("gla_norm_w", [DEPTH, 64])
    C["trimask"] = kb.din("trimask", [2, 128, 512])
    C["bdmask"] = kb.din("bdmask", [128, 256])
    C["hsel"] = kb.din("hsel", [128, 4])
    C["gdn_norm_w"] = kb.din("gdn_norm_w", [DEPTH, 64])
    C["bones"] = kb.din("bones", [128, 128])
    C["gsel"] = kb.din("gsel", [16, 2])
    C["selq"] = kb.din("selq", [16, 512])
    C["selr"] = kb.din("selr", [16, 1024])
    C["gmask"] = kb.din("gmask", [4, 128, 128])
    C["smask"] = kb.din("smask", [5, 128, 128])
    C["hy_w1"] = kb.din("hy_w1", [DEPTH, 33, 64])
    C["hy_w2"] = kb.din("hy_w2", [DEPTH, 64, 64])
    C["hy_w3"] = kb.din("hy_w3", [DEPTH, 64, 64])
    C["hy_w4"] = kb.din("hy_w4", [DEPTH, 64, 512])
    C["hyp"] = kb.din("hyp", [DEPTH, 64, 6])
    C["altcol"] = kb.din("altcol", [128, 1])
    C["altrow"] = kb.din("altrow", [1, SEQ])
    kb.KSPEC, kb.KNYQ = {}, {}
    for (nm, L_, nch, N_) in HYCFG:
        C[f"zT_{nm}"] = kb.din(f"zT_{nm}", [33, L_])
        C[f"win_{nm}"] = kb.din(f"win_{nm}", [L_, 256])
        C[f"wN_{nm}"] = kb.din(f"wN_{nm}", [128, nch])
        C[f"ctab_{nm}"] = kb.din(f"ctab_{nm}", [L_, L_], BF16)
        C[f"stab_{nm}"] = kb.din(f"stab_{nm}", [L_, L_], BF16)
        kb.KSPEC[nm] = kb.dscr(f"KSPEC_{nm}", [nch, 128, 512])
        kb.KNYQ[nm] = kb.dscr(f"KNYQ_{nm}", [1, 256])
    kb.C = C
    out = kb.dout("out", [NB * SEQ, D])
    XR = kb.dscr("XR", [TOK, D])
    kb.YTD = [kb.dscr(f"YTD{b}", [128, 8 * LS], BF16) for b in range(NB)]

    G = kb.gstack
    ident = kb.sb(G, "ident_sb", [128, 128])
    identb = kb.sb(G, "identb_sb", [128, 128], BF16)
    sel = kb.sb(G, "sel_sb", [3, 3 * 128])
    scT = kb.sb(G, "scT", [128, 8 * 3])
    P.dma("sync", ident[:], ident_d)
    P.dma("sync", sel[:], sel_d.rearrange("k j p -> k (j p)"))
    P.dma("sync", scT[:], c3T.rearrange("p k j -> p (k j)"))
    P.v(lambda e: e.tensor_copy(identb[:], ident[:]), [ident], [identb])
    P.a(lambda e: e.activation(scT[:], scT[:], AF.Silu), [scT], [scT])
    adaT = kb.sb(G, "adaT", [128, 3 * 48])
    ada1T = kb.sb(G, "ada1T", [128, 3 * 48])
    ADA = kb.dscr("ADA", [DEPTH * 3, 6 * D])
    small = kb.sb(G, "small_sb", [128, 64])

    def acol(j, m):
        return adaT[:, j * 48 + m: j * 48 + m + 1]

    def a1col(j, m):
        return ada1T[:, j * 48 + m: j * 48 + m + 1]

    for l in range(L):
        src = xin if l == 0 else XR
        last = (l == L - 1)
        with contextlib.ExitStack() as S:
            wblk = [kb.sb(S, f"adaw{i}", [128, 8 * 512]) for i in range(2)]
            bb = kb.sb(S, "adab", [3, 6 * D])
            ada_sb = kb.sb(S, "ada_sb", [3, 6 * D])
            pa = [kb.ps(S, f"adaps{i}", [128, 512]) for i in range(2)]
            pt = kb.ps(S, "adapt", [128, 3 * 48])
            P.dma("sync", bb[:], b_ada[l:l + 1, :].partition_broadcast(3) if False else
                  b_ada[l:l + 1, :].to_broadcast([3, 6 * D]))
            P.dma("sync", small[:], smallT[l])
            for n in range(12):
                wb = wblk[n % 2]
                P.dma("sync" if n % 2 else "gpsimd", wb[:].rearrange("p (k c) -> p k c", k=8),
                      w_ada[l, :, n * 512:(n + 1) * 512].rearrange("(k p) c -> p k c", p=128),
                      writes=[wb])
                pp = pa[n % 2]
                for k in range(8):
                    P.mm(pp[0:3, :], scT[:, k * 3:(k + 1) * 3], wb[:, k * 512:(k + 1) * 512],
                         start=(k == 0), stop=(k == 7), writes=[pp])
                P.v(lambda e, pp=pp, n=n: e.tensor_tensor(ada_sb[:, n * 512:(n + 1) * 512], pp[0:3, :],
                                                          bb[:, n * 512:(n + 1) * 512], ALU.add),
                    [pp, bb], [ada_sb])
            for j in range(3):
                pass
            for m in range(48):
                P.tr(pt[:, m * 3:(m + 1) * 3], ada_sb[0:3, m * 128:(m + 1) * 128], ident[0:3, 0:3],
                     writes=[pt])
            P.v(lambda e: e.tensor_copy(adaT[:].rearrange("p (j m) -> p j m", j=3),
                                        pt[:].rearrange("p (m j) -> p j m", j=3)), [pt], [adaT])
            P.v(lambda e: e.tensor_scalar_add(ada1T[:], adaT[:], 1.0), [adaT], [ada1T])
            P.dma("sync", ADA[l * 3:(l + 1) * 3, :], ada_sb[:])
        P.barrier()
        if kb.dbg.get("stop") == "ada":
            break
        if "adaT" in kb.dbg:
            dd = kb.dout(f"dbg_adaT{l}", [128, 144])
            P.dma("sync", dd, adaT[:])

        if "hy" in kb.mixers:
            with contextlib.ExitStack() as S:
                hyf_phase(kb, S, l, C)
            P.barrier()
        if kb.dbg.get("stop") == "hyf":
            break
        for b in range(NB):
            with contextlib.ExitStack() as S:
                mix_phase(kb, S, l, b, src, XR, w_in, w_out, lnp, small, ident, identb, sel, ADA,
                          acol, a1col)
            P.barrier()
            if kb.dbg.get("stop") in ("gla", "gdn"):
                break
        if kb.dbg.get("stop") in ("xm", "sc", "op", "gla", "gdn"):
            break
        with contextlib.ExitStack() as S:
            moe_phase(kb, S, l, last, XR, out, lnp, w_router, b_router, w_gate, w_up, w_down, bguT,
                      b_down, ident, sel, ADA, acol, a1col)
        P.barrier()
    P.emit()
    return kb


def ln_tile(kb, S_names, t2, gbc, bbc, outt, stats, mv, rstd, key_t2, key_out):
    P = kb.P
    for h in range(2):
        P.v(lambda e, h=h: e.bn_stats(stats[:, h * 6:(h + 1) * 6], t2[:, h * 512:(h + 1) * 512]),
            [key_t2], [stats])
    P.v(lambda e: e.bn_aggr(mv[:], stats[:]), [stats], [mv])
    P.a(lambda e: e.activation(rstd[:], mv[:, 1:2], AF.Sqrt, bias=LN_EPS), [mv], [rstd])
    P.v(lambda e: e.reciprocal(rstd[:], rstd[:]), [rstd], [rstd])
    P.v(lambda e: e.tensor_scalar(t2[:], t2[:], mv[:, 0:1], rstd[:, 0:1], ALU.subtract, ALU.mult),
        [key_t2, mv, rstd], [key_t2])
    P.g(lambda e: e.tensor_tensor(t2[:], t2[:], gbc, ALU.mult), [key_t2, gbc], [key_t2])
    P.g(lambda e: e.tensor_tensor(outt[:], t2[:], bbc, ALU.add), [key_t2, bbc], [key_out])


def gate_bcast(kb, dst, l, j, col0, ADA):
    r = l * 3 + j
    kb.P.dma("sync", dst[:], ADA[r:r + 1, col0:col0 + D].to_broadcast([128, D]))


def mix_phase(kb, S, l, b, src, XR, w_in, w_out, lnp, small, ident, identb, sel, ADA, acol, a1col):
    nc, P = kb.nc, kb.P
    C = kb.C
    tok0 = b * LS
    xmT = kb.sb(S, "xmT", [128, 8 * LS], BF16)
    YT2 = kb.sb(S, "YT2", [128, 2 * LS], BF16)
    xmT3 = xmT[:].rearrange("p (k t) -> p k t", k=8)
    YT23 = YT2[:].rearrange("p (k t) -> p k t", k=2)
    YTDb = kb.YTD[b].rearrange("p (k t) -> p k t", k=8)

    def yt_flush(k0):
        P.dma("sync", YTDb[:, k0:k0 + 2, :], YT23, reads=[("YT2", 0), ("YT2", 1)], writes=[("YTD", k0)])
    groups = [(0, 256, 2)] + [(256 + i * 512, 512, b) for i in range(4)]

    with contextlib.ExitStack() as S1:
        xt = [kb.sb(S1, f"xm_x{i}", [128, 4 * D]) for i in range(2)]
        pb = [kb.ps(S1, f"xm_ps{i}", [128, 512]) for i in range(4)]
        for gi, (t0, n, j) in enumerate(groups):
            x4 = xt[gi % 2]
            nt = n // 128
            P.dma("sync", x4[:, 0:nt * D].rearrange("p (a d) -> p a d", a=nt),
                  src[tok0 + t0: tok0 + t0 + n, :].rearrange("(a p) d -> p a d", p=128), writes=[x4])
            for k in range(8):
                pp = pb[k % 4]
                for a in range(nt):
                    P.tr(pp[:, a * 128:(a + 1) * 128], x4[:, a * D + k * 128: a * D + (k + 1) * 128],
                         ident[:], writes=[pp])
                dst = xmT3[:, k, t0:t0 + n]
                if k % 2 == 0:
                    P.v(lambda e, dst=dst, pp=pp, n=n, j=j, k=k: e.tensor_scalar(
                        dst, pp[:, 0:n], a1col(j, 8 + k), acol(j, k), ALU.mult, ALU.add),
                        [pp, "adaT", "ada1T"], [("xmT", gi)])
                else:
                    P.a(lambda e, dst=dst, pp=pp, n=n, j=j, k=k: e.activation(
                        dst, pp[:, 0:n], AF.Identity, bias=acol(j, k), scale=a1col(j, 8 + k)),
                        [pp, "adaT", "ada1T"], [("xmT", gi)])
    P.barrier()
    if kb.dbg.get("stop") == "xm":
        return
    xm_keys = [("xmT", gi) for gi in range(5)]

    def inproj_fm(S2, c0, ncols, dsts, evac=None):
        nch = (ncols + 127) // 128
        wsl = kb.sb(S2, f"wsl_{c0}", [128, 8 * ncols], BF16)
        P.dma("gpsimd", wsl[:].rearrange("p (k c) -> p k c", k=8),
              w_in[l, :, c0:c0 + ncols].rearrange("(k p) c -> p k c", p=128), writes=[wsl])
        pp2 = [kb.ps(S2, f"ip_ps{c0}_{i}", [128, 512]) for i in range(2)]
        i = 0
        for gi, (t0, n, j) in enumerate(groups):
            for ch in range(nch):
                m = min(128, ncols - ch * 128)
                pp = pp2[i % 2]
                i += 1
                for k in range(8):
                    P.mm(pp[0:m, 0:n], wsl[:, k * ncols + ch * 128: k * ncols + ch * 128 + m],
                         xmT3[:, k, t0:t0 + n], start=(k == 0), stop=(k == 7),
                         reads=[wsl, ("xmT", gi)], writes=[pp])
                d = dsts[ch]
                if i % 2:
                    P.v(lambda e, d=d, pp=pp, m=m, n=n, t0=t0: e.tensor_copy(d[0:m, t0:t0 + n], pp[0:m, 0:n]),
                        [pp], [d])
                else:
                    P.a(lambda e, d=d, pp=pp, m=m, n=n, t0=t0: e.copy(d[0:m, t0:t0 + n], pp[0:m, 0:n]),
                        [pp], [d])

    def conv3(dst, srcp, wcols, bias=None, eng="vector"):
        w0, w1, w2 = wcols
        if bias is None:
            P.op(eng, lambda e: e.tensor_scalar(dst[:], srcp[:], w1, None, ALU.mult), [srcp, small], [dst])
        else:
            P.op(eng, lambda e: e.tensor_scalar(dst[:], srcp[:], w1, bias, ALU.mult, ALU.add),
                 [srcp, small], [dst])
        for (o0, rows, rl) in ((0, 1, CTX), (CTX, SEQ // 64, 64)):
            dv = dst[:, o0:o0 + rows * rl].rearrange("p (r c) -> p r c", r=rows)
            sv = srcp[:, o0:o0 + rows * rl].rearrange("p (r c) -> p r c", r=rows)
            P.v(lambda e, dv=dv, sv=sv, rl=rl: e.scalar_tensor_tensor(
                dv[:, :, 1:rl], sv[:, :, 0:rl - 1], w0, dv[:, :, 1:rl], ALU.mult, ALU.add),
                [srcp, dst, small], [dst])
            P.v(lambda e, dv=dv, sv=sv, rl=rl: e.scalar_tensor_tensor(
                dv[:, :, 0:rl - 1], sv[:, :, 1:rl], w2, dv[:, :, 0:rl - 1], ALU.mult, ALU.add),
                [srcp, dst, small], [dst])

    off = {"hy": 0, "sc": 2, "gla": 4, "gdn": 6}
    for name, k0 in off.items():
        if name not in kb.mixers:
            P.g(lambda e: e.memset(YT2[:], 0.0), [], [("YT2", 0), ("YT2", 1)])
            yt_flush(k0)

    if "hy" in kb.mixers:
        with contextlib.ExitStack() as S2:
            hyena_mixer(kb, S2, l, b, xmT3, YT23, groups, w_in, C, small, identb, conv3, yt_flush)
        P.barrier()
    if kb.dbg.get("stop") == "hy":
        return

    if "sc" in kb.mixers:
        with contextlib.ExitStack() as S2:
            pch = [kb.sb(S2, f"sc_p{i}", [128, LS]) for i in range(6)]
            inproj_fm(S2, SC0, 768, pch)
            cv = kb.sb(S2, "sc_cv", [128, LS])
            for ch in range(2):
                Bc, Cc, hc = pch[ch], pch[2 + ch], pch[4 + ch]
                P.g(lambda e, Cc=Cc, hc=hc: e.tensor_tensor(Cc[:], Cc[:], hc[:], ALU.mult), [Cc, hc], [Cc])
                wc = tuple(small[:, 26 + 3 * ch + k: 27 + 3 * ch + k] for k in range(3))
                conv3(cv, Cc, wc)
                P.v(lambda e, Bc=Bc, ch=ch: e.tensor_tensor(YT23[:, ch, :], Bc[:], cv[:], ALU.mult),
                    [Bc, cv], [("YT2", ch)])
            yt_flush(2)
        P.barrier()
    if kb.dbg.get("stop") == "sc":
        return


    if "gla" in kb.mixers:
        with contextlib.ExitStack() as S2:
            gla_mixer(kb, S2, l, b, xmT3, YTView(YT23, 4), groups, w_in, C, small, identb)
            yt_flush(4)
        P.barrier()
    if kb.dbg.get("stop") == "gla":
        return

    if "gdn" in kb.mixers:
        with contextlib.ExitStack() as S2:
            gdn_mixer(kb, S2, l, b, xmT3, YTView(YT23, 6), groups, w_in, C, small, ident, identb, conv3)
            yt_flush(6)
        P.barrier()
    if kb.dbg.get("stop") == "gdn":
        return

    with contextlib.ExitStack() as S3:
        YT = kb.sb(S3, "YT", [128, 8 * LS], BF16)
        YT3 = YT[:].rearrange("p (k t) -> p k t", k=8)
        P.dma("sync", YT[:], kb.YTD[b], reads=[("YTD", k) for k in (0, 2, 4, 6)], writes=[("YT", k) for k in range(8)])
        if "YT" in kb.dbg and l == 0:
            dd = kb.dout(f"dbg_YT{b}", [128, 8 * LS], BF16)
            P.dma("sync", dd, YT[:], reads=[("YT", k) for k in range(8)])
        wo = kb.sb(S3, "wo", [128, 8 * D], BF16)
        P.dma("gpsimd", wo[:].rearrange("p (k c) -> p k c", k=8),
              w_out[l].rearrange("(k p) c -> p k c", p=128), writes=[wo])
        gbc = kb.sb(S3, "ln1g", [128, D])
        bbc = kb.sb(S3, "ln1b", [128, D])
        P.dma("sync", gbc[:], lnp[l, 0:1, :].to_broadcast([128, D]))
        P.dma("sync", bbc[:], lnp[l, 1:2, :].to_broadcast([128, D]))
        po = [kb.ps(S3, f"op_ps{i}", [128, 512]) for i in range(4)]
        g1 = {}
        for j in (b, 2):
            g1[j] = kb.sb(S3, f"g1bc{j}", [128, D])
            gate_bcast(kb, g1[j], l, j, 2048, ADA)
        xts = [kb.sb(S3, f"op_x{i}", [128, D]) for i in range(2)]
        t1s = [kb.sb(S3, f"op_t{i}", [128, D]) for i in range(2)]
        ots = [kb.sb(S3, f"op_o{i}", [128, D]) for i in range(2)]
        stats = kb.sb(S3, "op_stats", [128, 12])
        mv = kb.sb(S3, "op_mv", [128, 2])
        rstd = kb.sb(S3, "op_rstd", [128, 1])
        yk = [("YT", k) for k in range(8)]
        for ti in range(NT):
            j = 2 if ti < 2 else b
            xt, t1, ot = xts[ti % 2], t1s[ti % 2], ots[ti % 2]
            r0 = tok0 + ti * 128
            P.dma("sync", xt[:], src[r0:r0 + 128, :])
            for h in range(2):
                pp = po[(ti % 2) * 2 + h]
                for k in range(8):
                    P.mm(pp[:, :], YT3[:, k, ti * 128:(ti + 1) * 128], wo[:, k * D + h * 512: k * D + (h + 1) * 512],
                         start=(k == 0), stop=(k == 7), reads=[wo] + yk, writes=[pp])
                P.v(lambda e, pp=pp, t1=t1, h=h, j=j: e.tensor_tensor(
                    t1[:, h * 512:(h + 1) * 512], pp[:, :], g1[j][:, h * 512:(h + 1) * 512], ALU.mult),
                    [pp, g1[j]], [t1])
            P.v(lambda e, xt=xt, t1=t1: e.scalar_tensor_tensor(t1[:], xt[:], float(DN_ALPHA), t1[:],
                                                               ALU.mult, ALU.add), [xt, t1], [t1])
            ln_tile(kb, None, t1, gbc[:], bbc[:], ot, stats, mv, rstd, t1, ot)
            P.dma("sync", XR[r0:r0 + 128, :], ot[:], writes=[("XR", r0 // 128)])


def finish_norm_gate(kb, S2, osum, rtok, nwb, sq_ap, sq_key, ytok, y_key, pT, YT3, k0, identb):
    P = kb.P
    o3 = osum[:].rearrange("p (a v) -> p a v", v=64)
    ms = kb.sb(S2, "fin_ms", [128, NT * 4])
    for hf in range(2):
        P.v(lambda e, hf=hf: e.tensor_tensor(sq_ap, osum[:, hf * LS:(hf + 1) * LS], osum[:, hf * LS:(hf + 1) * LS], ALU.mult),
            [osum], [sq_key])
        P.v(lambda e, hf=hf: e.reduce_sum(ms[:, hf * NT * 2:(hf + 1) * NT * 2], sq_ap.rearrange("p (a v) -> p a v", v=64), AX.X),
            [sq_key], [ms])
    P.a(lambda e: e.activation(ms[:], ms[:], AF.Sqrt, bias=1e-6, scale=1.0 / 64), [ms], [ms])
    P.v(lambda e: e.reciprocal(ms[:], ms[:]), [ms], [ms])
    msbc = ms[:].rearrange("p (a o) -> p a o", o=1).to_broadcast([128, NT * 4, 64])
    nwbc = nwb[:].rearrange("p (o v) -> p o v", o=1).to_broadcast([128, NT * 4, 64])
    P.v(lambda e: e.tensor_tensor(o3, o3, msbc, ALU.mult), [osum, ms], [osum])
    P.g(lambda e: e.tensor_tensor(o3, o3, nwbc, ALU.mult), [osum, nwb], [osum])
    P.v(lambda e: e.tensor_tensor(ytok, osum[:], rtok[:], ALU.mult), [osum, rtok], [y_key])
    for ti in range(NT):
        for c2 in range(2):
            P.tr(pT[:, (ti % 4) * 256 + c2 * 128:(ti % 4) * 256 + (c2 + 1) * 128],
                 ytok[:, ti * 256 + c2 * 128: ti * 256 + (c2 + 1) * 128], identb[:], reads=[y_key, identb], writes=[pT])
            P.v(lambda e, ti=ti, c2=c2: e.tensor_copy(
                YT3[:, k0 + c2, ti * 128:(ti + 1) * 128],
                pT[:, (ti % 4) * 256 + c2 * 128:(ti % 4) * 256 + (c2 + 1) * 128]), [pT], [("YT2", c2)])


def inproj_fm_cols(kb, S2, l, w_in, c0, ncols, xmT3, groups, dst_fn, tag):
    P = kb.P
    nch = (ncols + 127) // 128
    wsl = kb.sb(S2, f"wsl_{tag}", [128, 8 * ncols], BF16)
    P.dma("gpsimd", wsl[:].rearrange("p (k c) -> p k c", k=8),
          w_in[l, :, c0:c0 + ncols].rearrange("(k p) c -> p k c", p=128), writes=[wsl])
    pp2 = [kb.ps(S2, f"ipc_ps{tag}_{i}", [128, 512]) for i in range(2)]
    i = 0
    for gi, (t0, n, j) in enumerate(groups):
        for ch in range(nch):
            m = min(128, ncols - ch * 128)
            pp = pp2[i % 2]
            i += 1
            for k in range(8):
                P.mm(pp[0:m, 0:n], wsl[:, k * ncols + ch * 128: k * ncols + ch * 128 + m],
                     xmT3[:, k, t0:t0 + n], start=(k == 0), stop=(k == 7),
                     reads=[wsl, ("xmT", gi)], writes=[pp])
            dst_fn(ch, t0, n, pp, m)


def inproj_tm_cols(kb, S2, l, w_in, c0, ncols, xmT3, dst_fn, tag):
    P = kb.P
    wsl = kb.sb(S2, f"wslt_{tag}", [128, 8 * ncols], BF16)
    P.dma("gpsimd", wsl[:].rearrange("p (k c) -> p k c", k=8),
          w_in[l, :, c0:c0 + ncols].rearrange("(k p) c -> p k c", p=128), writes=[wsl])
    pp2 = [kb.ps(S2, f"ipt_ps{tag}_{i}", [128, 512]) for i in range(2)]
    for ti in range(NT):
        gi = 0 if ti < 2 else 1 + (ti - 2) // 4
        pp = pp2[ti % 2]
        for k in range(8):
            P.mm(pp[:, 0:ncols], xmT3[:, k, ti * 128:(ti + 1) * 128], wsl[:, k * ncols:(k + 1) * ncols],
                 start=(k == 0), stop=(k == 7), reads=[wsl, ("xmT", gi)], writes=[pp])
        dst_fn(ti, pp)


def chunk_order(d):
    if d == 0:
        return list(range(NT))
    return [1, 0] + list(range(NT - 1, 1, -1))


def gla_mixer(kb, S2, l, b, xmT3, YT3, groups, w_in, C, small, identb):
    nc, P = kb.nc, kb.P
    G0 = GLA0
    qT = kb.sb(S2, "gla_qT", [128, LS])
    kT = kb.sb(S2, "gla_kT", [128, LS])
    vtok = kb.sb(S2, "gla_v", [128, NT * 256], BF16)
    rtok = kb.sb(S2, "gla_r", [128, NT * 256], BF16)
    osum = kb.sb(S2, "gla_o", [128, NT * 256])
    waT = kb.sb(S2, "gla_wa", [16, 256])
    nwb = kb.sb(S2, "gla_nw", [128, 64])
    negs = kb.sb(S2, "gla_negs", [128, 2])
    if kb.dbg.get("stop2") == "g0":
        return
    P.dma("sync", waT[:].rearrange("r (d c) -> r d c", d=2), C["gla_w_a"][l].rearrange("d r c -> r d c"))
    P.dma("sync", nwb[:], C["gla_norm_w"][l:l + 1, :].to_broadcast([128, 64]))
    P.v(lambda e: e.tensor_scalar(negs[:], small[:, 50:52], -1.0, None, ALU.mult), [small], [negs])
    with contextlib.ExitStack() as S3:
        def ev_qk(ch, t0, n, pp, m):
            d = qT if ch == 0 else kT
            P.v(lambda e: e.tensor_copy(d[:, t0:t0 + n], pp[:, 0:n]), [pp], [d])
        inproj_fm_cols(kb, S3, l, w_in, G0, 256, xmT3, groups, ev_qk, "glaqk")
    P.barrier()
    if kb.dbg.get("stop2") == "g0b":
        return
    with contextlib.ExitStack() as S3:
        def ev_vr(ti, pp):
            P.v(lambda e: e.tensor_copy(vtok[:, ti * 256:(ti + 1) * 256], pp[:, 0:256]), [pp], [vtok])
            P.a(lambda e: e.activation(rtok[:, ti * 256:(ti + 1) * 256], pp[:, 256:512], AF.Silu), [pp], [rtok])
        inproj_tm_cols(kb, S3, l, w_in, G0 + 256, 512, xmT3, ev_vr, "glavr")
    P.barrier()
    if kb.dbg.get("stop2") == "g1":
        return
    cm = kb.sb(S2, "gla_cm", [128, LS], BF16)
    P.g(lambda e: e.memset(cm[:], 1.0), [], [cm])
    P.g(lambda e: e.memset(cm[:].rearrange("p (c t) -> p c t", t=128)[:, :, 0:1], 0.0), [cm], [cm])
    msk = kb.sb(S2, "gla_msk", [128, 2 * 512], BF16)
    P.dma("gpsimd", msk[:].rearrange("p (d c) -> p d c", d=2), C["trimask"].rearrange("d p c -> p d c"))
    bdm = kb.sb(S2, "gla_bdm", [128, 256])
    P.dma("sync", bdm[:], C["bdmask"])
    if kb.dbg.get("stop2") == "g2":
        return
    aT = kb.sb(S2, "gla_aT", [16, LS])
    Bt = kb.sb(S2, "gla_B", [128, LS])
    Tm = kb.sb(S2, "gla_T", [128, NT])
    eT = kb.sb(S2, "gla_eT", [128, NT])
    tmp = kb.sb(S2, "gla_tmp", [128, LS])
    qt_ = kb.sb(S2, "gla_qt", [128, LS], BF16)
    kt_ = kb.sb(S2, "gla_kt", [128, LS], BF16)
    qe_ = kb.sb(S2, "gla_qe", [128, LS], BF16)
    kp_ = kb.sb(S2, "gla_kp", [128, LS], BF16)
    kptok = kb.sb(S2, "gla_kptok", [128, NT * 128], BF16)
    ATs = [kb.sb(S2, f"gla_AT{i}", [128, 512], BF16) for i in range(2)]
    Qb = [kb.sb(S2, f"gla_Qb{i}", [128, 512], BF16) for i in range(2)]
    hsel = kb.sb(S2, "gla_hsel", [128, 4])
    P.dma("sync", hsel[:], C["hsel"])
    Sf = kb.sb(S2, "gla_Sf", [128, 256])
    Sb = kb.sb(S2, "gla_Sb", [128, 256], BF16)
    stmp = kb.sb(S2, "gla_stmp", [128, 256])
    pA = [kb.ps(S2, f"gla_pA{i}", [128, 512]) for i in range(2)]
    pO = [kb.ps(S2, f"gla_pO{i}", [128, 512]) for i in range(2)]
    pS = kb.ps(S2, "gla_pS", [128, 512])
    pT = kb.ps(S2, "gla_pT", [128, 1024], BF16)
    ex = kb.sb(S2, "gla_ex", [128, LS])
    B3 = Bt[:].rearrange("p (c t) -> p c t", t=128)
    tmp3 = tmp[:].rearrange("p (c t) -> p c t", t=128)
    SC = 32 ** -0.5
    for d in range(2):
        with contextlib.ExitStack() as S3:
            def ev_a(ch, t0, n, pp, m):
                P.v(lambda e: e.tensor_copy(aT[:, t0:t0 + n], pp[0:16, 0:n]), [pp], [aT])
            inproj_fm_cols(kb, S3, l, w_in, G0 + 768 + 16 * d, 16, xmT3, groups, ev_a, f"glaa{d}")
            pg = pA
            for gi, (t0, n, j) in enumerate(groups):
                pp = pg[gi % 2]
                P.mm(pp[:, 0:n], waT[:, d * 128:(d + 1) * 128], aT[:, t0:t0 + n])
                P.a(lambda e, pp=pp, t0=t0, n=n, d=d: e.activation(tmp[:, t0:t0 + n], pp[:, 0:n], AF.Exp,
                                                              bias=negs[:, d:d + 1], scale=-1.0),
                    [pp, negs], [tmp])
            P.a(lambda e: e.activation(tmp[:], tmp[:], AF.Ln, bias=1.0), [tmp], [tmp])
        P.barrier()
        if kb.dbg.get("stop2") == "g3":
            return
        P.v(lambda e: e.tensor_tensor_scan(Bt[:], cm[:], tmp[:], 0.0, ALU.mult, ALU.add), [cm, tmp], [Bt])
        if kb.dbg.get("stop2") == "g3b":
            return
        P.v(lambda e: e.tensor_copy(Tm[:].rearrange("p (c o) -> p c o", o=1), B3[:, :, 127:128]), [Bt], [Tm])
        Tbc = Tm[:].rearrange("p (c o) -> p c o", o=1).to_broadcast([128, NT, 128])
        if d == 1:
            P.v(lambda e: e.tensor_tensor(B3, Tbc, B3, ALU.subtract), [Tm, Bt], [Bt])
            P.g(lambda e: e.tensor_tensor(Bt[:], Bt[:], tmp[:], ALU.add), [Bt, tmp], [Bt])
        P.a(lambda e: e.activation(eT[:], Tm[:], AF.Exp, scale=-1.0 / 16), [Tm], [eT])
        Rbc = B3[:, :, 64:65].to_broadcast([128, NT, 128])
        P.v(lambda e: e.tensor_tensor(tmp3, B3, Rbc, ALU.subtract), [Bt], [tmp])
        P.a(lambda e: e.activation(ex[:], tmp[:], AF.Exp, scale=-1.0 / 16), [tmp], [ex])
        P.v(lambda e: e.scalar_tensor_tensor(qt_[:], qT[:], SC, ex[:], ALU.mult, ALU.mult), [qT, ex], [qt_])
        P.a(lambda e: e.activation(ex[:], tmp[:], AF.Exp, scale=1.0 / 16), [tmp, qt_], [ex])
        P.g(lambda e: e.tensor_tensor(kt_[:], kT[:], ex[:], ALU.mult), [kT, ex], [kt_])
        P.a(lambda e: e.activation(ex[:], Bt[:], AF.Exp, scale=-1.0 / 16), [Bt, kt_], [ex])
        P.v(lambda e: e.scalar_tensor_tensor(qe_[:], qT[:], SC, ex[:], ALU.mult, ALU.mult), [qT, ex], [qe_])
        P.v(lambda e: e.tensor_tensor(tmp3, Tbc, B3, ALU.subtract), [Tm, Bt], [tmp])
        P.a(lambda e: e.activation(ex[:], tmp[:], AF.Exp, scale=-1.0 / 16), [tmp, qe_], [ex])
        P.g(lambda e: e.tensor_tensor(kp_[:], kT[:], ex[:], ALU.mult), [kT, ex], [kp_])
        if kb.dbg.get("stop2") == "g4":
            return
        for c4 in range(0, NT, 8):
            nn = min(8, NT - c4)
            for cc in range(nn):
                c = c4 + cc
                P.tr(pT[:, cc * 128:(cc + 1) * 128], kp_[:, c * 128:(c + 1) * 128], identb[:], writes=[pT])
            P.v(lambda e, c4=c4, nn=nn: e.tensor_copy(kptok[:, c4 * 128:(c4 + nn) * 128], pT[:, 0:nn * 128]),
                [pT], [kptok])
        if kb.dbg.get("stop2") == "g5":
            return
        P.g(lambda e: e.memset(Sf[:], 0.0), [], [Sf])
        P.g(lambda e: e.memset(Sb[:], 0.0), [], [Sb])
        for ci, c in enumerate(chunk_order(d)):
            cs = slice(c * 128, (c + 1) * 128)
            pa, po, at = pA[ci % 2], pO[ci % 2], ATs[ci % 2]
            qb = Qb[ci % 2]
            P.g(lambda e, qb=qb, cs=cs: e.tensor_tensor(
                qb[:].rearrange("p (h i) -> p h i", h=4),
                qt_[:, cs].rearrange("p (o i) -> p o i", o=1).to_broadcast([128, 4, 128]),
                hsel[:].rearrange("p (h o) -> p h o", o=1).to_broadcast([128, 4, 128]), ALU.mult),
                [qt_, hsel], [qb])
            P.mm(pa[:, :], kt_[:, cs], qb[:], writes=[pa])
            P.v(lambda e, pa=pa, at=at, d=d: e.tensor_tensor(at[:], pa[:], msk[:, d * 512:(d + 1) * 512], ALU.mult),
                [pa, msk], [at])
            P.mm(po[:, 0:256], qe_[:, cs], Sb[:], start=True, stop=False, writes=[po])
            for h in range(4):
                P.mm(po[:, h * 64:(h + 1) * 64], at[:, h * 128:(h + 1) * 128],
                     vtok[:, c * 256 + h * 64: c * 256 + (h + 1) * 64], start=False, stop=(h == 3),
                     writes=[po])
            osl = osum[:, c * 256:(c + 1) * 256]
            if d == 0:
                P.a(lambda e, osl=osl, po=po: e.copy(osl, po[:, 0:256]), [po], [osum])
            else:
                P.v(lambda e, osl=osl, po=po: e.tensor_tensor(osl, osl, po[:, 0:256], ALU.add), [po, osum], [osum])
            P.mm(pS[:, 0:256], kptok[:, cs], vtok[:, c * 256:(c + 1) * 256])
            P.v(lambda e: e.tensor_tensor(stmp[:], pS[:, 0:256], bdm[:], ALU.mult), [pS, bdm], [stmp])
            P.v(lambda e, c=c: e.scalar_tensor_tensor(Sf[:], Sf[:], eT[:, c:c + 1], stmp[:], ALU.mult, ALU.add),
                [Sf, eT, stmp], [Sf])
            P.g(lambda e: e.tensor_copy(Sb[:], Sf[:]), [Sf], [Sb])
    if kb.dbg.get("stop2") == "pre":
        return
    finish_norm_gate(kb, S2, osum, rtok, nwb, tmp[:], tmp, Bt[:].bitcast(BF16), Bt, pT, YT3, 4, identb)


def gdn_mixer(kb, S2, l, b, xmT3, YT3, groups, w_in, C, small, ident, identb, conv3):
    nc, P = kb.nc, kb.P
    G0 = GDN0
    qh = [kb.sb(S2, f"gd_qh{i}", [128, LS], BF16) for i in range(2)]
    kh = [kb.sb(S2, f"gd_kh{i}", [128, LS], BF16) for i in range(2)]
    ktok = kb.sb(S2, "gd_ktok", [128, NT * 256], BF16)
    vtok = kb.sb(S2, "gd_vtok", [128, NT * 256], BF16)
    ztok = kb.sb(S2, "gd_ztok", [128, NT * 256], BF16)
    osum = kb.sb(S2, "gd_osum", [128, NT * 256])
    nwb = kb.sb(S2, "gd_nw", [128, 64])
    gcol = kb.sb(S2, "gd_gcol", [128, NT * 48])
    colE = kb.sb(S2, "gd_colE", [128, NT * 8])
    colA = kb.sb(S2, "gd_colA", [128, NT * 8])
    colK = kb.sb(S2, "gd_colK", [128, NT * 8])
    eTot = kb.sb(S2, "gd_eTot", [128, 4 * NT])
    qe = [[kb.sb(S2, f"gd_qe{d}{i}", [128, LS], BF16) for i in range(2)] for d in range(2)]
    Crow = kb.sb(S2, "gd_Crow", [16, LS])
    gc3 = gcol[:].rearrange("p (c x) -> p c x", x=48)
    P.dma("sync", nwb[:], C["gdn_norm_w"][l:l + 1, :].to_broadcast([128, 64]))
    with contextlib.ExitStack() as S3:
        pbuf = kb.sb(S3, "gd_pbuf", [128, LS])
        cbuf = kb.sb(S3, "gd_cbuf", [128, LS])
        bones = kb.sb(S3, "gd_bones", [128, 128])
        P.dma("sync", bones[:], C["bones"])
        wsl = kb.sb(S3, "gd_wsl", [128, 8 * 768], BF16)
        P.dma("gpsimd", wsl[:].rearrange("p (k c) -> p k c", k=8),
              w_in[l, :, G0:G0 + 768].rearrange("(k p) c -> p k c", p=128), writes=[wsl])
        pp2 = [kb.ps(S3, f"gd_ps{i}", [128, 512]) for i in range(2)]
        pn2 = [kb.ps(S3, f"gd_pn{i}", [128, 512]) for i in range(2)]
        ptb = kb.ps(S3, "gd_ptb", [128, 1024], BF16)
        i = 0
        for ch in range(6):
            for gi, (t0, n, j) in enumerate(groups):
                pp = pp2[i % 2]
                i += 1
                for k in range(8):
                    P.mm(pp[:, 0:n], wsl[:, k * 768 + ch * 128: k * 768 + (ch + 1) * 128],
                         xmT3[:, k, t0:t0 + n], start=(k == 0), stop=(k == 7),
                         reads=[wsl, ("xmT", gi)], writes=[pp])
                P.a(lambda e, pp=pp, t0=t0, n=n: e.copy(pbuf[:, t0:t0 + n], pp[:, 0:n]), [pp], [pbuf])
            wc = tuple(small[:, 32 + 3 * ch + k: 33 + 3 * ch + k] for k in range(3))
            conv3(cbuf, pbuf, wc)
            P.a(lambda e: e.activation(cbuf[:], cbuf[:], AF.Silu), [cbuf], [cbuf])
            if ch < 4:
                P.g(lambda e: e.tensor_tensor(pbuf[:], cbuf[:], cbuf[:], ALU.mult), [cbuf], [pbuf])
                for gi, (t0, n, j) in enumerate(groups):
                    pn = pn2[gi % 2]
                    P.mm(pn[:, 0:n], bones[:], pbuf[:, t0:t0 + n], writes=[pn])
                    P.a(lambda e, pn=pn, t0=t0, n=n: e.activation(pbuf[:, t0:t0 + n], pn[:, 0:n], AF.Sqrt, bias=1e-6),
                        [pn], [pbuf])
                P.v(lambda e: e.reciprocal(pbuf[:], pbuf[:]), [pbuf], [pbuf])
                dst = qh[ch] if ch < 2 else kh[ch - 2]
                sc_ = 0.125 if ch < 2 else 1.0
                P.v(lambda e, dst=dst, sc_=sc_: e.scalar_tensor_tensor(dst[:], cbuf[:], sc_, pbuf[:], ALU.mult, ALU.mult),
                    [cbuf, pbuf], [dst])
            else:
                P.v(lambda e: e.tensor_copy(pbuf[:].bitcast(BF16)[:, 0:LS], cbuf[:]), [cbuf], [pbuf])
                vb16 = pbuf[:].bitcast(BF16)
                for c4 in range(0, NT, 8):
                    nn = min(8, NT - c4)
                    for cc in range(nn):
                        c = c4 + cc
                        P.tr(ptb[:, cc * 128:(cc + 1) * 128], vb16[:, c * 128:(c + 1) * 128], identb[:],
                             reads=[pbuf, identb], writes=[ptb])
                    P.v(lambda e, c4=c4, nn=nn, ch=ch: e.tensor_copy(
                        vtok[:].rearrange("p (c x) -> p c x", x=256)[:, c4:c4 + nn, (ch - 4) * 128:(ch - 3) * 128],
                        ptb[:, 0:nn * 128].rearrange("p (c x) -> p c x", x=128)), [ptb], [vtok])
        for hc in range(2):
            for c4 in range(0, NT, 8):
                nn = min(8, NT - c4)
                for cc in range(nn):
                    c = c4 + cc
                    P.tr(ptb[:, cc * 128:(cc + 1) * 128], kh[hc][:, c * 128:(c + 1) * 128], identb[:], writes=[ptb])
                P.v(lambda e, c4=c4, nn=nn, hc=hc: e.tensor_copy(
                    ktok[:].rearrange("p (c x) -> p c x", x=256)[:, c4:c4 + nn, hc * 128:(hc + 1) * 128],
                    ptb[:, 0:nn * 128].rearrange("p (c x) -> p c x", x=128)), [ptb], [ktok])
    P.barrier()
    if kb.dbg.get("stop2") == "s1":
        return
    with contextlib.ExitStack() as S3:
        def ev_z(ti, pp):
            P.a(lambda e: e.activation(ztok[:, ti * 256:(ti + 1) * 256], pp[:, 0:256], AF.Silu), [pp], [ztok])
        inproj_tm_cols(kb, S3, l, w_in, G0 + 768, 256, xmT3, ev_z, "gdz")
    P.barrier()
    if kb.dbg.get("stop2") == "s2":
        return
    with contextlib.ExitStack() as S3:
        gT = kb.sb(S3, "gd_gT", [16, LS])
        SG = kb.sb(S3, "gd_SG", [16, LS])
        PRE = kb.sb(S3, "gd_PRE", [16, LS])
        SUF = kb.sb(S3, "gd_SUF", [16, LS])
        Rr = kb.sb(S3, "gd_R", [16, LS])
        cm = kb.sb(S3, "gd_cm", [16, LS], BF16)
        Tt = kb.sb(S3, "gd_T", [16, NT])
        ea = kb.sb(S3, "gd_ea", [16, 1])
        gsel = kb.sb(S3, "gd_gsel", [16, 2])
        selq = kb.sb(S3, "gd_selq", [16, 4 * 128])
        exb = kb.sb(S3, "gd_exb", [128, 512])
        P.dma("sync", gsel[:], C["gsel"])
        P.dma("sync", selq[:], C["selq"])
        P.g(lambda e: e.memset(cm[:], 1.0), [], [cm])
        P.g(lambda e: e.memset(cm[:].rearrange("p (c t) -> p c t", t=128)[:, :, 0:1], 0.0), [cm], [cm])
        P.a(lambda e: e.activation(ea[:], small[0:16, 53:54], AF.Exp), [small], [ea])
        with contextlib.ExitStack() as S4:
            def ev_g(ch, t0, n, pp, m):
                P.v(lambda e: e.tensor_copy(gT[:, t0:t0 + n], pp[0:16, 0:n]), [pp], [gT])
            inproj_fm_cols(kb, S4, l, w_in, G0 + 1024, 16, xmT3, groups, ev_g, "gdg")
        P.barrier()
        P.a(lambda e: e.activation(SG[:], gT[:], AF.Sigmoid), [gT], [SG])
        P.a(lambda e: e.activation(gT[:], gT[:], AF.Exp, bias=small[0:16, 52:53]), [gT, small, SG], [gT])
        P.a(lambda e: e.activation(gT[:], gT[:], AF.Ln, bias=1.0), [gT], [gT])
        P.v(lambda e: e.tensor_scalar(gT[:], gT[:], ea[:, 0:1], None, ALU.mult), [gT, ea], [gT])
        P.v(lambda e: e.tensor_tensor_scan(PRE[:], cm[:], gT[:], 0.0, ALU.mult, ALU.add), [cm, gT], [PRE])
        P3 = PRE[:].rearrange("p (c t) -> p c t", t=128)
        S3v = SUF[:].rearrange("p (c t) -> p c t", t=128)
        P.v(lambda e: e.tensor_copy(Tt[:].rearrange("p (c o) -> p c o", o=1), P3[:, :, 127:128]), [PRE], [Tt])
        Tbc = Tt[:].rearrange("p (c o) -> p c o", o=1).to_broadcast([16, NT, 128])
        P.v(lambda e: e.tensor_tensor(S3v, Tbc, P3, ALU.subtract), [Tt, PRE], [SUF])
        P.v(lambda e: e.tensor_tensor(SUF[:], SUF[:], gT[:], ALU.add), [SUF, gT], [SUF])
        P.v(lambda e: e.tensor_scalar(Rr[:], SUF[:], gsel[:, 0:1], None, ALU.mult), [SUF, gsel], [Rr])
        P.v(lambda e: e.scalar_tensor_tensor(Rr[:], PRE[:], gsel[:, 1:2], Rr[:], ALU.mult, ALU.add), [PRE, gsel, Rr], [Rr])
        P.v(lambda e: e.tensor_tensor(Rr[:], Rr[:], gT[:], ALU.subtract), [Rr, gT], [Rr])
        P.v(lambda e: e.tensor_scalar(Crow[:], PRE[:], gsel[:, 0:1], None, ALU.mult), [PRE, gsel], [Crow])
        P.v(lambda e: e.scalar_tensor_tensor(Crow[:], SUF[:], gsel[:, 1:2], Crow[:], ALU.mult, ALU.add),
            [SUF, gsel, Crow], [Crow])
        pg = [kb.ps(S3, f"gd_pg{i}", [128, 512]) for i in range(2)]
        for c in range(NT):
            pp = pg[c % 2]
            cs = slice(c * 128, (c + 1) * 128)
            P.tr(pp[:, 0:16], Crow[:, cs], ident[0:16, 0:16], writes=[pp])
            P.tr(pp[:, 16:32], Rr[:, cs], ident[0:16, 0:16], writes=[pp])
            P.tr(pp[:, 32:48], SG[:, cs], ident[0:16, 0:16], writes=[pp])
            P.v(lambda e, pp=pp, c=c: e.tensor_copy(gcol[:, c * 48:(c + 1) * 48], pp[:, 0:48]), [pp], [gcol])
        i = 0
        for d in range(2):
            for hc in range(2):
                sq_ = selq[:, (d * 2 + hc) * 128:(d * 2 + hc + 1) * 128]
                for gi, (t0, n, j) in enumerate(groups):
                    pp = pg[i % 2]
                    i += 1
                    P.mm(pp[:, 0:n], sq_, Crow[:, t0:t0 + n], writes=[pp])
                    P.a(lambda e, pp=pp, n=n: e.activation(exb[:, 0:n], pp[:, 0:n], AF.Exp, scale=-1.0), [pp], [exb])
                    P.v(lambda e, d=d, hc=hc, t0=t0, n=n: e.tensor_tensor(qe[d][hc][:, t0:t0 + n], qh[hc][:, t0:t0 + n],
                                                                        exb[:, 0:n], ALU.mult), [qh[hc], exb], [qe[d][hc]])
                pp = pg[i % 2]
                i += 1
                P.mm(pp[:, 0:NT], sq_, Tt[:], writes=[pp])
                P.a(lambda e, pp=pp, d=d, hc=hc: e.activation(eTot[:, (d * 2 + hc) * NT:(d * 2 + hc + 1) * NT], pp[:, 0:NT],
                                                              AF.Exp, scale=-1.0), [pp], [eTot])
    P.barrier()
    if kb.dbg.get("stop2") == "s3":
        return
    cE3 = colE[:].rearrange("p (c r) -> p c r", r=8)
    P.a(lambda e: e.activation(cE3, gc3[:, :, 0:8], AF.Exp, scale=-1.0), [gcol], [colE])
    P.v(lambda e: e.tensor_tensor(colA[:].rearrange("p (c r) -> p c r", r=8), cE3, gc3[:, :, 40:48], ALU.mult),
        [colE, gcol], [colA])
    P.a(lambda e: e.activation(colK[:].rearrange("p (c r) -> p c r", r=8), gc3[:, :, 16:24], AF.Exp, scale=-1.0),
        [gcol], [colK])
    if kb.dbg.get("stop2") == "s4":
        return
    selr = kb.sb(S2, "gd_selr", [16, 8 * 128])
    gmask = kb.sb(S2, "gd_gmask", [128, 4 * 128])
    id4 = kb.sb(S2, "gd_id4", [128, 512], BF16)
    P.dma("sync", selr[:], C["selr"])
    P.dma("sync", gmask[:].rearrange("p (m c) -> p m c", m=4), C["gmask"].rearrange("m p c -> p m c"))
    for h in range(4):
        P.v(lambda e, h=h: e.tensor_copy(id4[:, h * 128:(h + 1) * 128], identb[:]), [identb], [id4])
    u1 = kb.sb(S2, "gd_u1", [128, 512])
    dec = kb.sb(S2, "gd_dec", [128, 512])
    decT = kb.sb(S2, "gd_decT", [128, 512])
    Ab = [kb.sb(S2, f"gd_A{i}", [128, 512], BF16) for i in range(2)]
    ATb = [kb.sb(S2, f"gd_AT{i}", [128, 512], BF16) for i in range(2)]
    TTb = [kb.sb(S2, f"gd_TT{i}", [128, 512], BF16) for i in range(2)]
    attnT = kb.sb(S2, "gd_attnT", [128, 512], BF16)
    smask = kb.sb(S2, "gd_smask", [128, 5 * 128], BF16)
    P.dma("gpsimd", smask[:].rearrange("p (m c) -> p m c", m=5), C["smask"].rearrange("m p c -> p m c"))
    Bm = kb.sb(S2, "gd_Bm", [128, 512], BF16)
    BTm = kb.sb(S2, "gd_BTm", [128, 512], BF16)
    Xa = [kb.sb(S2, f"gd_Xa{i}", [128, 512], BF16) for i in range(3)]
    Xb = [kb.sb(S2, f"gd_Xb{i}", [128, 512], BF16) for i in range(3)]
    Pa = [kb.sb(S2, f"gd_Pa{i}", [128, 512], BF16) for i in range(2)]
    Pb = [kb.sb(S2, f"gd_Pb{i}", [128, 512], BF16) for i in range(2)]
    Wa = kb.sb(S2, "gd_Wa", [128, 512], BF16)
    Wb = kb.sb(S2, "gd_Wb", [128, 512], BF16)
    TTl = [kb.sb(S2, f"gd_TTl{i}", [128, 512], BF16) for i in range(2)]
    Tl = [kb.sb(S2, f"gd_Tl{i}", [128, 512], BF16) for i in range(2)]
    vb_ = kb.sb(S2, "gd_vb", [128, 256], BF16)
    kbe = kb.sb(S2, "gd_kbe", [128, 256], BF16)
    kdec = kb.sb(S2, "gd_kdec", [128, 256], BF16)
    wTn = kb.sb(S2, "gd_wTn", [128, 256], BF16)
    vn = kb.sb(S2, "gd_vn", [128, 256], BF16)
    Sf = kb.sb(S2, "gd_Sf", [128, 256])
    Sb = kb.sb(S2, "gd_Sb", [128, 256], BF16)
    bA = kb.ps(S2, "gd_bA", [128, 512])
    bB = kb.ps(S2, "gd_bB", [128, 512])
    bC = kb.ps(S2, "gd_bC", [128, 512])
    bT = kb.ps(S2, "gd_bT", [128, 1024], BF16)
    bW = kb.ps(S2, "gd_bW", [128, 512])
    bV = kb.ps(S2, "gd_bV", [128, 512])
    bO = kb.ps(S2, "gd_bO", [128, 512])
    bS = kb.ps(S2, "gd_bS", [128, 512])
    H4 = lambda t: t[:].rearrange("p (h x) -> p h x", h=4)
    hp = lambda h: slice((h % 2) * 64, (h % 2) * 64 + 64)
    for d in range(2):
        r0 = 4 * d
        P.g(lambda e: e.memset(Sf[:], 0.0), [], [Sf])
        P.g(lambda e: e.memset(Sb[:], 0.0), [], [Sb])
        for ci, c in enumerate(chunk_order(d)):
            cs = slice(c * 128, (c + 1) * 128)
            for h in range(4):
                P.mm(bA[:, h * 128:(h + 1) * 128], selr[:, (r0 + h) * 128:(r0 + h + 1) * 128], Crow[:, cs], writes=[bA])
            for h in range(4):
                P.v(lambda e, h=h, c=c, d=d, r0=r0: e.scalar_tensor_tensor(
                    u1[:, h * 128:(h + 1) * 128], bA[:, h * 128:(h + 1) * 128], gcol[:, c * 48 + r0 + h: c * 48 + r0 + h + 1],
                    gmask[:, d * 128:(d + 1) * 128], ALU.subtract, ALU.add), [bA, gcol, gmask], [u1])
            P.a(lambda e: e.activation(dec[:], u1[:], AF.Exp), [u1], [dec])
            for h in range(4):
                P.v(lambda e, h=h, c=c, d=d, r0=r0: e.scalar_tensor_tensor(
                    u1[:, h * 128:(h + 1) * 128], bA[:, h * 128:(h + 1) * 128], gcol[:, c * 48 + r0 + h: c * 48 + r0 + h + 1],
                    gmask[:, (2 + d) * 128:(3 + d) * 128], ALU.subtract, ALU.add), [bA, gcol, gmask, dec], [u1])
            P.a(lambda e: e.activation(decT[:], u1[:], AF.Exp, scale=-1.0), [u1], [decT])
            if kb.dbg.get("stop2") == "gdA":
                return
            kkb = (bB, bW)
            qkb = (bC, bV)
            for h in range(4):
                P.mm(kkb[h % 2][:, h * 128:(h + 1) * 128], kh[h // 2][hp(h), cs], kh[h // 2][hp(h), cs], writes=[kkb[h % 2]])
            for h in range(4):
                P.mm(qkb[h % 2][:, h * 128:(h + 1) * 128], kh[h // 2][hp(h), cs], qh[h // 2][hp(h), cs], writes=[qkb[h % 2]])
            A_, AT_ = Ab[0], ATb[0]
            for h in range(4):
                P.v(lambda e, h=h, c=c, r0=r0, A_=A_, kk=kkb[h % 2]: e.scalar_tensor_tensor(
                    A_[:, h * 128:(h + 1) * 128], kk[:, h * 128:(h + 1) * 128],
                    gcol[:, c * 48 + 40 + r0 + h: c * 48 + 40 + r0 + h + 1], dec[:, h * 128:(h + 1) * 128],
                    ALU.mult, ALU.mult), [kkb[h % 2], gcol, dec], [A_])
            for h in range(4):
                P.v(lambda e, h=h, qk=qkb[h % 2]: e.tensor_tensor(attnT[:, h * 128:(h + 1) * 128], qk[:, h * 128:(h + 1) * 128],
                                                                 decT[:, h * 128:(h + 1) * 128], ALU.mult),
                    [qkb[h % 2], decT], [attnT])
            for h in range(4):
                P.tr(bT[:, h * 128:(h + 1) * 128], A_[:, h * 128:(h + 1) * 128], identb[:], writes=[bT])
            P.a(lambda e, AT_=AT_: e.copy(AT_[:], bT[:, 0:512]), [bT], [AT_])
            if kb.dbg.get("stop2") == "gdB":
                return
            H = lambda t: t[:].rearrange("p (h x) -> p h x", h=4)
            mk = lambda m: smask[:, m * 128:(m + 1) * 128].rearrange("p (o x) -> p o x", o=1).to_broadcast([128, 4, 128])
            P.g(lambda e: e.tensor_tensor(H(Bm), H(A_), mk(0), ALU.mult), [A_, smask], [Bm])
            P.v(lambda e: e.tensor_tensor(H(BTm), bT[:, 0:512].rearrange("p (h x) -> p h x", h=4), mk(0), ALU.mult),
                [bT, smask], [BTm])
            P.g(lambda e: e.tensor_tensor(Xa[0][:], id4[:], Bm[:], ALU.subtract), [id4, Bm], [Xa[0]])
            P.v(lambda e: e.tensor_tensor(Xb[0][:], id4[:], BTm[:], ALU.subtract), [id4, BTm], [Xb[0]])

            def mm4(bank, lhs, rhs):
                for h in range(4):
                    hs = slice(h * 128, (h + 1) * 128)
                    P.mm(bank[:, hs], lhs[:, hs], rhs[:, hs], writes=[bank])
            Pw, PwT = Bm, BTm
            X, XT = Xa[0], Xb[0]
            for lv in range(2):
                Pn, PnT = Pa[lv], Pb[lv]
                mm4(bA, PwT, Pw)
                mm4(bB, Pw, PwT)
                P.a(lambda e, Pn=Pn: e.copy(Pn[:], bA[:]), [bA], [Pn])
                P.v(lambda e, PnT=PnT: e.tensor_copy(PnT[:], bB[:]), [bB], [PnT])
                Xn, XnT = Xa[lv + 1], Xb[lv + 1]
                mm4(bC, PnT, X)
                mm4(bA, X, PnT)
                P.v(lambda e, Xn=Xn, X=X: e.tensor_tensor(Xn[:], bC[:], X[:], ALU.add), [bC, X], [Xn])
                P.v(lambda e, XnT=XnT, XT=XT: e.tensor_tensor(XnT[:], bA[:], XT[:], ALU.add), [bA, XT], [XnT])
                Pw, PwT, X, XT = Pn, PnT, Xn, XnT
            Tm_, TT = X, XT
            for li in range(4):
                last_lv = (li == 3)
                mm4(bB, A_, TT)
                P.a(lambda e: e.copy(Wa[:], bB[:]), [bB], [Wa])
                if not last_lv:
                    mm4(bC, AT_, Tm_)
                    P.v(lambda e: e.tensor_copy(Wb[:], bC[:]), [bC], [Wb])
                mm4(bA, Tm_, Wa)
                TTn = TTl[li % 2]
                P.v(lambda e, li=li: e.tensor_tensor(H(Wa), bA[:].rearrange("p (h x) -> p h x", h=4), mk(1 + li), ALU.mult),
                    [bA, smask], [Wa])
                P.g(lambda e, TTn=TTn, TT=TT: e.tensor_tensor(TTn[:], Wa[:], TT[:], ALU.add), [Wa, TT], [TTn])
                if not last_lv:
                    mm4(bB, TT, Wb)
                    Tn = Tl[li % 2]
                    P.v(lambda e, li=li: e.tensor_tensor(H(Wb), bB[:].rearrange("p (h x) -> p h x", h=4), mk(1 + li), ALU.mult),
                        [bB, smask], [Wb])
                    P.g(lambda e, Tn=Tn, Tm_=Tm_: e.tensor_tensor(Tn[:], Wb[:], Tm_[:], ALU.add), [Wb, Tm_], [Tn])
                    Tm_ = Tn
                TT = TTn
            if kb.dbg.get("stop2") == "gdk1":
                P.v(lambda e, TT=TT: e.tensor_copy(TTb[0][:], TT[:]), [TT], [TTb[0]])
                return
            cA = colA[:, c * 8 + r0: c * 8 + r0 + 4].rearrange("p (h o) -> p h o", o=1).to_broadcast([128, 4, 64])
            cB = gcol[:, c * 48 + 40 + r0: c * 48 + 44 + r0].rearrange("p (h o) -> p h o", o=1).to_broadcast([128, 4, 64])
            cK = colK[:, c * 8 + r0: c * 8 + r0 + 4].rearrange("p (h o) -> p h o", o=1).to_broadcast([128, 4, 64])
            k4 = ktok[:, c * 256:(c + 1) * 256].rearrange("p (h x) -> p h x", h=4)
            v4 = vtok[:, c * 256:(c + 1) * 256].rearrange("p (h x) -> p h x", h=4)
            P.g(lambda e, v4=v4, cB=cB: e.tensor_tensor(H4(vb_), v4, cB, ALU.mult), [vtok, gcol], [vb_])
            P.g(lambda e, k4=k4, cA=cA: e.tensor_tensor(H4(kbe), k4, cA, ALU.mult), [ktok, colA], [kbe])
            P.g(lambda e, k4=k4, cK=cK: e.tensor_tensor(H4(kdec), k4, cK, ALU.mult), [ktok, colK], [kdec])
            for h in range(4):
                P.mm(bW[hp(h), (h // 2) * 128:(h // 2 + 1) * 128], kbe[:, h * 64:(h + 1) * 64], TT[:, h * 128:(h + 1) * 128],
                     writes=[bW])
            P.v(lambda e: e.tensor_scalar(wTn[:], bW[:, 0:256], -1.0, None, ALU.mult), [bW], [wTn])
            if kb.dbg.get("stop2") == "gd1":
                return
            for hc in range(2):
                P.mm(bV[:, hc * 128:(hc + 1) * 128], wTn[:, hc * 128:(hc + 1) * 128], Sb[:, hc * 128:(hc + 1) * 128],
                     start=True, stop=False, writes=[bV])
                for hh in range(2):
                    h = hc * 2 + hh
                    P.mm(bV[:, h * 64:(h + 1) * 64], TT[:, h * 128:(h + 1) * 128], vb_[:, h * 64:(h + 1) * 64],
                         start=False, stop=(hh == 1), writes=[bV])
            P.a(lambda e: e.copy(vn[:], bV[:, 0:256]), [bV], [vn])
            for hc in range(2):
                P.mm(bO[:, hc * 128:(hc + 1) * 128], qe[d][hc][:, cs], Sb[:, hc * 128:(hc + 1) * 128],
                     start=True, stop=False, writes=[bO])
                for hh in range(2):
                    h = hc * 2 + hh
                    P.mm(bO[:, h * 64:(h + 1) * 64], attnT[:, h * 128:(h + 1) * 128], vn[:, h * 64:(h + 1) * 64],
                         start=False, stop=(hh == 1), writes=[bO])
            osl = osum[:, c * 256:(c + 1) * 256]
            if d == 0:
                P.a(lambda e, osl=osl: e.copy(osl, bO[:, 0:256]), [bO], [osum])
            else:
                P.v(lambda e, osl=osl: e.tensor_tensor(osl, osl, bO[:, 0:256], ALU.add), [bO, osum], [osum])
            for h in range(4):
                P.mm(bS[hp(h), h * 64:(h + 1) * 64], kdec[:, h * 64:(h + 1) * 64], vn[:, h * 64:(h + 1) * 64], writes=[bS])
            for hh in range(2):
                ps_ = slice(hh * 64, (hh + 1) * 64)
                Sv = Sf[ps_, :].rearrange("p (c x) -> p c x", c=2)[:, :, hh * 64:(hh + 1) * 64]
                Sbv = Sb[ps_, :].rearrange("p (c x) -> p c x", c=2)[:, :, hh * 64:(hh + 1) * 64]
                bSv = bS[ps_, 0:256].rearrange("p (c x) -> p c x", c=2)[:, :, hh * 64:(hh + 1) * 64]
                eTb = eTot[ps_, :].rearrange("p (x c) -> p x c", c=NT)[:, 2 * d:2 * d + 2, c:c + 1].to_broadcast([64, 2, 64])
                P.v(lambda e, Sv=Sv, eTb=eTb: e.tensor_tensor(Sv, Sv, eTb, ALU.mult), [Sf, eTot], [Sf])
                P.v(lambda e, Sv=Sv, bSv=bSv: e.tensor_tensor(Sv, Sv, bSv, ALU.add), [Sf, bS], [Sf])
                P.g(lambda e, Sv=Sv, Sbv=Sbv: e.tensor_copy(Sbv, Sv), [Sf], [Sb])
    if kb.dbg.get("stop2") == "pre":
        return
    finish_norm_gate(kb, S2, osum, ztok, nwb, vtok[:].bitcast(F32), vtok, ktok[:], ktok, bT, YT3, 6, identb)


TWO_PI = 2.0 * math.pi
HYCFG = (("lat", SEQ, 16, 2 * SEQ), ("ctx", CTX, 2, 2 * CTX))


def hyf_phase(kb, S, l, C):
    P = kb.P
    w1 = kb.sb(S, "hf_w1", [33, 64])
    w2 = kb.sb(S, "hf_w2", [64, 64])
    w3 = kb.sb(S, "hf_w3", [64, 64])
    w4 = kb.sb(S, "hf_w4", [64, 512])
    hyp = kb.sb(S, "hf_hyp", [64, 6])
    ones = kb.sb(S, "hf_ones", [128, 128])
    altc = kb.sb(S, "hf_altc", [128, 1], BF16)
    P.dma("sync", w1[:], C["hy_w1"][l])
    P.dma("sync", w2[:], C["hy_w2"][l])
    P.dma("sync", w3[:], C["hy_w3"][l])
    P.dma("sync", w4[:], C["hy_w4"][l])
    P.dma("sync", hyp[:], C["hyp"][l])
    P.g(lambda e: e.memset(ones[:], 1.0), [], [ones])
    P.dma("gpsimd", altc[:], C["altcol"])
    pp2 = [kb.ps(S, f"hf_pp{i}", [128, 512]) for i in range(2)]
    pk2 = [kb.ps(S, f"hf_pk{i}", [128, 512]) for i in range(2)]
    pl1 = kb.ps(S, "hf_pl1", [128, 512])
    pKr = kb.ps(S, "hf_pKr", [128, 512])
    pn = kb.ps(S, "hf_pn", [128, 512])
    def do_cfg(nm, L, nch, N):
        with contextlib.ExitStack() as S1:
            zT = kb.sb(S1, f"hf_zT{nm}", [33, L])
            hA = kb.sb(S1, f"hf_hA{nm}", [64, L])
            hB = kb.sb(S1, f"hf_hB{nm}", [64, L])
            win = kb.sb(S1, f"hf_win{nm}", [128, nch * 256])
            kfw = kb.sb(S1, f"hf_kfw{nm}", [128, nch * 256])
            kbw = kb.sb(S1, f"hf_kbw{nm}", [128, nch * 256])
            ksum = kb.sb(S1, f"hf_ksum{nm}", [128, nch * 256], BF16)
            kdif = kb.sb(S1, f"hf_kdif{nm}", [128, nch * 256], BF16)
            tmpa = kb.sb(S1, f"hf_tmpa{nm}", [128, 512])
            tmpi = kb.sb(S1, f"hf_tmpi{nm}", [128, 512], mybir.dt.int32)
            tmpf = kb.sb(S1, f"hf_tmpf{nm}", [128, 512])
            rl1 = kb.sb(S1, f"hf_rl1{nm}", [128, 256])
            wN = kb.sb(S1, f"hf_wN{nm}", [128, nch])
            spec = [kb.sb(S1, f"hf_spec{nm}{i}", [128, 512]) for i in range(2)]
            ctb = [kb.sb(S1, f"hf_ctb{nm}{i}", [128, nch * 128], BF16) for i in range(2)]
            stb = [kb.sb(S1, f"hf_stb{nm}{i}", [128, nch * 128], BF16) for i in range(2)]
            nyq = kb.sb(S1, f"hf_nyq{nm}", [1, 256])
            P.dma("sync", zT[:], C[f"zT_{nm}"])
            P.dma("sync", win[:].rearrange("p (i c) -> p i c", i=nch), C[f"win_{nm}"].rearrange("(i p) c -> p i c", p=128))
            P.dma("sync", wN[:], C[f"wN_{nm}"])
            grp = [(g0, min(512, L - g0)) for g0 in range(0, L, 512)]
            srcs = [(w1, 33, zT), (w2, 64, hA), (w3, 64, hB)]
            dsts = [hA, hB, hA]
            for li in range(3):
                w_, K_, src_ = srcs[li]
                dst_ = dsts[li]
                for gi, (g0, n) in enumerate(grp):
                    pp = pp2[gi % 2]
                    P.mm(pp[0:64, 0:n], w_[0:K_, :], src_[0:K_, g0:g0 + n], writes=[pp])
                    P.v(lambda e, pp=pp, n=n, li=li: e.tensor_scalar(tmpa[0:64, 0:n], pp[0:64, 0:n], hyp[:, li:li + 1],
                                                                   hyp[:, 3 + li:4 + li], ALU.add, ALU.mult), [pp, hyp], [tmpa])
                    P.v(lambda e, n=n: e.tensor_scalar(tmpa[0:64, 0:n], tmpa[0:64, 0:n], 1.0 / TWO_PI, 16.0,
                                                       ALU.mult, ALU.add), [tmpa], [tmpa])
                    P.v(lambda e, n=n: e.tensor_copy(tmpi[0:64, 0:n], tmpa[0:64, 0:n]), [tmpa], [tmpi])
                    P.v(lambda e, n=n: e.tensor_copy(tmpf[0:64, 0:n], tmpi[0:64, 0:n]), [tmpi], [tmpf])
                    P.v(lambda e, n=n: e.tensor_tensor(tmpa[0:64, 0:n], tmpa[0:64, 0:n], tmpf[0:64, 0:n], ALU.subtract),
                        [tmpa, tmpf], [tmpa])
                    P.v(lambda e, n=n: e.tensor_scalar(tmpf[0:64, 0:n], tmpa[0:64, 0:n], 0.5, None, ALU.is_gt), [tmpa], [tmpf])
                    P.v(lambda e, n=n: e.tensor_tensor(tmpa[0:64, 0:n], tmpa[0:64, 0:n], tmpf[0:64, 0:n], ALU.subtract),
                        [tmpa, tmpf], [tmpa])
                    P.a(lambda e, n=n, dst_=dst_, g0=g0: e.activation(dst_[:, g0:g0 + n], tmpa[0:64, 0:n], AF.Sin, scale=TWO_PI),
                        [tmpa], [dst_])
            h3 = hA
            for i in range(nch):
                pk = pk2[i % 2]
                P.mm(pk[:, :], h3[:, i * 128:(i + 1) * 128], w4[:, :], writes=[pk])
                P.v(lambda e, pk=pk, i=i: e.tensor_tensor(kfw[:, i * 256:(i + 1) * 256], pk[:, 0:256], win[:, i * 256:(i + 1) * 256],
                                                          ALU.mult), [pk, win], [kfw])
                P.v(lambda e, pk=pk, i=i: e.tensor_tensor(kbw[:, i * 256:(i + 1) * 256], pk[:, 256:512], win[:, i * 256:(i + 1) * 256],
                                                          ALU.mult), [pk, win], [kbw])
            P.g(lambda e: e.memset(kbw[0:1, 0:256], 0.0), [kbw], [kbw])
            for i in range(nch):
                P.a(lambda e, i=i: e.activation(tmpa[:, 0:256], kfw[:, i * 256:(i + 1) * 256], AF.Abs), [kfw], [tmpa])
                P.a(lambda e, i=i: e.activation(tmpa[:, 256:512], kbw[:, i * 256:(i + 1) * 256], AF.Abs), [kbw, tmpa], [tmpa])
                P.g(lambda e: e.tensor_tensor(tmpa[:, 0:256], tmpa[:, 0:256], tmpa[:, 256:512], ALU.add), [tmpa], [tmpa])
                P.mm(pl1[:, 0:256], ones[:, :], tmpa[:, 0:256], start=(i == 0), stop=(i == nch - 1), writes=[pl1])
            P.v(lambda e: e.reciprocal(rl1[:], pl1[:, 0:256]), [pl1], [rl1])
            rbc = rl1[:].rearrange("p (o c) -> p o c", o=1).to_broadcast([128, nch, 256])
            k3 = lambda t: t[:].rearrange("p (i c) -> p i c", i=nch)
            P.v(lambda e: e.tensor_tensor(k3(ksum), k3(kfw), k3(kbw), ALU.add), [kfw, kbw], [ksum])
            P.g(lambda e: e.tensor_tensor(k3(kdif), k3(kbw), k3(kfw), ALU.subtract), [kfw, kbw], [kdif])
            P.v(lambda e: e.tensor_tensor(k3(ksum), k3(ksum), rbc, ALU.mult), [ksum, rl1], [ksum])
            P.g(lambda e: e.tensor_tensor(k3(kdif), k3(kdif), rbc, ALU.mult), [kdif, rl1], [kdif])
            ctab, stab = C[f"ctab_{nm}"], C[f"stab_{nm}"]
            for j in range(nch):
                cb_, sb_ = ctb[j % 2], stb[j % 2]
                P.dma("sync", cb_[:].rearrange("p (i f) -> p i f", i=nch),
                      ctab[:, j * 128:(j + 1) * 128].rearrange("(i p) f -> p i f", p=128), writes=[cb_])
                P.dma("sync", sb_[:].rearrange("p (i f) -> p i f", i=nch),
                      stab[:, j * 128:(j + 1) * 128].rearrange("(i p) f -> p i f", p=128), writes=[sb_])
                for i in range(nch):
                    P.mm(pKr[:, 0:256], cb_[:, i * 128:(i + 1) * 128], ksum[:, i * 256:(i + 1) * 256],
                         start=(i == 0), stop=(i == nch - 1), writes=[pKr])
                for i in range(nch):
                    P.mm(pKr[:, 256:512], sb_[:, i * 128:(i + 1) * 128], kdif[:, i * 256:(i + 1) * 256],
                         start=(i == 0), stop=(i == nch - 1), skip_group_check=True, writes=[pKr])
                sp = spec[j % 2]
                P.v(lambda e, sp=sp, j=j: e.tensor_scalar(sp[:], pKr[:], wN[:, j:j + 1], None, ALU.mult), [pKr, wN], [sp])
                P.dma("sync", kb.KSPEC[nm][j], sp[:], writes=[("KSPEC", nm)])
            for i in range(nch):
                P.mm(pn[0:1, 0:256], altc[:, 0:1], ksum[:, i * 256:(i + 1) * 256], start=(i == 0), stop=(i == nch - 1),
                     writes=[pn])
            P.v(lambda e, N=N: e.tensor_scalar(nyq[:], pn[0:1, 0:256], 1.0 / N, None, ALU.mult), [pn], [nyq])
            P.dma("sync", kb.KNYQ[nm], nyq[:], writes=[("KSPEC", nm)])
        P.barrier()

    for cfg in HYCFG:
        do_cfg(*cfg)


def hyena_mixer(kb, S2, l, b, xmT3, YT23, groups, w_in, C, small, identb, conv3, yt_flush):
    P = kb.P
    pbuf = kb.sb(S2, "hy_pbuf", [128, LS])
    x0c = kb.sb(S2, "hy_x0c", [128, LS])
    x1c = kb.sb(S2, "hy_x1c", [128, LS])
    zc = kb.sb(S2, "hy_zc", [128, LS])
    zb = kb.sb(S2, "hy_zb", [128, LS], BF16)
    Zt = kb.sb(S2, "hy_Zt", [128, NT * 128], BF16)
    Yc = kb.sb(S2, "hy_Yc", [128, 16 * 128], BF16)
    Ys = kb.sb(S2, "hy_Ys", [128, 16 * 128], BF16)
    Yn = kb.sb(S2, "hy_Yn", [1, 128], BF16)
    knq = kb.sb(S2, "hy_knq", [1, 256])
    t1 = kb.sb(S2, "hy_t1", [128, 128])
    t2 = kb.sb(S2, "hy_t2", [128, 128])
    t3 = kb.sb(S2, "hy_t3", [128, 512])
    altc = kb.sb(S2, "hy_altc", [128, 1], BF16)
    altr = kb.sb(S2, "hy_altr", [1, SEQ], BF16)
    spec = [kb.sb(S2, f"hy_spec{i}", [128, 512]) for i in range(2)]
    tbA = [kb.sb(S2, f"hy_tbA{i}", [128, 2048], BF16) for i in range(2)]
    tbB = [kb.sb(S2, f"hy_tbB{i}", [128, 2048], BF16) for i in range(2)]
    wsl = kb.sb(S2, "hy_wsl", [128, 8 * 768], BF16)
    P.dma("gpsimd", wsl[:].rearrange("p (k c) -> p k c", k=8),
          w_in[l, :, HY0:HY0 + 768].rearrange("(k p) c -> p k c", p=128), writes=[wsl])
    P.dma("gpsimd", altc[:], C["altcol"])
    P.dma("gpsimd", altr[:], C["altrow"])
    pp2 = [kb.ps(S2, f"hy_pp{i}", [128, 512]) for i in range(2)]
    pT = kb.ps(S2, "hy_pT", [128, 1024], BF16)
    pF = kb.ps(S2, "hy_pF", [128, 512])
    pI = [kb.ps(S2, f"hy_pI{i}", [128, 512]) for i in range(4)]
    ii = 0
    for hf in range(2):
        dsts = {0: x0c, 2: x1c, 4: zc}
        for cb in (0, 2, 4):
            ch = cb + hf
            for gi, (t0, n, j) in enumerate(groups):
                pp = pp2[ii % 2]
                ii += 1
                for k in range(8):
                    P.mm(pp[:, 0:n], wsl[:, k * 768 + ch * 128: k * 768 + (ch + 1) * 128], xmT3[:, k, t0:t0 + n],
                         start=(k == 0), stop=(k == 7), reads=[wsl, ("xmT", gi)], writes=[pp])
                P.a(lambda e, pp=pp, t0=t0, n=n: e.copy(pbuf[:, t0:t0 + n], pp[:, 0:n]), [pp], [pbuf])
            wc = tuple(small[:, 3 * ch + k: 3 * ch + k + 1] for k in range(3))
            conv3(dsts[cb], pbuf, wc, bias=small[:, 18 + ch: 19 + ch])
        P.v(lambda e: e.tensor_tensor(zc[:], zc[:], x1c[:], ALU.mult), [zc, x1c], [zc])
        P.g(lambda e: e.tensor_copy(zb[:], zc[:]), [zc], [zb])
        for c4 in range(0, NT, 8):
            nn = min(8, NT - c4)
            for cc in range(nn):
                P.tr(pT[:, cc * 128:(cc + 1) * 128], zb[:, (c4 + cc) * 128:(c4 + cc + 1) * 128], identb[:], writes=[pT])
            P.v(lambda e, c4=c4, nn=nn: e.tensor_copy(Zt[:, c4 * 128:(c4 + nn) * 128], pT[:, 0:nn * 128]), [pT], [Zt])
        for (nm, L, nch, N) in HYCFG:
            tile0 = 2 if nm == "lat" else 0
            tok0 = CTX if nm == "lat" else 0
            ctab, stab = C[f"ctab_{nm}"], C[f"stab_{nm}"]
            P.dma("sync", knq[:], kb.KNYQ[nm], reads=[("KSPEC", nm)])
            for j in range(nch):
                ca, sa, sp = tbA[j % 2], tbB[j % 2], spec[j % 2]
                P.dma("sync", ca[:, 0:nch * 128].rearrange("p (i f) -> p i f", i=nch),
                      ctab[:, j * 128:(j + 1) * 128].rearrange("(i p) f -> p i f", p=128), writes=[ca])
                P.dma("sync", sa[:, 0:nch * 128].rearrange("p (i f) -> p i f", i=nch),
                      stab[:, j * 128:(j + 1) * 128].rearrange("(i p) f -> p i f", p=128), writes=[sa])
                P.dma("sync", sp[:], kb.KSPEC[nm][j], reads=[("KSPEC", nm)], writes=[sp])
                for i in range(nch):
                    P.mm(pF[:, 0:128], ca[:, i * 128:(i + 1) * 128], Zt[:, (tile0 + i) * 128:(tile0 + i + 1) * 128],
                         start=(i == 0), stop=(i == nch - 1), writes=[pF])
                for i in range(nch):
                    P.mm(pF[:, 128:256], sa[:, i * 128:(i + 1) * 128], Zt[:, (tile0 + i) * 128:(tile0 + i + 1) * 128],
                         start=(i == 0), stop=(i == nch - 1), skip_group_check=True, writes=[pF])
                kr = sp[:, hf * 128:(hf + 1) * 128]
                ki = sp[:, 256 + hf * 128: 256 + (hf + 1) * 128]
                ycj = Yc[:, j * 128:(j + 1) * 128]
                ysj = Ys[:, j * 128:(j + 1) * 128]
                P.v(lambda e, kr=kr: e.tensor_tensor(t1[:], pF[:, 0:128], kr, ALU.mult), [pF, sp], [t1])
                P.v(lambda e, ki=ki: e.tensor_tensor(t2[:], pF[:, 128:256], ki, ALU.mult), [pF, sp], [t2])
                P.g(lambda e, ycj=ycj: e.tensor_tensor(ycj, t1[:], t2[:], ALU.add), [t1, t2], [Yc])
                P.v(lambda e, kr=kr: e.tensor_tensor(t1[:], pF[:, 128:256], kr, ALU.mult), [pF, sp, Yc], [t1])
                P.v(lambda e, ki=ki: e.tensor_tensor(t2[:], pF[:, 0:128], ki, ALU.mult), [pF, sp, Yc], [t2])
                P.g(lambda e, ysj=ysj: e.tensor_tensor(ysj, t1[:], t2[:], ALU.subtract), [t1, t2], [Ys])
            for i in range(nch):
                P.mm(pF[0:1, 256:384], altc[:, 0:1], Zt[:, (tile0 + i) * 128:(tile0 + i + 1) * 128],
                     start=(i == 0), stop=(i == nch - 1), skip_group_check=True, writes=[pF])
            P.v(lambda e, hf=hf: e.tensor_tensor(Yn[:], pF[0:1, 256:384], knq[:, hf * 128:(hf + 1) * 128], ALU.mult), [pF, knq], [Yn])
            tgs = [(g0, min(512, L - g0)) for g0 in range(0, L, 512)]
            for j in range(nch):
                ca, sa = tbA[j % 2], tbB[j % 2]
                P.dma("sync", ca[:, 0:L], ctab[j * 128:(j + 1) * 128, :], writes=[ca])
                P.dma("sync", sa[:, 0:L], stab[j * 128:(j + 1) * 128, :], writes=[sa])
                for gi, (g0, n) in enumerate(tgs):
                    P.mm(pI[gi][:, 0:n], Yc[:, j * 128:(j + 1) * 128], ca[:, g0:g0 + n], start=(j == 0), stop=False,
                         writes=[pI[gi]])
                    P.mm(pI[gi][:, 0:n], Ys[:, j * 128:(j + 1) * 128], sa[:, g0:g0 + n], start=False, stop=False,
                         writes=[pI[gi]])
            for gi, (g0, n) in enumerate(tgs):
                P.mm(pI[gi][:, 0:n], Yn[:, :], altr[:, g0:g0 + n], start=False, stop=True, writes=[pI[gi]])
                a0 = tok0 + g0
                P.v(lambda e, gi=gi, n=n, a0=a0, hf=hf: e.scalar_tensor_tensor(t3[:, 0:n], zc[:, a0:a0 + n], small[:, 24 + hf:25 + hf],
                                                                       pI[gi][:, 0:n], ALU.mult, ALU.add),
                    [zc, small, pI[gi]], [t3])
                P.v(lambda e, n=n, a0=a0, hf=hf: e.tensor_tensor(YT23[:, hf, a0:a0 + n], t3[:, 0:n], x0c[:, a0:a0 + n], ALU.mult),
                    [t3, x0c], [("YT2", hf)])
    yt_flush(0)

TB = 1152
NTB = TB // 128


def moe_phase(kb, S, l, last, XR, out, lnp, w_router, b_router, w_gate, w_up, w_down, bguT, b_down,
              ident, sel, ADA, acol, a1col):
    nc, P = kb.nc, kb.P
    hT = kb.sb(S, "hT", [128, 8 * TB], BF16)
    hT3 = hT[:].rearrange("p (k t) -> p k t", k=8)
    acc = kb.sb(S, "acc", [128, NTB * D])
    cw = kb.sb(S, "cw", [128, NTB * NE])
    cwT = kb.sb(S, "cwT", [NE, TB])
    wr = kb.sb(S, "wr", [128, 8 * NE])
    brb = kb.sb(S, "brb", [128, NE])
    bdn = kb.sb(S, "bdn", [NE, D])
    bgu = kb.sb(S, "bgu", [128, 2 * NE * 8])
    gbc = kb.sb(S, "ln2g", [128, D])
    bbc = kb.sb(S, "ln2b", [128, D])
    g2 = {j: kb.sb(S, f"g2bc{j}", [128, D]) for j in range(2)}
    g2[2] = g2[1]
    wts = [[kb.sb(S, f"w{nm}{i}", [128, 8 * D], BF16) for nm in ("g", "u", "d")] for i in range(2)]
    actT = [kb.sb(S, f"actT{i}", [128, 8 * 512], BF16) for i in range(1)]
    hT32 = [kb.sb(S, f"hT32_{i}", [128, 8 * 128]) for i in range(1)]
    xts = [kb.sb(S, f"mx{i}", [128, D]) for i in range(1)]
    tg_ = [kb.sb(S, f"m_g{i}", [128, 512]) for i in range(1)] * 2
    ts_ = [kb.sb(S, f"m_s{i}", [128, 512]) for i in range(1)] * 2
    tu_ = [kb.sb(S, f"m_u{i}", [128, 512]) for i in range(1)] * 2
    lg = kb.sb(S, "m_lg", [128, NE])
    m8 = kb.sb(S, "m_m8", [128, 8])
    nmx = kb.sb(S, "m_nmx", [128, 1])
    msk = kb.sb(S, "m_msk", [128, NE])
    ssum = kb.sb(S, "m_ssum", [128, 1])
    stats = kb.sb(S, "m_stats", [128, 12])
    mv = kb.sb(S, "m_mv", [128, 2])
    rstd = kb.sb(S, "m_rstd", [128, 1])
    pg = [kb.ps(S, f"m_pg{i}", [128, 512]) for i in range(2)]
    pu = [kb.ps(S, f"m_pu{i}", [128, 512]) for i in range(2)]
    pd = [kb.ps(S, f"m_pd{i}", [128, 512]) for i in range(2)]
    pm = [kb.ps(S, f"m_pm{i}", [128, 512]) for i in range(2)]

    P.dma("sync", wr[:].rearrange("p (k e) -> p k e", k=8), w_router[l].rearrange("(k p) e -> p k e", p=128))
    P.dma("sync", brb[:], b_router[l:l + 1, :].to_broadcast([128, NE]))
    P.dma("sync", bdn[:], b_down[l])
    P.dma("sync", bgu[:], bguT[l].rearrange("p a e k -> p (a e k)"))
    P.dma("sync", gbc[:], lnp[l, 2:3, :].to_broadcast([128, D]))
    P.dma("sync", bbc[:], lnp[l, 3:4, :].to_broadcast([128, D]))

    nblk = TOK // TB
    wcount = [0]

    def load_w(e, buf):
        for nm, wsrc in zip(range(3), (w_gate, w_up, w_down)):
            wt = wts[buf][nm]
            for hh in range(2):
                P.dma("gpsimd", wt[:, hh * 4 * D:(hh + 1) * 4 * D].rearrange("p (k c) -> p k c", k=4),
                      wsrc[l, e, hh * 512:(hh + 1) * 512, :].rearrange("(k p) c -> p k c", p=128),
                      writes=[wt])

    for blk in range(nblk):
        bt0 = blk * TB
        bidx = bt0 // LS
        gate_bcast(kb, g2[0], l, bidx, 5 * 1024, ADA)
        if blk % 2 == 0:
            gate_bcast(kb, g2[1], l, 2, 5 * 1024, ADA)
        if kb.moe:
            load_w(0, 0)
        for ti in range(NTB):
            r0 = bt0 + ti * 128
            sidx = r0 - bidx * LS
            j = 2 if sidx < CTX else bidx
            xt = xts[0]
            h32 = hT32[0]
            P.dma("sync", xt[:], XR[r0:r0 + 128, :], reads=[("XR", r0 // 128)])
            for half in range(2):
                pp = pm[half]
                for kk in range(4):
                    k = half * 4 + kk
                    P.tr(pp[:, kk * 128:(kk + 1) * 128], xt[:, k * 128:(k + 1) * 128], ident[:], writes=[pp])
                for kk in range(4):
                    k = half * 4 + kk
                    P.v(lambda e, pp=pp, kk=kk, k=k, j=j, h32=h32: e.tensor_scalar(
                        h32[:, k * 128:(k + 1) * 128], pp[:, kk * 128:(kk + 1) * 128],
                        a1col(j, 32 + k), acol(j, 24 + k), ALU.mult, ALU.add),
                        [pp, "adaT", "ada1T"], [h32])
            P.g(lambda e, h32=h32, ti=ti: e.tensor_copy(
                hT3[:, :, ti * 128:(ti + 1) * 128], h32[:].rearrange("p (k t) -> p k t", k=8)),
                [h32], [("hT", ti)])
            if kb.dbg.get("stop") == "m1":
                continue
            pl = pd[ti % 2]
            for k in range(8):
                P.mm(pl[:, 0:NE], h32[:, k * 128:(k + 1) * 128], wr[:, k * NE:(k + 1) * NE],
                     start=(k == 0), stop=(k == 7), writes=[pl])
            P.v(lambda e, pl=pl: e.tensor_tensor(lg[:], pl[:, 0:NE], brb[:], ALU.add), [pl, brb], [lg])
            if kb.dbg.get("stop") == "m2":
                continue
            P.v(lambda e: e.max(out=m8[:], in_=lg[:]), [lg], [m8])
            P.v(lambda e: e.tensor_scalar(msk[:], lg[:], m8[:, 3:4], None, ALU.is_ge), [lg, m8], [msk])
            P.v(lambda e: e.tensor_scalar(nmx[:], m8[:, 0:1], -1.0, None, ALU.mult), [m8], [nmx])
            P.a(lambda e: e.activation(lg[:], lg[:], AF.Exp, bias=nmx[:, 0:1]), [lg, nmx], [lg])
            P.v(lambda e: e.tensor_tensor(lg[:], lg[:], msk[:], ALU.mult), [lg, msk], [lg])
            P.v(lambda e: e.reduce_sum(ssum[:], lg[:], AX.X), [lg], [ssum])
            P.v(lambda e: e.reciprocal(ssum[:], ssum[:]), [ssum], [ssum])
            cwt = cw[:, ti * NE:(ti + 1) * NE]
            P.v(lambda e, cwt=cwt: e.tensor_scalar(cwt, lg[:], ssum[:, 0:1], None, ALU.mult),
                [lg, ssum], [("cw", ti)])
            if kb.dbg.get("stop") == "m3":
                continue
            pt = pu[ti % 2]
            P.tr(pt[0:NE, 0:128], cwt, ident[:], reads=[("cw", ti), ident], writes=[pt])
            P.v(lambda e, pt=pt, ti=ti: e.tensor_copy(cwT[:, ti * 128:(ti + 1) * 128], pt[0:NE, 0:128]),
                [pt], [("cwT", ti)])
            for h in range(2):
                pp = pg[h]
                P.mm(pp[:, :], cwT[:, ti * 128:(ti + 1) * 128], bdn[:, h * 512:(h + 1) * 512],
                     reads=[("cwT", ti), bdn], writes=[pp])
                P.a(lambda e, pp=pp, ti=ti, h=h: e.copy(acc[:, ti * D + h * 512: ti * D + (h + 1) * 512], pp[:, :]),
                    [pp], [("acc", ti)])
        if kb.dbg.get("stop") in ("m1", "m2", "m3", "m4"):
            continue
        if "cw" in kb.dbg and blk == 0:
            dd = kb.dout(f"dbg_cw{l}", [128, NTB * NE])
            P.dma("sync", dd, cw[:], reads=[("cw", t) for t in range(NTB)])
        hkeys = [("hT", t) for t in range(NTB)]
        if kb.moe:
            for e_ in range(NE):
                buf = e_ % 2
                if e_ + 1 < NE:
                    load_w(e_ + 1, 1 - buf)
                wg, wu, wd = wts[buf]
                ci = 0
                for (g0, gn) in ((0, 512), (512, 512), (1024, 128)):
                    at = actT[0]
                    ci += 1
                    for fc in range(8):
                        p_g, p_u = pg[fc % 2], pu[fc % 2]
                        for k in range(8):
                            P.mm(p_g[:, 0:gn], wg[:, k * D + fc * 128: k * D + (fc + 1) * 128],
                                 hT3[:, k, g0:g0 + gn], start=(k == 0), stop=(k == 7),
                                 reads=[wg] + hkeys, writes=[p_g])
                        for k in range(8):
                            P.mm(p_u[:, 0:gn], wu[:, k * D + fc * 128: k * D + (fc + 1) * 128],
                                 hT3[:, k, g0:g0 + gn], start=(k == 0), stop=(k == 7),
                                 reads=[wu] + hkeys, writes=[p_u])
                        tg, tsg, tu = tg_[fc % 2], ts_[fc % 2], tu_[fc % 2]
                        bgc = bgu[:, (0 * NE + e_) * 8 + fc:(0 * NE + e_) * 8 + fc + 1]
                        buc = bgu[:, (1 * NE + e_) * 8 + fc:(1 * NE + e_) * 8 + fc + 1]
                        P.v(lambda e, tg=tg, p_g=p_g, gn=gn, bgc=bgc: e.tensor_scalar(
                            tg[:, 0:gn], p_g[:, 0:gn], bgc, 7.0, ALU.add, ALU.min), [p_g, bgu], [tg])
                        P.a(lambda e, tg=tg, tsg=tsg, gn=gn: e.activation(
                            tsg[:, 0:gn], tg[:, 0:gn], AF.Sigmoid, scale=1.702), [tg], [tsg])
                        P.v(lambda e, tu=tu, p_u=p_u, gn=gn, buc=buc: e.tensor_scalar(
                            tu[:, 0:gn], p_u[:, 0:gn], buc, 7.0, ALU.add, ALU.min), [p_u, bgu], [tu])
                        P.g(lambda e, tu=tu, gn=gn: e.tensor_scalar(
                            tu[:, 0:gn], tu[:, 0:gn], -7.0, 1.0, ALU.max, ALU.add), [tu], [tu])
                        P.g(lambda e, tg=tg, tsg=tsg, gn=gn: e.tensor_tensor(
                            tg[:, 0:gn], tg[:, 0:gn], tsg[:, 0:gn], ALU.mult), [tg, tsg], [tg])
                        P.g(lambda e, tg=tg, tu=tu, gn=gn, at=at, fc=fc: e.tensor_tensor(
                            at[:, fc * 512: fc * 512 + gn], tg[:, 0:gn], tu[:, 0:gn], ALU.mult),
                            [tg, tu], [at])
                    for tt in range(gn // 128):
                        ti = (g0 + tt * 128) // 128
                        for h in range(2):
                            pp = pd[h]
                            for fc in range(8):
                                P.mm(pp[:, :], at[:, fc * 512 + tt * 128: fc * 512 + (tt + 1) * 128],
                                     wd[:, fc * D + h * 512: fc * D + (h + 1) * 512],
                                     start=(fc == 0), stop=(fc == 7), reads=[at, wd], writes=[pp])
                            asl = acc[:, ti * D + h * 512: ti * D + (h + 1) * 512]
                            P.v(lambda e, pp=pp, asl=asl, ti=ti, e_=e_: e.scalar_tensor_tensor(
                                asl, pp[:, :], cw[:, ti * NE + e_: ti * NE + e_ + 1], asl, ALU.mult, ALU.add),
                                [pp, ("cw", ti), ("acc", ti)], [("acc", ti)])
        for ti in range(NTB):
            r0 = bt0 + ti * 128
            sidx = r0 - bidx * LS
            j = 2 if sidx < CTX else bidx
            xt = xts[0]
            at = acc[:, ti * D:(ti + 1) * D]
            P.dma("sync", xt[:], XR[r0:r0 + 128, :], reads=[("XR", r0 // 128)])
            P.g(lambda e, at=at, j=j: e.tensor_tensor(at, at, g2[0 if j < 2 else 1][:], ALU.mult), [("acc", ti), g2[0 if j < 2 else 1]], [("acc", ti)])
            P.v(lambda e, at=at, xt=xt: e.scalar_tensor_tensor(at, xt[:], float(DN_ALPHA), at, ALU.mult, ALU.add),
                [xt, ("acc", ti)], [("acc", ti)])

            class _T:
                def __init__(s, ap): s.ap = ap
                def __getitem__(s, idx): return s.ap[idx]
            ln_tile(kb, None, _T(at), gbc[:], bbc[:], _T(at), stats, mv, rstd, ("acc", ti), ("acc", ti))
            if kb.dbg.get("stop") == "m5":
                continue
            if last:
                if sidx >= CTX:
                    o0 = bidx * SEQ + sidx - CTX
                    P.dma("sync", out[o0:o0 + 128, :], at, reads=[("acc", ti)], writes=["out"])
            else:
                P.dma("sync", XR[r0:r0 + 128, :], at, reads=[("acc", ti)], writes=[("XR", r0 // 128)])


def _hy_consts(nm, L_, nch, N_):
    o = {}
    t = np.linspace(0.0, 1.0, L_, dtype=np.float32)[:, None]
    ang = (2.0 * math.pi * np.arange(L_, dtype=np.float32)[:, None] / L_).astype(np.float32)
    fb = np.linspace(1e-4, 15, 16, dtype=np.float32)[None, :]
    z = np.concatenate([t, np.cos(fb * ang), -np.sin(fb * ang)], axis=-1).astype(np.float32)
    o[f"zT_{nm}"] = np.ascontiguousarray(z.T)
    max_decay = math.log(1e-2) / 0.3
    min_decay = math.log(1e-2) / 1.5
    deltas = np.abs(np.linspace(min_decay, max_decay, 256, dtype=np.float32))
    o[f"win_{nm}"] = np.exp(-t * deltas[None, :]).astype(np.float32)
    tt = np.arange(L_, dtype=np.int64)
    ph = (np.outer(tt, tt) % N_).astype(np.float64) * (2.0 * math.pi / N_)
    o[f"ctab_{nm}"] = np.cos(ph).astype(ml_dtypes.bfloat16)
    o[f"stab_{nm}"] = np.sin(ph).astype(ml_dtypes.bfloat16)
    f_ = np.arange(L_).reshape(nch, 128).T
    o[f"wN_{nm}"] = np.where(f_ == 0, 1.0 / N_, 2.0 / N_).astype(np.float32)
    return o


def _prep_shared(inp):
    f = lambda a: np.ascontiguousarray(np.asarray(a, dtype=np.float32))
    sh = {}
    for k in ("w_ada", "b_ada", "w_in", "w_out", "w_router", "b_router", "w_gate", "w_up", "w_down", "b_down"):
        sh[k] = f(inp[k])
    sh["lnp"] = f(np.stack([inp["ln1_g"], inp["ln1_b"], inp["ln2_g"], inp["ln2_b"]], axis=1))
    bg = np.asarray(inp["b_gate"], np.float32).reshape(DEPTH, NE, 8, 128)
    bu = np.asarray(inp["b_up"], np.float32).reshape(DEPTH, NE, 8, 128)
    sh["bguT"] = f(np.stack([bg, bu], axis=1).transpose(0, 4, 1, 2, 3))
    sm = np.zeros((DEPTH, 128, 64), np.float32)

    def colT(a, nch):
        n = a.shape[1]
        return a.reshape(DEPTH, n, nch, 128).transpose(0, 3, 2, 1).reshape(DEPTH, 128, nch * n)
    sm[:, :, 0:18] = colT(np.asarray(inp["hy_conv_w"], np.float32), 6)
    sm[:, :, 18:24] = colT(np.asarray(inp["hy_conv_b"], np.float32)[:, None, :], 6)
    sm[:, :, 24:26] = colT(np.asarray(inp["hy_d"], np.float32)[:, None, :], 2)
    sm[:, :, 26:32] = colT(np.asarray(inp["sc_conv_w"], np.float32), 2)
    sm[:, :, 32:50] = colT(np.asarray(inp["gdn_conv_w"], np.float32), 6)
    sm[:, :, 50:52] = np.asarray(inp["gla_b_a"], np.float32).transpose(0, 2, 1)
    sh["smallT"] = sm
    sh["gla_w_a"] = f(inp["gla_w_a"])
    sh["gla_norm_w"] = f(inp["gla_norm_w"])
    jj = np.arange(128)[:, None]
    ii = np.arange(128)[None, :]
    tm = np.stack([np.tile((jj <= ii), (1, 4)), np.tile((jj >= ii), (1, 4))]).astype(np.float32)
    sh["trimask"] = tm
    sh["gdn_norm_w"] = f(inp["gdn_norm_w"])
    sm[:, 0:8, 52] = np.asarray(inp["gdn_dt_bias"], np.float32).reshape(DEPTH, 8)
    sm[:, 0:8, 53] = np.asarray(inp["gdn_a_log"], np.float32).reshape(DEPTH, 8)
    sh["bones"] = (np.arange(128)[:, None] // 64 == np.arange(128)[None, :] // 64).astype(np.float32)
    gs = np.zeros((16, 2), np.float32); gs[0:4, 0] = 1; gs[4:8, 1] = 1
    sh["gsel"] = gs
    sq_ = np.zeros((16, 4, 128), np.float32)
    for d_ in range(2):
        for hc in range(2):
            for m_ in range(128):
                sq_[4 * d_ + 2 * hc + m_ // 64, d_ * 2 + hc, m_] = 1
    sh["selq"] = sq_.reshape(16, 512)
    sr = np.zeros((16, 8, 128), np.float32)
    for r_ in range(8):
        sr[r_, r_, :] = 1
    sh["selr"] = sr.reshape(16, 1024)
    pp_ = np.arange(128)[:, None]; ff_ = np.arange(128)[None, :]
    BIG = 1.0e4
    sh["gmask"] = np.stack([np.where(ff_ < pp_, 0, -BIG), np.where(ff_ > pp_, 0, -BIG),
                            np.where(pp_ <= ff_, 0, BIG), np.where(pp_ >= ff_, 0, BIG)]).astype(np.float32)
    sms = [(pp_ // 8 == ff_ // 8).astype(np.float32)]
    for s_ in (8, 16, 32, 64):
        sms.append(-((pp_ // (2 * s_) == ff_ // (2 * s_)) & (pp_ // s_ != ff_ // s_)).astype(np.float32))
    sh["smask"] = np.stack(sms)
    for k_ in ("hy_w1", "hy_w2", "hy_w3", "hy_w4"):
        sh[k_] = f(inp[k_])
    sh["hyp"] = f(np.stack([inp["hy_b1"], inp["hy_b2"], inp["hy_b3"], inp["hy_freq"][:, 0], inp["hy_freq"][:, 1],
                            inp["hy_freq"][:, 2]], axis=-1))
    sh["altcol"] = ((-1.0) ** np.arange(128)).astype(np.float32)[:, None]
    sh["altrow"] = ((-1.0) ** np.arange(SEQ)).astype(np.float32)[None, :]
    for (nm, L_, nch, N_) in HYCFG:
        sh.update(_hy_consts(nm, L_, nch, N_))
    sh["hsel"] = (np.arange(128)[:, None] // 32 == np.arange(4)[None, :]).astype(np.float32)
    sh["bdmask"] = (np.arange(128)[:, None] // 32 == np.arange(256)[None, :] // 64).astype(np.float32)
    sh["ident"] = np.eye(128, dtype=np.float32)
    s3 = np.zeros((3, 3, 128), np.float32)
    for j in range(3):
        s3[j, j, :] = 1.0
    sh["sel3"] = s3
    return sh


def _prep_core(inp, i):
    b0 = i * NB
    xs = []
    for b in range(b0, b0 + NB):
        xs.append(np.asarray(inp["ctx"][b], np.float32))
        xs.append(np.asarray(inp["x"][b], np.float32))
    xin = np.ascontiguousarray(np.concatenate(xs, axis=0))
    c3 = np.stack([np.asarray(inp["c"][b0], np.float32), np.asarray(inp["c"][b0 + 1], np.float32),
                   np.asarray(inp["c_ctx"], np.float32)], axis=0)
    c3T = np.ascontiguousarray(c3.reshape(3, 8, 128).transpose(2, 1, 0))
    return {"xin": xin, "c3T": c3T}


def kernel(**inputs):
    n = 8
    kb = build()
    sh = _prep_shared(inputs)
    in_maps = []
    for i in range(n):
        m = dict(sh)
        m.update(_prep_core(inputs, i))
        in_maps.append(m)
    res = run_bass_kernel_spmd(kb.nc, in_maps, core_ids=list(range(n)))
    outs = [np.asarray(r["out"], dtype=np.float32).reshape(NB, SEQ, D) for r in res.results]
    return np.concatenate(outs, axis=0)
```

```python
import contextlib
import math
import numpy as np
import ml_dtypes
import concourse.bass as bass
import concourse.mybir as mybir
from concourse.bass_utils import run_bass_kernel_spmd

F32 = mybir.dt.float32
BF16 = mybir.dt.bfloat16
AF = mybir.ActivationFunctionType
ALU = mybir.AluOpType
AX = mybir.AxisListType

D = 1024
DEPTH = 4
NB = 2
CTX = 256
SEQ = 2048
LS = CTX + SEQ
NT = LS // 128
TOK = NB * LS
D_IN = 3376
NE = 32
LN_EPS = 1e-5
DN_ALPHA = (2 * DEPTH) ** 0.25
HY0, SC0, GLA0, GDN0 = 0, 768, 1536, 2336

EPOCH = 12000
NSLOT = 12
COMPUTE = ("tensor", "vector", "scalar", "gpsimd")
QUEUES = ("sync", "gpsimd")
ALLENG = COMPUTE + ("sync",)


def _keys(aps):
    out = []
    for a in aps:
        if a is None:
            continue
        if isinstance(a, (str, tuple)):
            out.append(a)
        else:
            out.append(getattr(a, "tensor", a).name)
    return out


class Prog:
    def __init__(self, nc):
        self.nc = nc
        self.ops = []
        self.psum = set()

    def op(self, eng, fn, reads=(), writes=(), dma=False, barrier=False):
        r, w = _keys(reads), _keys(writes)
        w = w + [k for k in r if k in self.psum and k not in w]
        self.ops.append(dict(eng=eng, fn=fn, reads=r, writes=w, dma=dma, barrier=barrier))

    def barrier(self):
        self.ops.append(dict(eng=None, barrier=True))

    def dma(self, q, out, in_, reads=None, writes=None, **kw):
        r = [in_] if reads is None else reads
        w = [out] if writes is None else writes
        self.op(q, lambda e: e.dma_start(out=out, in_=in_, **kw), r, w, dma=True)

    def mm(self, out, lhsT, rhs, start=True, stop=True, reads=None, writes=None, **kw):
        r = [lhsT, rhs] if reads is None else reads
        w = [out] if writes is None else writes
        self.op("tensor", lambda e: e.matmul(out, lhsT, rhs, start=start, stop=stop, **kw), r, w)

    def tr(self, out, in_, ident, reads=None, writes=None):
        r = [in_, ident] if reads is None else reads
        w = [out] if writes is None else writes
        self.op("tensor", lambda e: e.transpose(out, in_, ident), r, w)

    def v(self, fn, reads, writes):
        self.op("vector", fn, reads, writes)

    def a(self, fn, reads, writes):
        self.op("scalar", fn, reads, writes)

    def g(self, fn, reads, writes):
        self.op("gpsimd", fn, reads, writes)

    def emit(self):
        nc = self.nc
        ops = [o for o in self.ops]
        seq = {e: 0 for e in ALLENG}
        dma_cnt = {q: 0 for q in QUEUES}
        last_w, readers = {}, {}
        last_tok = {}
        dma_since = []
        force = {e: set() for e in ALLENG}
        seen = {e: {} for e in ALLENG}
        waited = set()
        real = []
        for o in ops:
            if o.get("barrier") and o["eng"] is None:
                B = set(last_tok.values()) | set(dma_since)
                dma_since = []
                for e in ALLENG:
                    force[e] |= B
                continue
            e = o["eng"]
            deps = set(force[e])
            force[e] = set()
            for k in o["reads"]:
                if k in last_w:
                    deps.add(last_w[k])
            for k in o["writes"]:
                if k in last_w:
                    deps.add(last_w[k])
                deps.update(readers.get(k, ()))
            if o["dma"]:
                di = dma_cnt[e]
                dma_cnt[e] += 1
                tok = ("d", e, di)
                if di >= NSLOT:
                    deps.add(("d", e, di - NSLOT))
                seq[e] += 1
                dma_since.append(tok)
            else:
                tok = ("c", e, seq[e])
                seq[e] += 1
                last_tok[e] = tok
            need, best = [], {}
            for t in deps:
                if t == tok:
                    continue
                if t[0] == "c":
                    src = t[1]
                    if src == e and e == "tensor":
                        continue
                    if seen[e].get(src, -1) >= t[2]:
                        continue
                    if best.get(src, -1) < t[2]:
                        best[src] = t[2]
                else:
                    if t in seen[e]:
                        continue
                    need.append(t)
                    seen[e][t] = True
            for src, idx in best.items():
                need.append(("c", src, idx))
                seen[e][src] = idx
            waited.update(need)
            o["deps"], o["tok"] = need, tok
            for k in o["reads"]:
                readers.setdefault(k, []).append(tok)
            for k in o["writes"]:
                last_w[k] = tok
                readers[k] = []
            real.append(o)
        incno, cnt = {}, {e: 0 for e in COMPUTE}
        for o in real:
            t = o["tok"]
            if t[0] == "c" and t in waited:
                incno[t] = cnt[t[1]]
                cnt[t[1]] += 1
        stack = contextlib.ExitStack()
        sems = {e: [stack.enter_context(nc.semaphore(f"s_{e}_{j}"))
                    for j in range(max((cnt[e] + EPOCH - 1) // EPOCH, 1))] for e in COMPUTE}
        dsems = {q: [stack.enter_context(nc.semaphore(f"d_{q}_{j}")) for j in range(NSLOT)]
                 for q in QUEUES}

        def wait_args(t):
            if t[0] == "c":
                k = incno[t]
                return sems[t[1]][k // EPOCH], (k % EPOCH) + 1
            return dsems[t[1]][t[2] % NSLOT], 16 * (t[2] // NSLOT + 1)

        per_eng = {e: [] for e in ALLENG}
        for o in real:
            per_eng[o["eng"]].append(o)

        def run_engine(ename, eng):
            for o in per_eng[ename]:
                for t in o["deps"]:
                    s, val = wait_args(t)
                    eng.wait_ge(s, val)
                ins = o["fn"](eng)
                t = o["tok"]
                if t[0] == "d":
                    ins.then_inc(wait_args(t)[0], 16)
                elif t in incno:
                    ins.then_inc(sems[t[1]][incno[t] // EPOCH], 1)
            if ename in QUEUES:
                for di in range(max(0, dma_cnt[ename] - NSLOT), dma_cnt[ename]):
                    s, val = wait_args(("d", ename, di))
                    eng.wait_ge(s, val)

        with nc.Block() as block:
            block.tensor(lambda eng: run_engine("tensor", eng))
            block.vector(lambda eng: run_engine("vector", eng))
            block.scalar(lambda eng: run_engine("scalar", eng))
            block.gpsimd(lambda eng: run_engine("gpsimd", eng))
            block.sync(lambda eng: run_engine("sync", eng))
        stack.close()
        self.stats = dict(n_ops=len(real), incs=dict(cnt), dmas=dict(dma_cnt))


class KB:
    def __init__(self, nlayers=DEPTH, dbg=None, mixers=("hy", "sc", "gla", "gdn"), moe=True):
        self.nc = bass.Bass("TRN2", target_bir_lowering=False)
        self.P = Prog(self.nc)
        self.nl = nlayers
        self.dbg = dbg or {}
        self.mixers = mixers
        self.moe = moe
        self.gstack = contextlib.ExitStack()
        self.rr = 0
        self.names = {}

    def din(self, name, shape, dt=F32):
        return self.nc.dram_tensor(name, list(shape), dt, kind="ExternalInput").ap()

    def dout(self, name, shape, dt=F32):
        return self.nc.dram_tensor(name, list(shape), dt, kind="ExternalOutput").ap()

    def dscr(self, name, shape, dt=F32):
        if name in self.dbg:
            return self.nc.dram_tensor(name, list(shape), dt, kind="ExternalOutput").ap()
        return self.nc.dram_tensor(name, list(shape), dt, kind="Internal").ap()

    def _uniq(self, name):
        n = self.names.get(name, 0)
        self.names[name] = n + 1
        return name if n == 0 else f"{name}__{n}"

    def sb(self, st, name, shape, dt=F32):
        return st.enter_context(self.nc.sbuf_tensor(self._uniq(name), list(shape), dt))

    def ps(self, st, name, shape, dt=F32):
        nm = self._uniq(name)
        self.P.psum.add(nm)
        return st.enter_context(self.nc.psum_tensor(nm, list(shape), dt))

    def ew(self):
        self.rr += 1
        return "vector" if self.rr % 2 else "gpsimd"


class YTView:
    def __init__(self, ap3, k0):
        self.ap3, self.k0 = ap3, k0

    def __getitem__(self, idx):
        p, k, t = idx
        if isinstance(k, slice):
            k = slice(k.start - self.k0, k.stop - self.k0)
        else:
            k = k - self.k0
        return self.ap3[p, k, t]


def bcast_rows(ap_row, n):
    return ap_row.partition_broadcast(n)


def build(nlayers=DEPTH, dbg=None, mixers=("hy", "sc", "gla", "gdn"), moe=True):
    kb = KB(nlayers, dbg, mixers, moe)
    nc, P = kb.nc, kb.P
    L = nlayers
    xin = kb.din("xin", [TOK, D])
    c3T = kb.din("c3T", [128, 8, 3])
    w_ada = kb.din("w_ada", [DEPTH, D, 6 * D])
    b_ada = kb.din("b_ada", [DEPTH, 6 * D])
    w_in = kb.din("w_in", [DEPTH, D, D_IN])
    w_out = kb.din("w_out", [DEPTH, D, D])
    lnp = kb.din("lnp", [DEPTH, 4, D])
    w_router = kb.din("w_router", [DEPTH, D, NE])
    b_router = kb.din("b_router", [DEPTH, NE])
    w_gate = kb.din("w_gate", [DEPTH, NE, D, D]) if kb.moe else None
    w_up = kb.din("w_up", [DEPTH, NE, D, D]) if kb.moe else None
    w_down = kb.din("w_down", [DEPTH, NE, D, D]) if kb.moe else None
    bguT = kb.din("bguT", [DEPTH, 128, 2, NE, 8])
    b_down = kb.din("b_down", [DEPTH, NE, D])
    smallT = kb.din("smallT", [DEPTH, 128, 64])
    ident_d = kb.din("ident", [128, 128])
    sel_d = kb.din("sel3", [3, 3, 128])
    C = {}
    C["gla_w_a"] = kb.din("gla_w_a", [DEPTH, 2, 16, 128])
    C["gla_norm_w"] = kb.din("gla_norm_w", [DEPTH, 64])
    C["trimask"] = kb.din("trimask", [2, 128, 512])
    C["bdmask"] = kb.din("bdmask", [128, 256])
    C["hsel"] = kb.din("hsel", [128, 4])
    C["gdn_norm_w"] = kb.din("gdn_norm_w", [DEPTH, 64])
    C["bones"] = kb.din("bones", [128, 128])
    C["gsel"] = kb.din("gsel", [16, 2])
    C["selq"] = kb.din("selq", [16, 512])
    C["selr"] = kb.din("selr", [16, 1024])
    C["gmask"] = kb.din("gmask", [4, 128, 128])
    C["smask"] = kb.din("smask", [5, 128, 128])
    C["hy_w1"] = kb.din("hy_w1", [DEPTH, 33, 64])
    C["hy_w2"] = kb.din("hy_w2", [DEPTH, 64, 64])
    C["hy_w3"] = kb.din("hy_w3", [DEPTH, 64, 64])
    C["hy_w4"] = kb.din("hy_w4", [DEPTH, 64, 512])
    C["hyp"] = kb.din("hyp", [DEPTH, 64, 6])
    C["altcol"] = kb.din("altcol", [128, 1])
    C["altrow"] = kb.din("altrow", [1, SEQ])
    kb.KSPEC, kb.KNYQ = {}, {}
    for (nm, L_, nch, N_) in HYCFG:
        C[f"zT_{nm}"] = kb.din(f"zT_{nm}", [33, L_])
        C[f"win_{nm}"] = kb.din(f"win_{nm}", [L_, 256])
        C[f"wN_{nm}"] = kb.din(f"wN_{nm}", [128, nch])
        C[f"ctab_{nm}"] = kb.din(f"ctab_{nm}", [L_, L_], BF16)
        C[f"stab_{nm}"] = kb.din(f"stab_{nm}", [L_, L_], BF16)
        kb.KSPEC[nm] = kb.dscr(f"KSPEC_{nm}", [nch, 128, 512])
        kb.KNYQ[nm] = kb.dscr(f"KNYQ_{nm}", [1, 256])
    kb.C = C
    out = kb.dout("out", [NB * SEQ, D])
    XR = kb.dscr("XR", [TOK, D])
    kb.YTD = [kb.dscr(f"YTD{b}", [128, 8 * LS], BF16) for b in range(NB)]

    G = kb.gstack
    ident = kb.sb(G, "ident_sb", [128, 128])
    identb = kb.sb(G, "identb_sb", [128, 128], BF16)
    sel = kb.sb(G, "sel_sb", [3, 3 * 128])
    scT = kb.sb(G, "scT", [128, 8 * 3])
    P.dma("sync", ident[:], ident_d)
    P.dma("sync", sel[:], sel_d.rearrange("k j p -> k (j p)"))
    P.dma("sync", scT[:], c3T.rearrange("p k j -> p (k j)"))
    P.v(lambda e: e.tensor_copy(identb[:], ident[:]), [ident], [identb])
    P.a(lambda e: e.activation(scT[:], scT[:], AF.Silu), [scT], [scT])
    adaT = kb.sb(G, "adaT", [128, 3 * 48])
    ada1T = kb.sb(G, "ada1T", [128, 3 * 48])
    ADA = kb.dscr("ADA", [DEPTH * 3, 6 * D])
    small = kb.sb(G, "small_sb", [128, 64])

    def acol(j, m):
        return adaT[:, j * 48 + m: j * 48 + m + 1]

    def a1col(j, m):
        return ada1T[:, j * 48 + m: j * 48 + m + 1]

    for l in range(L):
        src = xin if l == 0 else XR
        last = (l == L - 1)
        with contextlib.ExitStack() as S:
            wblk = [kb.sb(S, f"adaw{i}", [128, 8 * 512]) for i in range(2)]
            bb = kb.sb(S, "adab", [3, 6 * D])
            ada_sb = kb.sb(S, "ada_sb", [3, 6 * D])
            pa = [kb.ps(S, f"adaps{i}", [128, 512]) for i in range(2)]
            pt = kb.ps(S, "adapt", [128, 3 * 48])
            P.dma("sync", bb[:], b_ada[l:l + 1, :].partition_broadcast(3) if False else
                  b_ada[l:l + 1, :].to_broadcast([3, 6 * D]))
            P.dma("sync", small[:], smallT[l])
            for n in range(12):
                wb = wblk[n % 2]
                P.dma("sync" if n % 2 else "gpsimd", wb[:].rearrange("p (k c) -> p k c", k=8),
                      w_ada[l, :, n * 512:(n + 1) * 512].rearrange("(k p) c -> p k c", p=128),
                      writes=[wb])
                pp = pa[n % 2]
                for k in range(8):
                    P.mm(pp[0:3, :], scT[:, k * 3:(k + 1) * 3], wb[:, k * 512:(k + 1) * 512],
                         start=(k == 0), stop=(k == 7), writes=[pp])
                P.v(lambda e, pp=pp, n=n: e.tensor_tensor(ada_sb[:, n * 512:(n + 1) * 512], pp[0:3, :],
                                                          bb[:, n * 512:(n + 1) * 512], ALU.add),
                    [pp, bb], [ada_sb])
            for j in range(3):
                pass
            for m in range(48):
                P.tr(pt[:, m * 3:(m + 1) * 3], ada_sb[0:3, m * 128:(m + 1) * 128], ident[0:3, 0:3],
                     writes=[pt])
            P.v(lambda e: e.tensor_copy(adaT[:].rearrange("p (j m) -> p j m", j=3),
                                        pt[:].rearrange("p (m j) -> p j m", j=3)), [pt], [adaT])
            P.v(lambda e: e.tensor_scalar_add(ada1T[:], adaT[:], 1.0), [adaT], [ada1T])
            P.dma("sync", ADA[l * 3:(l + 1) * 3, :], ada_sb[:])
        P.barrier()
        if kb.dbg.get("stop") == "ada":
            break
        if "adaT" in kb.dbg:
            dd = kb.dout(f"dbg_adaT{l}", [128, 144])
            P.dma("sync", dd, adaT[:])

        if "hy" in kb.mixers:
            with contextlib.ExitStack() as S:
                hyf_phase(kb, S, l, C)
            P.barrier()
        if kb.dbg.get("stop") == "hyf":
            break
        for b in range(NB):
            with contextlib.ExitStack() as S:
                mix_phase(kb, S, l, b, src, XR, w_in, w_out, lnp, small, ident, identb, sel, ADA,
                          acol, a1col)
            P.barrier()
            if kb.dbg.get("stop") in ("gla", "gdn"):
                break
        if kb.dbg.get("stop") in ("xm", "sc", "op", "gla", "gdn"):
            break
        with contextlib.ExitStack() as S:
            moe_phase(kb, S, l, last, XR, out, lnp, w_router, b_router, w_gate, w_up, w_down, bguT,
                      b_down, ident, sel, ADA, acol, a1col)
        P.barrier()
    P.emit()
    return kb


def ln_tile(kb, S_names, t2, gbc, bbc, outt, stats, mv, rstd, key_t2, key_out):
    P = kb.P
    for h in range(2):
        P.v(lambda e, h=h: e.bn_stats(stats[:, h * 6:(h + 1) * 6], t2[:, h * 512:(h + 1) * 512]),
            [key_t2], [stats])
    P.v(lambda e: e.bn_aggr(mv[:], stats[:]), [stats], [mv])
    P.a(lambda e: e.activation(rstd[:], mv[:, 1:2], AF.Sqrt, bias=LN_EPS), [mv], [rstd])
    P.v(lambda e: e.reciprocal(rstd[:], rstd[:]), [rstd], [rstd])
    P.v(lambda e: e.tensor_scalar(t2[:], t2[:], mv[:, 0:1], rstd[:, 0:1], ALU.subtract, ALU.mult),
        [key_t2, mv, rstd], [key_t2])
    P.g(lambda e: e.tensor_tensor(t2[:], t2[:], gbc, ALU.mult), [key_t2, gbc], [key_t2])
    P.g(lambda e: e.tensor_tensor(outt[:], t2[:], bbc, ALU.add), [key_t2, bbc], [key_out])


def gate_bcast(kb, dst, l, j, col0, ADA):
    r = l * 3 + j
    kb.P.dma("sync", dst[:], ADA[r:r + 1, col0:col0 + D].to_broadcast([128, D]))


def mix_phase(kb, S, l, b, src, XR, w_in, w_out, lnp, small, ident, identb, sel, ADA, acol, a1col):
    nc, P = kb.nc, kb.P
    C = kb.C
    tok0 = b * LS
    xmT = kb.sb(S, "xmT", [128, 8 * LS], BF16)
    YT2 = kb.sb(S, "YT2", [128, 2 * LS], BF16)
    xmT3 = xmT[:].rearrange("p (k t) -> p k t", k=8)
    YT23 = YT2[:].rearrange("p (k t) -> p k t", k=2)
    YTDb = kb.YTD[b].rearrange("p (k t) -> p k t", k=8)

    def yt_flush(k0):
        P.dma("sync", YTDb[:, k0:k0 + 2, :], YT23, reads=[("YT2", 0), ("YT2", 1)], writes=[("YTD", k0)])
    groups = [(0, 256, 2)] + [(256 + i * 512, 512, b) for i in range(4)]

    with contextlib.ExitStack() as S1:
        xt = [kb.sb(S1, f"xm_x{i}", [128, 4 * D]) for i in range(2)]
        pb = [kb.ps(S1, f"xm_ps{i}", [128, 512]) for i in range(4)]
        for gi, (t0, n, j) in enumerate(groups):
            x4 = xt[gi % 2]
            nt = n // 128
            P.dma("sync", x4[:, 0:nt * D].rearrange("p (a d) -> p a d", a=nt),
                  src[tok0 + t0: tok0 + t0 + n, :].rearrange("(a p) d -> p a d", p=128), writes=[x4])
            for k in range(8):
                pp = pb[k % 4]
                for a in range(nt):
                    P.tr(pp[:, a * 128:(a + 1) * 128], x4[:, a * D + k * 128: a * D + (k + 1) * 128],
                         ident[:], writes=[pp])
                dst = xmT3[:, k, t0:t0 + n]
                if k % 2 == 0:
                    P.v(lambda e, dst=dst, pp=pp, n=n, j=j, k=k: e.tensor_scalar(
                        dst, pp[:, 0:n], a1col(j, 8 + k), acol(j, k), ALU.mult, ALU.add),
                        [pp, "adaT", "ada1T"], [("xmT", gi)])
                else:
                    P.a(lambda e, dst=dst, pp=pp, n=n, j=j, k=k: e.activation(
                        dst, pp[:, 0:n], AF.Identity, bias=acol(j, k), scale=a1col(j, 8 + k)),
                        [pp, "adaT", "ada1T"], [("xmT", gi)])
    P.barrier()
    if kb.dbg.get("stop") == "xm":
        return
    xm_keys = [("xmT", gi) for gi in range(5)]

    def inproj_fm(S2, c0, ncols, dsts, evac=None):
        nch = (ncols + 127) // 128
        wsl = kb.sb(S2, f"wsl_{c0}", [128, 8 * ncols], BF16)
        P.dma("gpsimd", wsl[:].rearrange("p (k c) -> p k c", k=8),
              w_in[l, :, c0:c0 + ncols].rearrange("(k p) c -> p k c", p=128), writes=[wsl])
        pp2 = [kb.ps(S2, f"ip_ps{c0}_{i}", [128, 512]) for i in range(2)]
        i = 0
        for gi, (t0, n, j) in enumerate(groups):
            for ch in range(nch):
                m = min(128, ncols - ch * 128)
                pp = pp2[i % 2]
                i += 1
                for k in range(8):
                    P.mm(pp[0:m, 0:n], wsl[:, k * ncols + ch * 128: k * ncols + ch * 128 + m],
                         xmT3[:, k, t0:t0 + n], start=(k == 0), stop=(k == 7),
                         reads=[wsl, ("xmT", gi)], writes=[pp])
                d = dsts[ch]
                if i % 2:
                    P.v(lambda e, d=d, pp=pp, m=m, n=n, t0=t0: e.tensor_copy(d[0:m, t0:t0 + n], pp[0:m, 0:n]),
                        [pp], [d])
                else:
                    P.a(lambda e, d=d, pp=pp, m=m, n=n, t0=t0: e.copy(d[0:m, t0:t0 + n], pp[0:m, 0:n]),
                        [pp], [d])

    def conv3(dst, srcp, wcols, bias=None, eng="vector"):
        w0, w1, w2 = wcols
        if bias is None:
            P.op(eng, lambda e: e.tensor_scalar(dst[:], srcp[:], w1, None, ALU.mult), [srcp, small], [dst])
        else:
            P.op(eng, lambda e: e.tensor_scalar(dst[:], srcp[:], w1, bias, ALU.mult, ALU.add),
                 [srcp, small], [dst])
        for (o0, rows, rl) in ((0, 1, CTX), (CTX, SEQ // 64, 64)):
            dv = dst[:, o0:o0 + rows * rl].rearrange("p (r c) -> p r c", r=rows)
            sv = srcp[:, o0:o0 + rows * rl].rearrange("p (r c) -> p r c", r=rows)
            P.v(lambda e, dv=dv, sv=sv, rl=rl: e.scalar_tensor_tensor(
                dv[:, :, 1:rl], sv[:, :, 0:rl - 1], w0, dv[:, :, 1:rl], ALU.mult, ALU.add),
                [srcp, dst, small], [dst])
            P.v(lambda e, dv=dv, sv=sv, rl=rl: e.scalar_tensor_tensor(
                dv[:, :, 0:rl - 1], sv[:, :, 1:rl], w2, dv[:, :, 0:rl - 1], ALU.mult, ALU.add),
                [srcp, dst, small], [dst])

    off = {"hy": 0, "sc": 2, "gla": 4, "gdn": 6}
    for name, k0 in off.items():
        if name not in kb.mixers:
            P.g(lambda e: e.memset(YT2[:], 0.0), [], [("YT2", 0), ("YT2", 1)])
            yt_flush(k0)

    if "hy" in kb.mixers:
        with contextlib.ExitStack() as S2:
            hyena_mixer(kb, S2, l, b, xmT3, YT23, groups, w_in, C, small, identb, conv3, yt_flush)
        P.barrier()
    if kb.dbg.get("stop") == "hy":
        return

    if "sc" in kb.mixers:
        with contextlib.ExitStack() as S2:
            pch = [kb.sb(S2, f"sc_p{i}", [128, LS]) for i in range(6)]
            inproj_fm(S2, SC0, 768, pch)
            cv = kb.sb(S2, "sc_cv", [128, LS])
            for ch in range(2):
                Bc, Cc, hc = pch[ch], pch[2 + ch], pch[4 + ch]
                P.g(lambda e, Cc=Cc, hc=hc: e.tensor_tensor(Cc[:], Cc[:], hc[:], ALU.mult), [Cc, hc], [Cc])
                wc = tuple(small[:, 26 + 3 * ch + k: 27 + 3 * ch + k] for k in range(3))
                conv3(cv, Cc, wc)
                P.v(lambda e, Bc=Bc, ch=ch: e.tensor_tensor(YT23[:, ch, :], Bc[:], cv[:], ALU.mult),
                    [Bc, cv], [("YT2", ch)])
            yt_flush(2)
        P.barrier()
    if kb.dbg.get("stop") == "sc":
        return


    if "gla" in kb.mixers:
        with contextlib.ExitStack() as S2:
            gla_mixer(kb, S2, l, b, xmT3, YTView(YT23, 4), groups, w_in, C, small, identb)
            yt_flush(4)
        P.barrier()
    if kb.dbg.get("stop") == "gla":
        return

    if "gdn" in kb.mixers:
        with contextlib.ExitStack() as S2:
            gdn_mixer(kb, S2, l, b, xmT3, YTView(YT23, 6), groups, w_in, C, small, ident, identb, conv3)
            yt_flush(6)
        P.barrier()
    if kb.dbg.get("stop") == "gdn":
        return

    with contextlib.ExitStack() as S3:
        YT = kb.sb(S3, "YT", [128, 8 * LS], BF16)
        YT3 = YT[:].rearrange("p (k t) -> p k t", k=8)
        P.dma("sync", YT[:], kb.YTD[b], reads=[("YTD", k) for k in (0, 2, 4, 6)], writes=[("YT", k) for k in range(8)])
        if "YT" in kb.dbg and l == 0:
            dd = kb.dout(f"dbg_YT{b}", [128, 8 * LS], BF16)
            P.dma("sync", dd, YT[:], reads=[("YT", k) for k in range(8)])
        wo = kb.sb(S3, "wo", [128, 8 * D], BF16)
        P.dma("gpsimd", wo[:].rearrange("p (k c) -> p k c", k=8),
              w_out[l].rearrange("(k p) c -> p k c", p=128), writes=[wo])
        gbc = kb.sb(S3, "ln1g", [128, D])
        bbc = kb.sb(S3, "ln1b", [128, D])
        P.dma("sync", gbc[:], lnp[l, 0:1, :].to_broadcast([128, D]))
        P.dma("sync", bbc[:], lnp[l, 1:2, :].to_broadcast([128, D]))
        po = [kb.ps(S3, f"op_ps{i}", [128, 512]) for i in range(4)]
        g1 = {}
        for j in (b, 2):
            g1[j] = kb.sb(S3, f"g1bc{j}", [128, D])
            gate_bcast(kb, g1[j], l, j, 2048, ADA)
        xts = [kb.sb(S3, f"op_x{i}", [128, D]) for i in range(2)]
        t1s = [kb.sb(S3, f"op_t{i}", [128, D]) for i in range(2)]
        ots = [kb.sb(S3, f"op_o{i}", [128, D]) for i in range(2)]
        stats = kb.sb(S3, "op_stats", [128, 12])
        mv = kb.sb(S3, "op_mv", [128, 2])
        rstd = kb.sb(S3, "op_rstd", [128, 1])
        yk = [("YT", k) for k in range(8)]
        for ti in range(NT):
            j = 2 if ti < 2 else b
            xt, t1, ot = xts[ti % 2], t1s[ti % 2], ots[ti % 2]
            r0 = tok0 + ti * 128
            P.dma("sync", xt[:], src[r0:r0 + 128, :])
            for h in range(2):
                pp = po[(ti % 2) * 2 + h]
                for k in range(8):
                    P.mm(pp[:, :], YT3[:, k, ti * 128:(ti + 1) * 128], wo[:, k * D + h * 512: k * D + (h + 1) * 512],
                         start=(k == 0), stop=(k == 7), reads=[wo] + yk, writes=[pp])
                P.v(lambda e, pp=pp, t1=t1, h=h, j=j: e.tensor_tensor(
                    t1[:, h * 512:(h + 1) * 512], pp[:, :], g1[j][:, h * 512:(h + 1) * 512], ALU.mult),
                    [pp, g1[j]], [t1])
            P.v(lambda e, xt=xt, t1=t1: e.scalar_tensor_tensor(t1[:], xt[:], float(DN_ALPHA), t1[:],
                                                               ALU.mult, ALU.add), [xt, t1], [t1])
            ln_tile(kb, None, t1, gbc[:], bbc[:], ot, stats, mv, rstd, t1, ot)
            P.dma("sync", XR[r0:r0 + 128, :], ot[:], writes=[("XR", r0 // 128)])


def finish_norm_gate(kb, S2, osum, rtok, nwb, sq_ap, sq_key, ytok, y_key, pT, YT3, k0, identb):
    P = kb.P
    o3 = osum[:].rearrange("p (a v) -> p a v", v=64)
    ms = kb.sb(S2, "fin_ms", [128, NT * 4])
    for hf in range(2):
        P.v(lambda e, hf=hf: e.tensor_tensor(sq_ap, osum[:, hf * LS:(hf + 1) * LS], osum[:, hf * LS:(hf + 1) * LS], ALU.mult),
            [osum], [sq_key])
        P.v(lambda e, hf=hf: e.reduce_sum(ms[:, hf * NT * 2:(hf + 1) * NT * 2], sq_ap.rearrange("p (a v) -> p a v", v=64), AX.X),
            [sq_key], [ms])
    P.a(lambda e: e.activation(ms[:], ms[:], AF.Sqrt, bias=1e-6, scale=1.0 / 64), [ms], [ms])
    P.v(lambda e: e.reciprocal(ms[:], ms[:]), [ms], [ms])
    msbc = ms[:].rearrange("p (a o) -> p a o", o=1).to_broadcast([128, NT * 4, 64])
    nwbc = nwb[:].rearrange("p (o v) -> p o v", o=1).to_broadcast([128, NT * 4, 64])
    P.v(lambda e: e.tensor_tensor(o3, o3, msbc, ALU.mult), [osum, ms], [osum])
    P.g(lambda e: e.tensor_tensor(o3, o3, nwbc, ALU.mult), [osum, nwb], [osum])
    P.v(lambda e: e.tensor_tensor(ytok, osum[:], rtok[:], ALU.mult), [osum, rtok], [y_key])
    for ti in range(NT):
        for c2 in range(2):
            P.tr(pT[:, (ti % 4) * 256 + c2 * 128:(ti % 4) * 256 + (c2 + 1) * 128],
                 ytok[:, ti * 256 + c2 * 128: ti * 256 + (c2 + 1) * 128], identb[:], reads=[y_key, identb], writes=[pT])
            P.v(lambda e, ti=ti, c2=c2: e.tensor_copy(
                YT3[:, k0 + c2, ti * 128:(ti + 1) * 128],
                pT[:, (ti % 4) * 256 + c2 * 128:(ti % 4) * 256 + (c2 + 1) * 128]), [pT], [("YT2", c2)])


def inproj_fm_cols(kb, S2, l, w_in, c0, ncols, xmT3, groups, dst_fn, tag):
    P = kb.P
    nch = (ncols + 127) // 128
    wsl = kb.sb(S2, f"wsl_{tag}", [128, 8 * ncols], BF16)
    P.dma("gpsimd", wsl[:].rearrange("p (k c) -> p k c", k=8),
          w_in[l, :, c0:c0 + ncols].rearrange("(k p) c -> p k c", p=128), writes=[wsl])
    pp2 = [kb.ps(S2, f"ipc_ps{tag}_{i}", [128, 512]) for i in range(2)]
    i = 0
    for gi, (t0, n, j) in enumerate(groups):
        for ch in range(nch):
            m = min(128, ncols - ch * 128)
            pp = pp2[i % 2]
            i += 1
            for k in range(8):
                P.mm(pp[0:m, 0:n], wsl[:, k * ncols + ch * 128: k * ncols + ch * 128 + m],
                     xmT3[:, k, t0:t0 + n], start=(k == 0), stop=(k == 7),
                     reads=[wsl, ("xmT", gi)], writes=[pp])
            dst_fn(ch, t0, n, pp, m)


def inproj_tm_cols(kb, S2, l, w_in, c0, ncols, xmT3, dst_fn, tag):
    P = kb.P
    wsl = kb.sb(S2, f"wslt_{tag}", [128, 8 * ncols], BF16)
    P.dma("gpsimd", wsl[:].rearrange("p (k c) -> p k c", k=8),
          w_in[l, :, c0:c0 + ncols].rearrange("(k p) c -> p k c", p=128), writes=[wsl])
    pp2 = [kb.ps(S2, f"ipt_ps{tag}_{i}", [128, 512]) for i in range(2)]
    for ti in range(NT):
        gi = 0 if ti < 2 else 1 + (ti - 2) // 4
        pp = pp2[ti % 2]
        for k in range(8):
            P.mm(pp[:, 0:ncols], xmT3[:, k, ti * 128:(ti + 1) * 128], wsl[:, k * ncols:(k + 1) * ncols],
                 start=(k == 0), stop=(k == 7), reads=[wsl, ("xmT", gi)], writes=[pp])
        dst_fn(ti, pp)


def chunk_order(d):
    if d == 0:
        return list(range(NT))
    return [1, 0] + list(range(NT - 1, 1, -1))


def gla_mixer(kb, S2, l, b, xmT3, YT3, groups, w_in, C, small, identb):
    nc, P = kb.nc, kb.P
    G0 = GLA0
    qT = kb.sb(S2, "gla_qT", [128, LS])
    kT = kb.sb(S2, "gla_kT", [128, LS])
    vtok = kb.sb(S2, "gla_v", [128, NT * 256], BF16)
    rtok = kb.sb(S2, "gla_r", [128, NT * 256], BF16)
    osum = kb.sb(S2, "gla_o", [128, NT * 256])
    waT = kb.sb(S2, "gla_wa", [16, 256])
    nwb = kb.sb(S2, "gla_nw", [128, 64])
    negs = kb.sb(S2, "gla_negs", [128, 2])
    if kb.dbg.get("stop2") == "g0":
        return
    P.dma("sync", waT[:].rearrange("r (d c) -> r d c", d=2), C["gla_w_a"][l].rearrange("d r c -> r d c"))
    P.dma("sync", nwb[:], C["gla_norm_w"][l:l + 1, :].to_broadcast([128, 64]))
    P.v(lambda e: e.tensor_scalar(negs[:], small[:, 50:52], -1.0, None, ALU.mult), [small], [negs])
    with contextlib.ExitStack() as S3:
        def ev_qk(ch, t0, n, pp, m):
            d = qT if ch == 0 else kT
            P.v(lambda e: e.tensor_copy(d[:, t0:t0 + n], pp[:, 0:n]), [pp], [d])
        inproj_fm_cols(kb, S3, l, w_in, G0, 256, xmT3, groups, ev_qk, "glaqk")
    P.barrier()
    if kb.dbg.get("stop2") == "g0b":
        return
    with contextlib.ExitStack() as S3:
        def ev_vr(ti, pp):
            P.v(lambda e: e.tensor_copy(vtok[:, ti * 256:(ti + 1) * 256], pp[:, 0:256]), [pp], [vtok])
            P.a(lambda e: e.activation(rtok[:, ti * 256:(ti + 1) * 256], pp[:, 256:512], AF.Silu), [pp], [rtok])
        inproj_tm_cols(kb, S3, l, w_in, G0 + 256, 512, xmT3, ev_vr, "glavr")
    P.barrier()
    if kb.dbg.get("stop2") == "g1":
        return
    cm = kb.sb(S2, "gla_cm", [128, LS], BF16)
    P.g(lambda e: e.memset(cm[:], 1.0), [], [cm])
    P.g(lambda e: e.memset(cm[:].rearrange("p (c t) -> p c t", t=128)[:, :, 0:1], 0.0), [cm], [cm])
    msk = kb.sb(S2, "gla_msk", [128, 2 * 512], BF16)
    P.dma("gpsimd", msk[:].rearrange("p (d c) -> p d c", d=2), C["trimask"].rearrange("d p c -> p d c"))
    bdm = kb.sb(S2, "gla_bdm", [128, 256])
    P.dma("sync", bdm[:], C["bdmask"])
    if kb.dbg.get("stop2") == "g2":
        return
    aT = kb.sb(S2, "gla_aT", [16, LS])
    Bt = kb.sb(S2, "gla_B", [128, LS])
    Tm = kb.sb(S2, "gla_T", [128, NT])
    eT = kb.sb(S2, "gla_eT", [128, NT])
    tmp = kb.sb(S2, "gla_tmp", [128, LS])
    qt_ = kb.sb(S2, "gla_qt", [128, LS], BF16)
    kt_ = kb.sb(S2, "gla_kt", [128, LS], BF16)
    qe_ = kb.sb(S2, "gla_qe", [128, LS], BF16)
    kp_ = kb.sb(S2, "gla_kp", [128, LS], BF16)
    kptok = kb.sb(S2, "gla_kptok", [128, NT * 128], BF16)
    ATs = [kb.sb(S2, f"gla_AT{i}", [128, 512], BF16) for i in range(2)]
    Qb = [kb.sb(S2, f"gla_Qb{i}", [128, 512], BF16) for i in range(2)]
    hsel = kb.sb(S2, "gla_hsel", [128, 4])
    P.dma("sync", hsel[:], C["hsel"])
    Sf = kb.sb(S2, "gla_Sf", [128, 256])
    Sb = kb.sb(S2, "gla_Sb", [128, 256], BF16)
    stmp = kb.sb(S2, "gla_stmp", [128, 256])
    pA = [kb.ps(S2, f"gla_pA{i}", [128, 512]) for i in range(2)]
    pO = [kb.ps(S2, f"gla_pO{i}", [128, 512]) for i in range(2)]
    pS = kb.ps(S2, "gla_pS", [128, 512])
    pT = kb.ps(S2, "gla_pT", [128, 1024], BF16)
    ex = kb.sb(S2, "gla_ex", [128, LS])
    B3 = Bt[:].rearrange("p (c t) -> p c t", t=128)
    tmp3 = tmp[:].rearrange("p (c t) -> p c t", t=128)
    SC = 32 ** -0.5
    for d in range(2):
        with contextlib.ExitStack() as S3:
            def ev_a(ch, t0, n, pp, m):
                P.v(lambda e: e.tensor_copy(aT[:, t0:t0 + n], pp[0:16, 0:n]), [pp], [aT])
            inproj_fm_cols(kb, S3, l, w_in, G0 + 768 + 16 * d, 16, xmT3, groups, ev_a, f"glaa{d}")
            pg = pA
            for gi, (t0, n, j) in enumerate(groups):
                pp = pg[gi % 2]
                P.mm(pp[:, 0:n], waT[:, d * 128:(d + 1) * 128], aT[:, t0:t0 + n])
                P.a(lambda e, pp=pp, t0=t0, n=n, d=d: e.activation(tmp[:, t0:t0 + n], pp[:, 0:n], AF.Exp,
                                                              bias=negs[:, d:d + 1], scale=-1.0),
                    [pp, negs], [tmp])
            P.a(lambda e: e.activation(tmp[:], tmp[:], AF.Ln, bias=1.0), [tmp], [tmp])
        P.barrier()
        if kb.dbg.get("stop2") == "g3":
            return
        P.v(lambda e: e.tensor_tensor_scan(Bt[:], cm[:], tmp[:], 0.0, ALU.mult, ALU.add), [cm, tmp], [Bt])
        if kb.dbg.get("stop2") == "g3b":
            return
        P.v(lambda e: e.tensor_copy(Tm[:].rearrange("p (c o) -> p c o", o=1), B3[:, :, 127:128]), [Bt], [Tm])
        Tbc = Tm[:].rearrange("p (c o) -> p c o", o=1).to_broadcast([128, NT, 128])
        if d == 1:
            P.v(lambda e: e.tensor_tensor(B3, Tbc, B3, ALU.subtract), [Tm, Bt], [Bt])
            P.g(lambda e: e.tensor_tensor(Bt[:], Bt[:], tmp[:], ALU.add), [Bt, tmp], [Bt])
        P.a(lambda e: e.activation(eT[:], Tm[:], AF.Exp, scale=-1.0 / 16), [Tm], [eT])
        Rbc = B3[:, :, 64:65].to_broadcast([128, NT, 128])
        P.v(lambda e: e.tensor_tensor(tmp3, B3, Rbc, ALU.subtract), [Bt], [tmp])
        P.a(lambda e: e.activation(ex[:], tmp[:], AF.Exp, scale=-1.0 / 16), [tmp], [ex])
        P.v(lambda e: e.scalar_tensor_tensor(qt_[:], qT[:], SC, ex[:], ALU.mult, ALU.mult), [qT, ex], [qt_])
        P.a(lambda e: e.activation(ex[:], tmp[:], AF.Exp, scale=1.0 / 16), [tmp, qt_], [ex])
        P.g(lambda e: e.tensor_tensor(kt_[:], kT[:], ex[:], ALU.mult), [kT, ex], [kt_])
        P.a(lambda e: e.activation(ex[:], Bt[:], AF.Exp, scale=-1.0 / 16), [Bt, kt_], [ex])
        P.v(lambda e: e.scalar_tensor_tensor(qe_[:], qT[:], SC, ex[:], ALU.mult, ALU.mult), [qT, ex], [qe_])
        P.v(lambda e: e.tensor_tensor(tmp3, Tbc, B3, ALU.subtract), [Tm, Bt], [tmp])
        P.a(lambda e: e.activation(ex[:], tmp[:], AF.Exp, scale=-1.0 / 16), [tmp, qe_], [ex])
        P.g(lambda e: e.tensor_tensor(kp_[:], kT[:], ex[:], ALU.mult), [kT, ex], [kp_])
        if kb.dbg.get("stop2") == "g4":
            return
        for c4 in range(0, NT, 8):
            nn = min(8, NT - c4)
            for cc in range(nn):
                c = c4 + cc
                P.tr(pT[:, cc * 128:(cc + 1) * 128], kp_[:, c * 128:(c + 1) * 128], identb[:], writes=[pT])
            P.v(lambda e, c4=c4, nn=nn: e.tensor_copy(kptok[:, c4 * 128:(c4 + nn) * 128], pT[:, 0:nn * 128]),
                [pT], [kptok])
        if kb.dbg.get("stop2") == "g5":
            return
        P.g(lambda e: e.memset(Sf[:], 0.0), [], [Sf])
        P.g(lambda e: e.memset(Sb[:], 0.0), [], [Sb])
        for ci, c in enumerate(chunk_order(d)):
            cs = slice(c * 128, (c + 1) * 128)
            pa, po, at = pA[ci % 2], pO[ci % 2], ATs[ci % 2]
            qb = Qb[ci % 2]
            P.g(lambda e, qb=qb, cs=cs: e.tensor_tensor(
                qb[:].rearrange("p (h i) -> p h i", h=4),
                qt_[:, cs].rearrange("p (o i) -> p o i", o=1).to_broadcast([128, 4, 128]),
                hsel[:].rearrange("p (h o) -> p h o", o=1).to_broadcast([128, 4, 128]), ALU.mult),
                [qt_, hsel], [qb])
            P.mm(pa[:, :], kt_[:, cs], qb[:], writes=[pa])
            P.v(lambda e, pa=pa, at=at, d=d: e.tensor_tensor(at[:], pa[:], msk[:, d * 512:(d + 1) * 512], ALU.mult),
                [pa, msk], [at])
            P.mm(po[:, 0:256], qe_[:, cs], Sb[:], start=True, stop=False, writes=[po])
            for h in range(4):
                P.mm(po[:, h * 64:(h + 1) * 64], at[:, h * 128:(h + 1) * 128],
                     vtok[:, c * 256 + h * 64: c * 256 + (h + 1) * 64], start=False, stop=(h == 3),
                     writes=[po])
            osl = osum[:, c * 256:(c + 1) * 256]
            if d == 0:
                P.a(lambda e, osl=osl, po=po: e.copy(osl, po[:, 0:256]), [po], [osum])
            else:
                P.v(lambda e, osl=osl, po=po: e.tensor_tensor(osl, osl, po[:, 0:256], ALU.add), [po, osum], [osum])
            P.mm(pS[:, 0:256], kptok[:, cs], vtok[:, c * 256:(c + 1) * 256])
            P.v(lambda e: e.tensor_tensor(stmp[:], pS[:, 0:256], bdm[:], ALU.mult), [pS, bdm], [stmp])
            P.v(lambda e, c=c: e.scalar_tensor_tensor(Sf[:], Sf[:], eT[:, c:c + 1], stmp[:], ALU.mult, ALU.add),
                [Sf, eT, stmp], [Sf])
            P.g(lambda e: e.tensor_copy(Sb[:], Sf[:]), [Sf], [Sb])
    if kb.dbg.get("stop2") == "pre":
        return
    finish_norm_gate(kb, S2, osum, rtok, nwb, tmp[:], tmp, Bt[:].bitcast(BF16), Bt, pT, YT3, 4, identb)


def gdn_mixer(kb, S2, l, b, xmT3, YT3, groups, w_in, C, small, ident, identb, conv3):
    nc, P = kb.nc, kb.P
    G0 = GDN0
    qh = [kb.sb(S2, f"gd_qh{i}", [128, LS], BF16) for i in range(2)]
    kh = [kb.sb(S2, f"gd_kh{i}", [128, LS], BF16) for i in range(2)]
    ktok = kb.sb(S2, "gd_ktok", [128, NT * 256], BF16)
    vtok = kb.sb(S2, "gd_vtok", [128, NT * 256], BF16)
    ztok = kb.sb(S2, "gd_ztok", [128, NT * 256], BF16)
    osum = kb.sb(S2, "gd_osum", [128, NT * 256])
    nwb = kb.sb(S2, "gd_nw", [128, 64])
    gcol = kb.sb(S2, "gd_gcol", [128, NT * 48])
    colE = kb.sb(S2, "gd_colE", [128, NT * 8])
    colA = kb.sb(S2, "gd_colA", [128, NT * 8])
    colK = kb.sb(S2, "gd_colK", [128, NT * 8])
    eTot = kb.sb(S2, "gd_eTot", [128, 4 * NT])
    qe = [[kb.sb(S2, f"gd_qe{d}{i}", [128, LS], BF16) for i in range(2)] for d in range(2)]
    Crow = kb.sb(S2, "gd_Crow", [16, LS])
    gc3 = gcol[:].rearrange("p (c x) -> p c x", x=48)
    P.dma("sync", nwb[:], C["gdn_norm_w"][l:l + 1, :].to_broadcast([128, 64]))
    with contextlib.ExitStack() as S3:
        pbuf = kb.sb(S3, "gd_pbuf", [128, LS])
        cbuf = kb.sb(S3, "gd_cbuf", [128, LS])
        bones = kb.sb(S3, "gd_bones", [128, 128])
        P.dma("sync", bones[:], C["bones"])
        wsl = kb.sb(S3, "gd_wsl", [128, 8 * 768], BF16)
        P.dma("gpsimd", wsl[:].rearrange("p (k c) -> p k c", k=8),
              w_in[l, :, G0:G0 + 768].rearrange("(k p) c -> p k c", p=128), writes=[wsl])
        pp2 = [kb.ps(S3, f"gd_ps{i}", [128, 512]) for i in range(2)]
        pn2 = [kb.ps(S3, f"gd_pn{i}", [128, 512]) for i in range(2)]
        ptb = kb.ps(S3, "gd_ptb", [128, 1024], BF16)
        i = 0
        for ch in range(6):
            for gi, (t0, n, j) in enumerate(groups):
                pp = pp2[i % 2]
                i += 1
                for k in range(8):
                    P.mm(pp[:, 0:n], wsl[:, k * 768 + ch * 128: k * 768 + (ch + 1) * 128],
                         xmT3[:, k, t0:t0 + n], start=(k == 0), stop=(k == 7),
                         reads=[wsl, ("xmT", gi)], writes=[pp])
                P.a(lambda e, pp=pp, t0=t0, n=n: e.copy(pbuf[:, t0:t0 + n], pp[:, 0:n]), [pp], [pbuf])
            wc = tuple(small[:, 32 + 3 * ch + k: 33 + 3 * ch + k] for k in range(3))
            conv3(cbuf, pbuf, wc)
            P.a(lambda e: e.activation(cbuf[:], cbuf[:], AF.Silu), [cbuf], [cbuf])
            if ch < 4:
                P.g(lambda e: e.tensor_tensor(pbuf[:], cbuf[:], cbuf[:], ALU.mult), [cbuf], [pbuf])
                for gi, (t0, n, j) in enumerate(groups):
                    pn = pn2[gi % 2]
                    P.mm(pn[:, 0:n], bones[:], pbuf[:, t0:t0 + n], writes=[pn])
                    P.a(lambda e, pn=pn, t0=t0, n=n: e.activation(pbuf[:, t0:t0 + n], pn[:, 0:n], AF.Sqrt, bias=1e-6),
                        [pn], [pbuf])
                P.v(lambda e: e.reciprocal(pbuf[:], pbuf[:]), [pbuf], [pbuf])
                dst = qh[ch] if ch < 2 else kh[ch - 2]
                sc_ = 0.125 if ch < 2 else 1.0
                P.v(lambda e, dst=dst, sc_=sc_: e.scalar_tensor_tensor(dst[:], cbuf[:], sc_, pbuf[:], ALU.mult, ALU.mult),
                    [cbuf, pbuf], [dst])
            else:
                P.v(lambda e: e.tensor_copy(pbuf[:].bitcast(BF16)[:, 0:LS], cbuf[:]), [cbuf], [pbuf])
                vb16 = pbuf[:].bitcast(BF16)
                for c4 in range(0, NT, 8):
                    nn = min(8, NT - c4)
                    for cc in range(nn):
                        c = c4 + cc
                        P.tr(ptb[:, cc * 128:(cc + 1) * 128], vb16[:, c * 128:(c + 1) * 128], identb[:],
                             reads=[pbuf, identb], writes=[ptb])
                    P.v(lambda e, c4=c4, nn=nn, ch=ch: e.tensor_copy(
                        vtok[:].rearrange("p (c x) -> p c x", x=256)[:, c4:c4 + nn, (ch - 4) * 128:(ch - 3) * 128],
                        ptb[:, 0:nn * 128].rearrange("p (c x) -> p c x", x=128)), [ptb], [vtok])
        for hc in range(2):
            for c4 in range(0, NT, 8):
                nn = min(8, NT - c4)
                for cc in range(nn):
                    c = c4 + cc
                    P.tr(ptb[:, cc * 128:(cc + 1) * 128], kh[hc][:, c * 128:(c + 1) * 128], identb[:], writes=[ptb])
                P.v(lambda e, c4=c4, nn=nn, hc=hc: e.tensor_copy(
                    ktok[:].rearrange("p (c x) -> p c x", x=256)[:, c4:c4 + nn, hc * 128:(hc + 1) * 128],
                    ptb[:, 0:nn * 128].rearrange("p (c x) -> p c x", x=128)), [ptb], [ktok])
    P.barrier()
    if kb.dbg.get("stop2") == "s1":
        return
    with contextlib.ExitStack() as S3:
        def ev_z(ti, pp):
            P.a(lambda e: e.activation(ztok[:, ti * 256:(ti + 1) * 256], pp[:, 0:256], AF.Silu), [pp], [ztok])
        inproj_tm_cols(kb, S3, l, w_in, G0 + 768, 256, xmT3, ev_z, "gdz")
    P.barrier()
    if kb.dbg.get("stop2") == "s2":
        return
    with contextlib.ExitStack() as S3:
        gT = kb.sb(S3, "gd_gT", [16, LS])
        SG = kb.sb(S3, "gd_SG", [16, LS])
        PRE = kb.sb(S3, "gd_PRE", [16, LS])
        SUF = kb.sb(S3, "gd_SUF", [16, LS])
        Rr = kb.sb(S3, "gd_R", [16, LS])
        cm = kb.sb(S3, "gd_cm", [16, LS], BF16)
        Tt = kb.sb(S3, "gd_T", [16, NT])
        ea = kb.sb(S3, "gd_ea", [16, 1])
        gsel = kb.sb(S3, "gd_gsel", [16, 2])
        selq = kb.sb(S3, "gd_selq", [16, 4 * 128])
        exb = kb.sb(S3, "gd_exb", [128, 512])
        P.dma("sync", gsel[:], C["gsel"])
        P.dma("sync", selq[:], C["selq"])
        P.g(lambda e: e.memset(cm[:], 1.0), [], [cm])
        P.g(lambda e: e.memset(cm[:].rearrange("p (c t) -> p c t", t=128)[:, :, 0:1], 0.0), [cm], [cm])
        P.a(lambda e: e.activation(ea[:], small[0:16, 53:54], AF.Exp), [small], [ea])
        with contextlib.ExitStack() as S4:
            def ev_g(ch, t0, n, pp, m):
                P.v(lambda e: e.tensor_copy(gT[:, t0:t0 + n], pp[0:16, 0:n]), [pp], [gT])
            inproj_fm_cols(kb, S4, l, w_in, G0 + 1024, 16, xmT3, groups, ev_g, "gdg")
        P.barrier()
        P.a(lambda e: e.activation(SG[:], gT[:], AF.Sigmoid), [gT], [SG])
        P.a(lambda e: e.activation(gT[:], gT[:], AF.Exp, bias=small[0:16, 52:53]), [gT, small, SG], [gT])
        P.a(lambda e: e.activation(gT[:], gT[:], AF.Ln, bias=1.0), [gT], [gT])
        P.v(lambda e: e.tensor_scalar(gT[:], gT[:], ea[:, 0:1], None, ALU.mult), [gT, ea], [gT])
        P.v(lambda e: e.tensor_tensor_scan(PRE[:], cm[:], gT[:], 0.0, ALU.mult, ALU.add), [cm, gT], [PRE])
        P3 = PRE[:].rearrange("p (c t) -> p c t", t=128)
        S3v = SUF[:].rearrange("p (c t) -> p c t", t=128)
        P.v(lambda e: e.tensor_copy(Tt[:].rearrange("p (c o) -> p c o", o=1), P3[:, :, 127:128]), [PRE], [Tt])
        Tbc = Tt[:].rearrange("p (c o) -> p c o", o=1).to_broadcast([16, NT, 128])
        P.v(lambda e: e.tensor_tensor(S3v, Tbc, P3, ALU.subtract), [Tt, PRE], [SUF])
        P.v(lambda e: e.tensor_tensor(SUF[:], SUF[:], gT[:], ALU.add), [SUF, gT], [SUF])
        P.v(lambda e: e.tensor_scalar(Rr[:], SUF[:], gsel[:, 0:1], None, ALU.mult), [SUF, gsel], [Rr])
        P.v(lambda e: e.scalar_tensor_tensor(Rr[:], PRE[:], gsel[:, 1:2], Rr[:], ALU.mult, ALU.add), [PRE, gsel, Rr], [Rr])
        P.v(lambda e: e.tensor_tensor(Rr[:], Rr[:], gT[:], ALU.subtract), [Rr, gT], [Rr])
        P.v(lambda e: e.tensor_scalar(Crow[:], PRE[:], gsel[:, 0:1], None, ALU.mult), [PRE, gsel], [Crow])
        P.v(lambda e: e.scalar_tensor_tensor(Crow[:], SUF[:], gsel[:, 1:2], Crow[:], ALU.mult, ALU.add),
            [SUF, gsel, Crow], [Crow])
        pg = [kb.ps(S3, f"gd_pg{i}", [128, 512]) for i in range(2)]
        for c in range(NT):
            pp = pg[c % 2]
            cs = slice(c * 128, (c + 1) * 128)
            P.tr(pp[:, 0:16], Crow[:, cs], ident[0:16, 0:16], writes=[pp])
            P.tr(pp[:, 16:32], Rr[:, cs], ident[0:16, 0:16], writes=[pp])
            P.tr(pp[:, 32:48], SG[:, cs], ident[0:16, 0:16], writes=[pp])
            P.v(lambda e, pp=pp, c=c: e.tensor_copy(gcol[:, c * 48:(c + 1) * 48], pp[:, 0:48]), [pp], [gcol])
        i = 0
        for d in range(2):
            for hc in range(2):
                sq_ = selq[:, (d * 2 + hc) * 128:(d * 2 + hc + 1) * 128]
                for gi, (t0, n, j) in enumerate(groups):
                    pp = pg[i % 2]
                    i += 1
                    P.mm(pp[:, 0:n], sq_, Crow[:, t0:t0 + n], writes=[pp])
                    P.a(lambda e, pp=pp, n=n: e.activation(exb[:, 0:n], pp[:, 0:n], AF.Exp, scale=-1.0), [pp], [exb])
                    P.v(lambda e, d=d, hc=hc, t0=t0, n=n: e.tensor_tensor(qe[d][hc][:, t0:t0 + n], qh[hc][:, t0:t0 + n],
                                                                        exb[:, 0:n], ALU.mult), [qh[hc], exb], [qe[d][hc]])
                pp = pg[i % 2]
                i += 1
                P.mm(pp[:, 0:NT], sq_, Tt[:], writes=[pp])
                P.a(lambda e, pp=pp, d=d, hc=hc: e.activation(eTot[:, (d * 2 + hc) * NT:(d * 2 + hc + 1) * NT], pp[:, 0:NT],
                                                              AF.Exp, scale=-1.0), [pp], [eTot])
    P.barrier()
    if kb.dbg.get("stop2") == "s3":
        return
    cE3 = colE[:].rearrange("p (c r) -> p c r", r=8)
    P.a(lambda e: e.activation(cE3, gc3[:, :, 0:8], AF.Exp, scale=-1.0), [gcol], [colE])
    P.v(lambda e: e.tensor_tensor(colA[:].rearrange("p (c r) -> p c r", r=8), cE3, gc3[:, :, 40:48], ALU.mult),
        [colE, gcol], [colA])
    P.a(lambda e: e.activation(colK[:].rearrange("p (c r) -> p c r", r=8), gc3[:, :, 16:24], AF.Exp, scale=-1.0),
        [gcol], [colK])
    if kb.dbg.get("stop2") == "s4":
        return
    selr = kb.sb(S2, "gd_selr", [16, 8 * 128])
    gmask = kb.sb(S2, "gd_gmask", [128, 4 * 128])
    id4 = kb.sb(S2, "gd_id4", [128, 512], BF16)
    P.dma("sync", selr[:], C["selr"])
    P.dma("sync", gmask[:].rearrange("p (m c) -> p m c", m=4), C["gmask"].rearrange("m p c -> p m c"))
    for h in range(4):
        P.v(lambda e, h=h: e.tensor_copy(id4[:, h * 128:(h + 1) * 128], identb[:]), [identb], [id4])
    u1 = kb.sb(S2, "gd_u1", [128, 512])
    dec = kb.sb(S2, "gd_dec", [128, 512])
    decT = kb.sb(S2, "gd_decT", [128, 512])
    Ab = [kb.sb(S2, f"gd_A{i}", [128, 512], BF16) for i in range(2)]
    ATb = [kb.sb(S2, f"gd_AT{i}", [128, 512], BF16) for i in range(2)]
    TTb = [kb.sb(S2, f"gd_TT{i}", [128, 512], BF16) for i in range(2)]
    attnT = kb.sb(S2, "gd_attnT", [128, 512], BF16)
    smask = kb.sb(S2, "gd_smask", [128, 5 * 128], BF16)
    P.dma("gpsimd", smask[:].rearrange("p (m c) -> p m c", m=5), C["smask"].rearrange("m p c -> p m c"))
    Bm = kb.sb(S2, "gd_Bm", [128, 512], BF16)
    BTm = kb.sb(S2, "gd_BTm", [128, 512], BF16)
    Xa = [kb.sb(S2, f"gd_Xa{i}", [128, 512], BF16) for i in range(3)]
    Xb = [kb.sb(S2, f"gd_Xb{i}", [128, 512], BF16) for i in range(3)]
    Pa = [kb.sb(S2, f"gd_Pa{i}", [128, 512], BF16) for i in range(2)]
    Pb = [kb.sb(S2, f"gd_Pb{i}", [128, 512], BF16) for i in range(2)]
    Wa = kb.sb(S2, "gd_Wa", [128, 512], BF16)
    Wb = kb.sb(S2, "gd_Wb", [128, 512], BF16)
    TTl = [kb.sb(S2, f"gd_TTl{i}", [128, 512], BF16) for i in range(2)]
    Tl = [kb.sb(S2, f"gd_Tl{i}", [128, 512], BF16) for i in range(2)]
    vb_ = kb.sb(S2, "gd_vb", [128, 256], BF16)
    kbe = kb.sb(S2, "gd_kbe", [128, 256], BF16)
    kdec = kb.sb(S2, "gd_kdec", [128, 256], BF16)
    wTn = kb.sb(S2, "gd_wTn", [128, 256], BF16)
    vn = kb.sb(S2, "gd_vn", [128, 256], BF16)
    Sf = kb.sb(S2, "gd_Sf", [128, 256])
    Sb = kb.sb(S2, "gd_Sb", [128, 256], BF16)
    bA = kb.ps(S2, "gd_bA", [128, 512])
    bB = kb.ps(S2, "gd_bB", [128, 512])
    bC = kb.ps(S2, "gd_bC", [128, 512])
    bT = kb.ps(S2, "gd_bT", [128, 1024], BF16)
    bW = kb.ps(S2, "gd_bW", [128, 512])
    bV = kb.ps(S2, "gd_bV", [128, 512])
    bO = kb.ps(S2, "gd_bO", [128, 512])
    bS = kb.ps(S2, "gd_bS", [128, 512])
    H4 = lambda t: t[:].rearrange("p (h x) -> p h x", h=4)
    hp = lambda h: slice((h % 2) * 64, (h % 2) * 64 + 64)
    for d in range(2):
        r0 = 4 * d
        P.g(lambda e: e.memset(Sf[:], 0.0), [], [Sf])
        P.g(lambda e: e.memset(Sb[:], 0.0), [], [Sb])
        for ci, c in enumerate(chunk_order(d)):
            cs = slice(c * 128, (c + 1) * 128)
            for h in range(4):
                P.mm(bA[:, h * 128:(h + 1) * 128], selr[:, (r0 + h) * 128:(r0 + h + 1) * 128], Crow[:, cs], writes=[bA])
            for h in range(4):
                P.v(lambda e, h=h, c=c, d=d, r0=r0: e.scalar_tensor_tensor(
                    u1[:, h * 128:(h + 1) * 128], bA[:, h * 128:(h + 1) * 128], gcol[:, c * 48 + r0 + h: c * 48 + r0 + h + 1],
                    gmask[:, d * 128:(d + 1) * 128], ALU.subtract, ALU.add), [bA, gcol, gmask], [u1])
            P.a(lambda e: e.activation(dec[:], u1[:], AF.Exp), [u1], [dec])
            for h in range(4):
                P.v(lambda e, h=h, c=c, d=d, r0=r0: e.scalar_tensor_tensor(
                    u1[:, h * 128:(h + 1) * 128], bA[:, h * 128:(h + 1) * 128], gcol[:, c * 48 + r0 + h: c * 48 + r0 + h + 1],
                    gmask[:, (2 + d) * 128:(3 + d) * 128], ALU.subtract, ALU.add), [bA, gcol, gmask, dec], [u1])
            P.a(lambda e: e.activation(decT[:], u1[:], AF.Exp, scale=-1.0), [u1], [decT])
            if kb.dbg.get("stop2") == "gdA":
                return
            kkb = (bB, bW)
            qkb = (bC, bV)
            for h in range(4):
                P.mm(kkb[h % 2][:, h * 128:(h + 1) * 128], kh[h // 2][hp(h), cs], kh[h // 2][hp(h), cs], writes=[kkb[h % 2]])
            for h in range(4):
                P.mm(qkb[h % 2][:, h * 128:(h + 1) * 128], kh[h // 2][hp(h), cs], qh[h // 2][hp(h), cs], writes=[qkb[h % 2]])
            A_, AT_ = Ab[0], ATb[0]
            for h in range(4):
                P.v(lambda e, h=h, c=c, r0=r0, A_=A_, kk=kkb[h % 2]: e.scalar_tensor_tensor(
                    A_[:, h * 128:(h + 1) * 128], kk[:, h * 128:(h + 1) * 128],
                    gcol[:, c * 48 + 40 + r0 + h: c * 48 + 40 + r0 + h + 1], dec[:, h * 128:(h + 1) * 128],
                    ALU.mult, ALU.mult), [kkb[h % 2], gcol, dec], [A_])
            for h in range(4):
                P.v(lambda e, h=h, qk=qkb[h % 2]: e.tensor_tensor(attnT[:, h * 128:(h + 1) * 128], qk[:, h * 128:(h + 1) * 128],
                                                                 decT[:, h * 128:(h + 1) * 128], ALU.mult),
                    [qkb[h % 2], decT], [attnT])
            for h in range(4):
                P.tr(bT[:, h * 128:(h + 1) * 128], A_[:, h * 128:(h + 1) * 128], identb[:], writes=[bT])
            P.a(lambda e, AT_=AT_: e.copy(AT_[:], bT[:, 0:512]), [bT], [AT_])
            if kb.dbg.get("stop2") == "gdB":
                return
            H = lambda t: t[:].rearrange("p (h x) -> p h x", h=4)
            mk = lambda m: smask[:, m * 128:(m + 1) * 128].rearrange("p (o x) -> p o x", o=1).to_broadcast([128, 4, 128])
            P.g(lambda e: e.tensor_tensor(H(Bm), H(A_), mk(0), ALU.mult), [A_, smask], [Bm])
            P.v(lambda e: e.tensor_tensor(H(BTm), bT[:, 0:512].rearrange("p (h x) -> p h x", h=4), mk(0), ALU.mult),
                [bT, smask], [BTm])
            P.g(lambda e: e.tensor_tensor(Xa[0][:], id4[:], Bm[:], ALU.subtract), [id4, Bm], [Xa[0]])
            P.v(lambda e: e.tensor_tensor(Xb[0][:], id4[:], BTm[:], ALU.subtract), [id4, BTm], [Xb[0]])

            def mm4(bank, lhs, rhs):
                for h in range(4):
                    hs = slice(h * 128, (h + 1) * 128)
                    P.mm(bank[:, hs], lhs[:, hs], rhs[:, hs], writes=[bank])
            Pw, PwT = Bm, BTm
            X, XT = Xa[0], Xb[0]
            for lv in range(2):
                Pn, PnT = Pa[lv], Pb[lv]
                mm4(bA, PwT, Pw)
                mm4(bB, Pw, PwT)
                P.a(lambda e, Pn=Pn: e.copy(Pn[:], bA[:]), [bA], [Pn])
                P.v(lambda e, PnT=PnT: e.tensor_copy(PnT[:], bB[:]), [bB], [PnT])
                Xn, XnT = Xa[lv + 1], Xb[lv + 1]
                mm4(bC, PnT, X)
                mm4(bA, X, PnT)
                P.v(lambda e, Xn=Xn, X=X: e.tensor_tensor(Xn[:], bC[:], X[:], ALU.add), [bC, X], [Xn])
                P.v(lambda e, XnT=XnT, XT=XT: e.tensor_tensor(XnT[:], bA[:], XT[:], ALU.add), [bA, XT], [XnT])
                Pw, PwT, X, XT = Pn, PnT, Xn, XnT
            Tm_, TT = X, XT
            for li in range(4):
                last_lv = (li == 3)
                mm4(bB, A_, TT)
                P.a(lambda e: e.copy(Wa[:], bB[:]), [bB], [Wa])
                if not last_lv:
                    mm4(bC, AT_, Tm_)
                    P.v(lambda e: e.tensor_copy(Wb[:], bC[:]), [bC], [Wb])
                mm4(bA, Tm_, Wa)
                TTn = TTl[li % 2]
                P.v(lambda e, li=li: e.tensor_tensor(H(Wa), bA[:].rearrange("p (h x) -> p h x", h=4), mk(1 + li), ALU.mult),
                    [bA, smask], [Wa])
                P.g(lambda e, TTn=TTn, TT=TT: e.tensor_tensor(TTn[:], Wa[:], TT[:], ALU.add), [Wa, TT], [TTn])
                if not last_lv:
                    mm4(bB, TT, Wb)
                    Tn = Tl[li % 2]
                    P.v(lambda e, li=li: e.tensor_tensor(H(Wb), bB[:].rearrange("p (h x) -> p h x", h=4), mk(1 + li), ALU.mult),
                        [bB, smask], [Wb])
                    P.g(lambda e, Tn=Tn, Tm_=Tm_: e.tensor_tensor(Tn[:], Wb[:], Tm_[:], ALU.add), [Wb, Tm_], [Tn])
                    Tm_ = Tn
                TT = TTn
            if kb.dbg.get("stop2") == "gdk1":
                P.v(lambda e, TT=TT: e.tensor_copy(TTb[0][:], TT[:]), [TT], [TTb[0]])
                return
            cA = colA[:, c * 8 + r0: c * 8 + r0 + 4].rearrange("p (h o) -> p h o", o=1).to_broadcast([128, 4, 64])
            cB = gcol[:, c * 48 + 40 + r0: c * 48 + 44 + r0].rearrange("p (h o) -> p h o", o=1).to_broadcast([128, 4, 64])
            cK = colK[:, c * 8 + r0: c * 8 + r0 + 4].rearrange("p (h o) -> p h o", o=1).to_broadcast([128, 4, 64])
            k4 = ktok[:, c * 256:(c + 1) * 256].rearrange("p (h x) -> p h x", h=4)
            v4 = vtok[:, c * 256:(c + 1) * 256].rearrange("p (h x) -> p h x", h=4)
            P.g(lambda e, v4=v4, cB=cB: e.tensor_tensor(H4(vb_), v4, cB, ALU.mult), [vtok, gcol], [vb_])
            P.g(lambda e, k4=k4, cA=cA: e.tensor_tensor(H4(kbe), k4, cA, ALU.mult), [ktok, colA], [kbe])
            P.g(lambda e, k4=k4, cK=cK: e.tensor_tensor(H4(kdec), k4, cK, ALU.mult), [ktok, colK], [kdec])
            for h in range(4):
                P.mm(bW[hp(h), (h // 2) * 128:(h // 2 + 1) * 128], kbe[:, h * 64:(h + 1) * 64], TT[:, h * 128:(h + 1) * 128],
                     writes=[bW])
            P.v(lambda e: e.tensor_scalar(wTn[:], bW[:, 0:256], -1.0, None, ALU.mult), [bW], [wTn])
            if kb.dbg.get("stop2") == "gd1":
                return
            for hc in range(2):
                P.mm(bV[:, hc * 128:(hc + 1) * 128], wTn[:, hc * 128:(hc + 1) * 128], Sb[:, hc * 128:(hc + 1) * 128],
                     start=True, stop=False, writes=[bV])
                for hh in range(2):
                    h = hc * 2 + hh
                    P.mm(bV[:, h * 64:(h + 1) * 64], TT[:, h * 128:(h + 1) * 128], vb_[:, h * 64:(h + 1) * 64],
                         start=False, stop=(hh == 1), writes=[bV])
            P.a(lambda e: e.copy(vn[:], bV[:, 0:256]), [bV], [vn])
            for hc in range(2):
                P.mm(bO[:, hc * 128:(hc + 1) * 128], qe[d][hc][:, cs], Sb[:, hc * 128:(hc + 1) * 128],
                     start=True, stop=False, writes=[bO])
                for hh in range(2):
                    h = hc * 2 + hh
                    P.mm(bO[:, h * 64:(h + 1) * 64], attnT[:, h * 128:(h + 1) * 128], vn[:, h * 64:(h + 1) * 64],
                         start=False, stop=(hh == 1), writes=[bO])
            osl = osum[:, c * 256:(c + 1) * 256]
            if d == 0:
                P.a(lambda e, osl=osl: e.copy(osl, bO[:, 0:256]), [bO], [osum])
            else:
                P.v(lambda e, osl=osl: e.tensor_tensor(osl, osl, bO[:, 0:256], ALU.add), [bO, osum], [osum])
            for h in range(4):
                P.mm(bS[hp(h), h * 64:(h + 1) * 64], kdec[:, h * 64:(h + 1) * 64], vn[:, h * 64:(h + 1) * 64], writes=[bS])
            for hh in range(2):
                ps_ = slice(hh * 64, (hh + 1) * 64)
                Sv = Sf[ps_, :].rearrange("p (c x) -> p c x", c=2)[:, :, hh * 64:(hh + 1) * 64]
                Sbv = Sb[ps_, :].rearrange("p (c x) -> p c x", c=2)[:, :, hh * 64:(hh + 1) * 64]
                bSv = bS[ps_, 0:256].rearrange("p (c x) -> p c x", c=2)[:, :, hh * 64:(hh + 1) * 64]
                eTb = eTot[ps_, :].rearrange("p (x c) -> p x c", c=NT)[:, 2 * d:2 * d + 2, c:c + 1].to_broadcast([64, 2, 64])
                P.v(lambda e, Sv=Sv, eTb=eTb: e.tensor_tensor(Sv, Sv, eTb, ALU.mult), [Sf, eTot], [Sf])
                P.v(lambda e, Sv=Sv, bSv=bSv: e.tensor_tensor(Sv, Sv, bSv, ALU.add), [Sf, bS], [Sf])
                P.g(lambda e, Sv=Sv, Sbv=Sbv: e.tensor_copy(Sbv, Sv), [Sf], [Sb])
    if kb.dbg.get("stop2") == "pre":
        return
    finish_norm_gate(kb, S2, osum, ztok, nwb, vtok[:].bitcast(F32), vtok, ktok[:], ktok, bT, YT3, 6, identb)


TWO_PI = 2.0 * math.pi
HYCFG = (("lat", SEQ, 16, 2 * SEQ), ("ctx", CTX, 2, 2 * CTX))


def hyf_phase(kb, S, l, C):
    P = kb.P
    w1 = kb.sb(S, "hf_w1", [33, 64])
    w2 = kb.sb(S, "hf_w2", [64, 64])
    w3 = kb.sb(S, "hf_w3", [64, 64])
    w4 = kb.sb(S, "hf_w4", [64, 512])
    hyp = kb.sb(S, "hf_hyp", [64, 6])
    ones = kb.sb(S, "hf_ones", [128, 128])
    altc = kb.sb(S, "hf_altc", [128, 1], BF16)
    P.dma("sync", w1[:], C["hy_w1"][l])
    P.dma("sync", w2[:], C["hy_w2"][l])
    P.dma("sync", w3[:], C["hy_w3"][l])
    P.dma("sync", w4[:], C["hy_w4"][l])
    P.dma("sync", hyp[:], C["hyp"][l])
    P.g(lambda e: e.memset(ones[:], 1.0), [], [ones])
    P.dma("gpsimd", altc[:], C["altcol"])
    pp2 = [kb.ps(S, f"hf_pp{i}", [128, 512]) for i in range(2)]
    pk2 = [kb.ps(S, f"hf_pk{i}", [128, 512]) for i in range(2)]
    pl1 = kb.ps(S, "hf_pl1", [128, 512])
    pKr = kb.ps(S, "hf_pKr", [128, 512])
    pn = kb.ps(S, "hf_pn", [128, 512])
    def do_cfg(nm, L, nch, N):
        with contextlib.ExitStack() as S1:
            zT = kb.sb(S1, f"hf_zT{nm}", [33, L])
            hA = kb.sb(S1, f"hf_hA{nm}", [64, L])
            hB = kb.sb(S1, f"hf_hB{nm}", [64, L])
            win = kb.sb(S1, f"hf_win{nm}", [128, nch * 256])
            kfw = kb.sb(S1, f"hf_kfw{nm}", [128, nch * 256])
            kbw = kb.sb(S1, f"hf_kbw{nm}", [128, nch * 256])
            ksum = kb.sb(S1, f"hf_ksum{nm}", [128, nch * 256], BF16)
            kdif = kb.sb(S1, f"hf_kdif{nm}", [128, nch * 256], BF16)
            tmpa = kb.sb(S1, f"hf_tmpa{nm}", [128, 512])
            tmpi = kb.sb(S1, f"hf_tmpi{nm}", [128, 512], mybir.dt.int32)
            tmpf = kb.sb(S1, f"hf_tmpf{nm}", [128, 512])
            rl1 = kb.sb(S1, f"hf_rl1{nm}", [128, 256])
            wN = kb.sb(S1, f"hf_wN{nm}", [128, nch])
            spec = [kb.sb(S1, f"hf_spec{nm}{i}", [128, 512]) for i in range(2)]
            ctb = [kb.sb(S1, f"hf_ctb{nm}{i}", [128, nch * 128], BF16) for i in range(2)]
            stb = [kb.sb(S1, f"hf_stb{nm}{i}", [128, nch * 128], BF16) for i in range(2)]
            nyq = kb.sb(S1, f"hf_nyq{nm}", [1, 256])
            P.dma("sync", zT[:], C[f"zT_{nm}"])
            P.dma("sync", win[:].rearrange("p (i c) -> p i c", i=nch), C[f"win_{nm}"].rearrange("(i p) c -> p i c", p=128))
            P.dma("sync", wN[:], C[f"wN_{nm}"])
            grp = [(g0, min(512, L - g0)) for g0 in range(0, L, 512)]
            srcs = [(w1, 33, zT), (w2, 64, hA), (w3, 64, hB)]
            dsts = [hA, hB, hA]
            for li in range(3):
                w_, K_, src_ = srcs[li]
                dst_ = dsts[li]
                for gi, (g0, n) in enumerate(grp):
                    pp = pp2[gi % 2]
                    P.mm(pp[0:64, 0:n], w_[0:K_, :], src_[0:K_, g0:g0 + n], writes=[pp])
                    P.v(lambda e, pp=pp, n=n, li=li: e.tensor_scalar(tmpa[0:64, 0:n], pp[0:64, 0:n], hyp[:, li:li + 1],
                                                                   hyp[:, 3 + li:4 + li], ALU.add, ALU.mult), [pp, hyp], [tmpa])
                    P.v(lambda e, n=n: e.tensor_scalar(tmpa[0:64, 0:n], tmpa[0:64, 0:n], 1.0 / TWO_PI, 16.0,
                                                       ALU.mult, ALU.add), [tmpa], [tmpa])
                    P.v(lambda e, n=n: e.tensor_copy(tmpi[0:64, 0:n], tmpa[0:64, 0:n]), [tmpa], [tmpi])
                    P.v(lambda e, n=n: e.tensor_copy(tmpf[0:64, 0:n], tmpi[0:64, 0:n]), [tmpi], [tmpf])
                    P.v(lambda e, n=n: e.tensor_tensor(tmpa[0:64, 0:n], tmpa[0:64, 0:n], tmpf[0:64, 0:n], ALU.subtract),
                        [tmpa, tmpf], [tmpa])
                    P.v(lambda e, n=n: e.tensor_scalar(tmpf[0:64, 0:n], tmpa[0:64, 0:n], 0.5, None, ALU.is_gt), [tmpa], [tmpf])
                    P.v(lambda e, n=n: e.tensor_tensor(tmpa[0:64, 0:n], tmpa[0:64, 0:n], tmpf[0:64, 0:n], ALU.subtract),
                        [tmpa, tmpf], [tmpa])
                    P.a(lambda e, n=n, dst_=dst_, g0=g0: e.activation(dst_[:, g0:g0 + n], tmpa[0:64, 0:n], AF.Sin, scale=TWO_PI),
                        [tmpa], [dst_])
            h3 = hA
            for i in range(nch):
                pk = pk2[i % 2]
                P.mm(pk[:, :], h3[:, i * 128:(i + 1) * 128], w4[:, :], writes=[pk])
                P.v(lambda e, pk=pk, i=i: e.tensor_tensor(kfw[:, i * 256:(i + 1) * 256], pk[:, 0:256], win[:, i * 256:(i + 1) * 256],
                                                          ALU.mult), [pk, win], [kfw])
                P.v(lambda e, pk=pk, i=i: e.tensor_tensor(kbw[:, i * 256:(i + 1) * 256], pk[:, 256:512], win[:, i * 256:(i + 1) * 256],
                                                          ALU.mult), [pk, win], [kbw])
            P.g(lambda e: e.memset(kbw[0:1, 0:256], 0.0), [kbw], [kbw])
            for i in range(nch):
                P.a(lambda e, i=i: e.activation(tmpa[:, 0:256], kfw[:, i * 256:(i + 1) * 256], AF.Abs), [kfw], [tmpa])
                P.a(lambda e, i=i: e.activation(tmpa[:, 256:512], kbw[:, i * 256:(i + 1) * 256], AF.Abs), [kbw, tmpa], [tmpa])
                P.g(lambda e: e.tensor_tensor(tmpa[:, 0:256], tmpa[:, 0:256], tmpa[:, 256:512], ALU.add), [tmpa], [tmpa])
                P.mm(pl1[:, 0:256], ones[:, :], tmpa[:, 0:256], start=(i == 0), stop=(i == nch - 1), writes=[pl1])
            P.v(lambda e: e.reciprocal(rl1[:], pl1[:, 0:256]), [pl1], [rl1])
            rbc = rl1[:].rearrange("p (o c) -> p o c", o=1).to_broadcast([128, nch, 256])
            k3 = lambda t: t[:].rearrange("p (i c) -> p i c", i=nch)
            P.v(lambda e: e.tensor_tensor(k3(ksum), k3(kfw), k3(kbw), ALU.add), [kfw, kbw], [ksum])
            P.g(lambda e: e.tensor_tensor(k3(kdif), k3(kbw), k3(kfw), ALU.subtract), [kfw, kbw], [kdif])
            P.v(lambda e: e.tensor_tensor(k3(ksum), k3(ksum), rbc, ALU.mult), [ksum, rl1], [ksum])
            P.g(lambda e: e.tensor_tensor(k3(kdif), k3(kdif), rbc, ALU.mult), [kdif, rl1], [kdif])
            ctab, stab = C[f"ctab_{nm}"], C[f"stab_{nm}"]
            for j in range(nch):
                cb_, sb_ = ctb[j % 2], stb[j % 2]
                P.dma("sync", cb_[:].rearrange("p (i f) -> p i f", i=nch),
                      ctab[:, j * 128:(j + 1) * 128].rearrange("(i p) f -> p i f", p=128), writes=[cb_])
                P.dma("sync", sb_[:].rearrange("p (i f) -> p i f", i=nch),
                      stab[:, j * 128:(j + 1) * 128].rearrange("(i p) f -> p i f", p=128), writes=[sb_])
                for i in range(nch):
                    P.mm(pKr[:, 0:256], cb_[:, i * 128:(i + 1) * 128], ksum[:, i * 256:(i + 1) * 256],
                         start=(i == 0), stop=(i == nch - 1), writes=[pKr])
                for i in range(nch):
                    P.mm(pKr[:, 256:512], sb_[:, i * 128:(i + 1) * 128], kdif[:, i * 256:(i + 1) * 256],
                         start=(i == 0), stop=(i == nch - 1), skip_group_check=True, writes=[pKr])
                sp = spec[j % 2]
                P.v(lambda e, sp=sp, j=j: e.tensor_scalar(sp[:], pKr[:], wN[:, j:j + 1], None, ALU.mult), [pKr, wN], [sp])
                P.dma("sync", kb.KSPEC[nm][j], sp[:], writes=[("KSPEC", nm)])
            for i in range(nch):
                P.mm(pn[0:1, 0:256], altc[:, 0:1], ksum[:, i * 256:(i + 1) * 256], start=(i == 0), stop=(i == nch - 1),
                     writes=[pn])
            P.v(lambda e, N=N: e.tensor_scalar(nyq[:], pn[0:1, 0:256], 1.0 / N, None, ALU.mult), [pn], [nyq])
            P.dma("sync", kb.KNYQ[nm], nyq[:], writes=[("KSPEC", nm)])
        P.barrier()

    for cfg in HYCFG:
        do_cfg(*cfg)


def hyena_mixer(kb, S2, l, b, xmT3, YT23, groups, w_in, C, small, identb, conv3, yt_flush):
    P = kb.P
    pbuf = kb.sb(S2, "hy_pbuf", [128, LS])
    x0c = kb.sb(S2, "hy_x0c", [128, LS])
    x1c = kb.sb(S2, "hy_x1c", [128, LS])
    zc = kb.sb(S2, "hy_zc", [128, LS])
    zb = kb.sb(S2, "hy_zb", [128, LS], BF16)
    Zt = kb.sb(S2, "hy_Zt", [128, NT * 128], BF16)
    Yc = kb.sb(S2, "hy_Yc", [128, 16 * 128], BF16)
    Ys = kb.sb(S2, "hy_Ys", [128, 16 * 128], BF16)
    Yn = kb.sb(S2, "hy_Yn", [1, 128], BF16)
    knq = kb.sb(S2, "hy_knq", [1, 256])
    t1 = kb.sb(S2, "hy_t1", [128, 128])
    t2 = kb.sb(S2, "hy_t2", [128, 128])
    t3 = kb.sb(S2, "hy_t3", [128, 512])
    altc = kb.sb(S2, "hy_altc", [128, 1], BF16)
    altr = kb.sb(S2, "hy_altr", [1, SEQ], BF16)
    spec = [kb.sb(S2, f"hy_spec{i}", [128, 512]) for i in range(2)]
    tbA = [kb.sb(S2, f"hy_tbA{i}", [128, 2048], BF16) for i in range(2)]
    tbB = [kb.sb(S2, f"hy_tbB{i}", [128, 2048], BF16) for i in range(2)]
    wsl = kb.sb(S2, "hy_wsl", [128, 8 * 768], BF16)
    P.dma("gpsimd", wsl[:].rearrange("p (k c) -> p k c", k=8),
          w_in[l, :, HY0:HY0 + 768].rearrange("(k p) c -> p k c", p=128), writes=[wsl])
    P.dma("gpsimd", altc[:], C["altcol"])
    P.dma("gpsimd", altr[:], C["altrow"])
    pp2 = [kb.ps(S2, f"hy_pp{i}", [128, 512]) for i in range(2)]
    pT = kb.ps(S2, "hy_pT", [128, 1024], BF16)
    pF = kb.ps(S2, "hy_pF", [128, 512])
    pI = [kb.ps(S2, f"hy_pI{i}", [128, 512]) for i in range(4)]
    ii = 0
    for hf in range(2):
        dsts = {0: x0c, 2: x1c, 4: zc}
        for cb in (0, 2, 4):
            ch = cb + hf
            for gi, (t0, n, j) in enumerate(groups):
                pp = pp2[ii % 2]
                ii += 1
                for k in range(8):
                    P.mm(pp[:, 0:n], wsl[:, k * 768 + ch * 128: k * 768 + (ch + 1) * 128], xmT3[:, k, t0:t0 + n],
                         start=(k == 0), stop=(k == 7), reads=[wsl, ("xmT", gi)], writes=[pp])
                P.a(lambda e, pp=pp, t0=t0, n=n: e.copy(pbuf[:, t0:t0 + n], pp[:, 0:n]), [pp], [pbuf])
            wc = tuple(small[:, 3 * ch + k: 3 * ch + k + 1] for k in range(3))
            conv3(dsts[cb], pbuf, wc, bias=small[:, 18 + ch: 19 + ch])
        P.v(lambda e: e.tensor_tensor(zc[:], zc[:], x1c[:], ALU.mult), [zc, x1c], [zc])
        P.g(lambda e: e.tensor_copy(zb[:], zc[:]), [zc], [zb])
        for c4 in range(0, NT, 8):
            nn = min(8, NT - c4)
            for cc in range(nn):
                P.tr(pT[:, cc * 128:(cc + 1) * 128], zb[:, (c4 + cc) * 128:(c4 + cc + 1) * 128], identb[:], writes=[pT])
            P.v(lambda e, c4=c4, nn=nn: e.tensor_copy(Zt[:, c4 * 128:(c4 + nn) * 128], pT[:, 0:nn * 128]), [pT], [Zt])
        for (nm, L, nch, N) in HYCFG:
            tile0 = 2 if nm == "lat" else 0
            tok0 = CTX if nm == "lat" else 0
            ctab, stab = C[f"ctab_{nm}"], C[f"stab_{nm}"]
            P.dma("sync", knq[:], kb.KNYQ[nm], reads=[("KSPEC", nm)])
            for j in range(nch):
                ca, sa, sp = tbA[j % 2], tbB[j % 2], spec[j % 2]
                P.dma("sync", ca[:, 0:nch * 128].rearrange("p (i f) -> p i f", i=nch),
                      ctab[:, j * 128:(j + 1) * 128].rearrange("(i p) f -> p i f", p=128), writes=[ca])
                P.dma("sync", sa[:, 0:nch * 128].rearrange("p (i f) -> p i f", i=nch),
                      stab[:, j * 128:(j + 1) * 128].rearrange("(i p) f -> p i f", p=128), writes=[sa])
                P.dma("sync", sp[:], kb.KSPEC[nm][j], reads=[("KSPEC", nm)], writes=[sp])
                for i in range(nch):
                    P.mm(pF[:, 0:128], ca[:, i * 128:(i + 1) * 128], Zt[:, (tile0 + i) * 128:(tile0 + i + 1) * 128],
                         start=(i == 0), stop=(i == nch - 1), writes=[pF])
                for i in range(nch):
                    P.mm(pF[:, 128:256], sa[:, i * 128:(i + 1) * 128], Zt[:, (tile0 + i) * 128:(tile0 + i + 1) * 128],
                         start=(i == 0), stop=(i == nch - 1), skip_group_check=True, writes=[pF])
                kr = sp[:, hf * 128:(hf + 1) * 128]
                ki = sp[:, 256 + hf * 128: 256 + (hf + 1) * 128]
                ycj = Yc[:, j * 128:(j + 1) * 128]
                ysj = Ys[:, j * 128:(j + 1) * 128]
                P.v(lambda e, kr=kr: e.tensor_tensor(t1[:], pF[:, 0:128], kr, ALU.mult), [pF, sp], [t1])
                P.v(lambda e, ki=ki: e.tensor_tensor(t2[:], pF[:, 128:256], ki, ALU.mult), [pF, sp], [t2])
                P.g(lambda e, ycj=ycj: e.tensor_tensor(ycj, t1[:], t2[:], ALU.add), [t1, t2], [Yc])
                P.v(lambda e, kr=kr: e.tensor_tensor(t1[:], pF[:, 128:256], kr, ALU.mult), [pF, sp, Yc], [t1])
                P.v(lambda e, ki=ki: e.tensor_tensor(t2[:], pF[:, 0:128], ki, ALU.mult), [pF, sp, Yc], [t2])
                P.g(lambda e, ysj=ysj: e.tensor_tensor(ysj, t1[:], t2[:], ALU.subtract), [t1, t2], [Ys])
            for i in range(nch):
                P.mm(pF[0:1, 256:384], altc[:, 0:1], Zt[:, (tile0 + i) * 128:(tile0 + i + 1) * 128],
                     start=(i == 0), stop=(i == nch - 1), skip_group_check=True, writes=[pF])
            P.v(lambda e, hf=hf: e.tensor_tensor(Yn[:], pF[0:1, 256:384], knq[:, hf * 128:(hf + 1) * 128], ALU.mult), [pF, knq], [Yn])
            tgs = [(g0, min(512, L - g0)) for g0 in range(0, L, 512)]
            for j in range(nch):
                ca, sa = tbA[j % 2], tbB[j % 2]
                P.dma("sync", ca[:, 0:L], ctab[j * 128:(j + 1) * 128, :], writes=[ca])
                P.dma("sync", sa[:, 0:L], stab[j * 128:(j + 1) * 128, :], writes=[sa])
                for gi, (g0, n) in enumerate(tgs):
                    P.mm(pI[gi][:, 0:n], Yc[:, j * 128:(j + 1) * 128], ca[:, g0:g0 + n], start=(j == 0), stop=False,
                         writes=[pI[gi]])
                    P.mm(pI[gi][:, 0:n], Ys[:, j * 128:(j + 1) * 128], sa[:, g0:g0 + n], start=False, stop=False,
                         writes=[pI[gi]])
            for gi, (g0, n) in enumerate(tgs):
                P.mm(pI[gi][:, 0:n], Yn[:, :], altr[:, g0:g0 + n], start=False, stop=True, writes=[pI[gi]])
                a0 = tok0 + g0
                P.v(lambda e, gi=gi, n=n, a0=a0, hf=hf: e.scalar_tensor_tensor(t3[:, 0:n], zc[:, a0:a0 + n], small[:, 24 + hf:25 + hf],
                                                                       pI[gi][:, 0:n], ALU.mult, ALU.add),
                    [zc, small, pI[gi]], [t3])
                P.v(lambda e, n=n, a0=a0, hf=hf: e.tensor_tensor(YT23[:, hf, a0:a0 + n], t3[:, 0:n], x0c[:, a0:a0 + n], ALU.mult),
                    [t3, x0c], [("YT2", hf)])
    yt_flush(0)

TB = 1152
NTB = TB // 128


def moe_phase(kb, S, l, last, XR, out, lnp, w_router, b_router, w_gate, w_up, w_down, bguT, b_down,
              ident, sel, ADA, acol, a1col):
    nc, P = kb.nc, kb.P
    hT = kb.sb(S, "hT", [128, 8 * TB], BF16)
    hT3 = hT[:].rearrange("p (k t) -> p k t", k=8)
    acc = kb.sb(S, "acc", [128, NTB * D])
    cw = kb.sb(S, "cw", [128, NTB * NE])
    cws = kb.sb(S, "cws", [128, NTB * NE])
    cwT = kb.sb(S, "cwT", [NE, TB])
    wr = kb.sb(S, "wr", [128, 8 * NE])
    brb = kb.sb(S, "brb", [128, NE])
    bdn = kb.sb(S, "bdn", [NE, D])
    bgu = kb.sb(S, "bgu", [128, 2 * NE * 8])
    gbc = kb.sb(S, "ln2g", [128, D])
    bbc = kb.sb(S, "ln2b", [128, D])
    g2 = {j: kb.sb(S, f"g2bc{j}", [128, D]) for j in range(2)}
    g2[2] = g2[1]
    wts = [[kb.sb(S, f"w{nm}{i}", [128, 8 * D], BF16) for nm in ("g", "u", "d")] for i in range(2)]
    actT = [kb.sb(S, f"actT{i}", [128, 8 * 512], BF16) for i in range(1)]
    hT32 = [kb.sb(S, f"hT32_{i}", [128, 8 * 128]) for i in range(1)]
    xts = [kb.sb(S, f"mx{i}", [128, D]) for i in range(1)]
    tg_ = [kb.sb(S, f"m_g{i}", [128, 512]) for i in range(1)] * 2
    ts_ = [kb.sb(S, f"m_s{i}", [128, 512]) for i in range(1)] * 2
    tu_ = [kb.sb(S, f"m_u{i}", [128, 512]) for i in range(1)] * 2
    lg = kb.sb(S, "m_lg", [128, NE])
    m8 = kb.sb(S, "m_m8", [128, 8])
    nmx = kb.sb(S, "m_nmx", [128, 1])
    msk = kb.sb(S, "m_msk", [128, NE])
    ssum = kb.sb(S, "m_ssum", [128, 1])
    stats = kb.sb(S, "m_stats", [128, 12])
    mv = kb.sb(S, "m_mv", [128, 2])
    rstd = kb.sb(S, "m_rstd", [128, 1])
    pg = [kb.ps(S, f"m_pg{i}", [128, 512]) for i in range(2)]
    pu = [kb.ps(S, f"m_pu{i}", [128, 512]) for i in range(2)]
    pd = [kb.ps(S, f"m_pd{i}", [128, 512]) for i in range(2)]
    pm = [kb.ps(S, f"m_pm{i}", [128, 512]) for i in range(2)]

    P.dma("sync", wr[:].rearrange("p (k e) -> p k e", k=8), w_router[l].rearrange("(k p) e -> p k e", p=128))
    P.dma("sync", brb[:], b_router[l:l + 1, :].to_broadcast([128, NE]))
    P.dma("sync", bdn[:], b_down[l])
    P.dma("sync", bgu[:], bguT[l].rearrange("p a e k -> p (a e k)"))
    P.dma("sync", gbc[:], lnp[l, 2:3, :].to_broadcast([128, D]))
    P.dma("sync", bbc[:], lnp[l, 3:4, :].to_broadcast([128, D]))

    nblk = TOK // TB
    wcount = [0]

    def load_w(e, buf):
        for nm, wsrc in zip(range(3), (w_gate, w_up, w_down)):
            wt = wts[buf][nm]
            for hh in range(2):
                P.dma("gpsimd", wt[:, hh * 4 * D:(hh + 1) * 4 * D].rearrange("p (k c) -> p k c", k=4),
                      wsrc[l, e, hh * 512:(hh + 1) * 512, :].rearrange("(k p) c -> p k c", p=128),
                      writes=[wt])

    for blk in range(nblk):
        bt0 = blk * TB
        bidx = bt0 // LS
        gate_bcast(kb, g2[0], l, bidx, 5 * 1024, ADA)
        if blk % 2 == 0:
            gate_bcast(kb, g2[1], l, 2, 5 * 1024, ADA)
        if kb.moe:
            load_w(0, 0)
        for ti in range(NTB):
            r0 = bt0 + ti * 128
            sidx = r0 - bidx * LS
            j = 2 if sidx < CTX else bidx
            xt = xts[0]
            h32 = hT32[0]
            P.dma("sync", xt[:], XR[r0:r0 + 128, :], reads=[("XR", r0 // 128)])
            for half in range(2):
                pp = pm[half]
                for kk in range(4):
                    k = half * 4 + kk
                    P.tr(pp[:, kk * 128:(kk + 1) * 128], xt[:, k * 128:(k + 1) * 128], ident[:], writes=[pp])
                for kk in range(4):
                    k = half * 4 + kk
                    P.v(lambda e, pp=pp, kk=kk, k=k, j=j, h32=h32: e.tensor_scalar(
                        h32[:, k * 128:(k + 1) * 128], pp[:, kk * 128:(kk + 1) * 128],
                        a1col(j, 32 + k), acol(j, 24 + k), ALU.mult, ALU.add),
                        [pp, "adaT", "ada1T"], [h32])
            P.g(lambda e, h32=h32, ti=ti: e.tensor_copy(
                hT3[:, :, ti * 128:(ti + 1) * 128], h32[:].rearrange("p (k t) -> p k t", k=8)),
                [h32], [("hT", ti)])
            if kb.dbg.get("stop") == "m1":
                continue
            pl = pd[ti % 2]
            for k in range(8):
                P.mm(pl[:, 0:NE], h32[:, k * 128:(k + 1) * 128], wr[:, k * NE:(k + 1) * NE],
                     start=(k == 0), stop=(k == 7), writes=[pl])
            P.v(lambda e, pl=pl: e.tensor_tensor(lg[:], pl[:, 0:NE], brb[:], ALU.add), [pl, brb], [lg])
            if kb.dbg.get("stop") == "m2":
                continue
            P.v(lambda e: e.max(out=m8[:], in_=lg[:]), [lg], [m8])
            P.v(lambda e: e.tensor_scalar(msk[:], lg[:], m8[:, 3:4], None, ALU.is_ge), [lg, m8], [msk])
            P.v(lambda e: e.tensor_scalar(nmx[:], m8[:, 0:1], -1.0, None, ALU.mult), [m8], [nmx])
            P.a(lambda e: e.activation(lg[:], lg[:], AF.Exp, bias=nmx[:, 0:1]), [lg, nmx], [lg])
            P.v(lambda e: e.tensor_tensor(lg[:], lg[:], msk[:], ALU.mult), [lg, msk], [lg])
            P.v(lambda e: e.reduce_sum(ssum[:], lg[:], AX.X), [lg], [ssum])
            P.v(lambda e: e.reciprocal(ssum[:], ssum[:]), [ssum], [ssum])
            cwt = cw[:, ti * NE:(ti + 1) * NE]
            P.v(lambda e, cwt=cwt: e.tensor_scalar(cwt, lg[:], ssum[:, 0:1], None, ALU.mult),
                [lg, ssum], [("cw", ti)])
            if kb.dbg.get("stop") == "m3":
                continue
            P.g(lambda e, cwt=cwt, ti=ti: e.tensor_scalar(cws[:, ti * NE:(ti + 1) * NE], cwt, 1.0 / 1.702, None, ALU.mult),
                [("cw", ti)], [("cws", ti)])
            pt = pu[ti % 2]
            P.tr(pt[0:NE, 0:128], cwt, ident[:], reads=[("cw", ti), ident], writes=[pt])
            P.v(lambda e, pt=pt, ti=ti: e.tensor_copy(cwT[:, ti * 128:(ti + 1) * 128], pt[0:NE, 0:128]),
                [pt], [("cwT", ti)])
            for h in range(2):
                pp = pg[h]
                P.mm(pp[:, :], cwT[:, ti * 128:(ti + 1) * 128], bdn[:, h * 512:(h + 1) * 512],
                     reads=[("cwT", ti), bdn], writes=[pp])
                P.a(lambda e, pp=pp, ti=ti, h=h: e.copy(acc[:, ti * D + h * 512: ti * D + (h + 1) * 512], pp[:, :]),
                    [pp], [("acc", ti)])
        if kb.dbg.get("stop") in ("m1", "m2", "m3", "m4"):
            continue
        if "cw" in kb.dbg and blk == 0:
            dd = kb.dout(f"dbg_cw{l}", [128, NTB * NE])
            P.dma("sync", dd, cw[:], reads=[("cw", t) for t in range(NTB)])
        hkeys = [("hT", t) for t in range(NTB)]
        if kb.moe:
            for e_ in range(NE):
                buf = e_ % 2
                if e_ + 1 < NE:
                    load_w(e_ + 1, 1 - buf)
                wg, wu, wd = wts[buf]
                ci = 0
                for (g0, gn) in ((0, 512), (512, 512), (1024, 128)):
                    at = actT[0]
                    ci += 1
                    for fc in range(8):
                        p_g, p_u = pg[fc % 2], pu[fc % 2]
                        for k in range(8):
                            P.mm(p_g[:, 0:gn], wg[:, k * D + fc * 128: k * D + (fc + 1) * 128],
                                 hT3[:, k, g0:g0 + gn], start=(k == 0), stop=(k == 7),
                                 reads=[wg] + hkeys, writes=[p_g])
                        for k in range(8):
                            P.mm(p_u[:, 0:gn], wu[:, k * D + fc * 128: k * D + (fc + 1) * 128],
                                 hT3[:, k, g0:g0 + gn], start=(k == 0), stop=(k == 7),
                                 reads=[wu] + hkeys, writes=[p_u])
                        tg, tsg, tu = tg_[fc % 2], ts_[fc % 2], tu_[fc % 2]
                        bgc = bgu[:, (0 * NE + e_) * 8 + fc:(0 * NE + e_) * 8 + fc + 1]
                        buc = bgu[:, (1 * NE + e_) * 8 + fc:(1 * NE + e_) * 8 + fc + 1]
                        P.v(lambda e, tg=tg, p_g=p_g, gn=gn, bgc=bgc: e.tensor_scalar(
                            tg[:, 0:gn], p_g[:, 0:gn], bgc, 7.0, ALU.add, ALU.min), [p_g, bgu], [tg])
                        P.a(lambda e, tg=tg, tsg=tsg, gn=gn: e.activation(
                            tsg[:, 0:gn], tg[:, 0:gn], AF.Silu, scale=1.702), [tg], [tsg])
                        P.v(lambda e, tu=tu, p_u=p_u, gn=gn, buc=buc: e.tensor_scalar(
                            tu[:, 0:gn], p_u[:, 0:gn], buc, 7.0, ALU.add, ALU.min), [p_u, bgu], [tu])
                        P.v(lambda e, tu=tu, gn=gn: e.tensor_scalar(
                            tu[:, 0:gn], tu[:, 0:gn], -7.0, 1.0, ALU.max, ALU.add), [tu], [tu])
                        P.g(lambda e, tsg=tsg, tu=tu, gn=gn, at=at, fc=fc: e.tensor_tensor(
                            at[:, fc * 512: fc * 512 + gn], tsg[:, 0:gn], tu[:, 0:gn], ALU.mult),
                            [tsg, tu], [at])
                    for tt in range(gn // 128):
                        ti = (g0 + tt * 128) // 128
                        for h in range(2):
                            pp = pd[h]
                            for fc in range(8):
                                P.mm(pp[:, :], at[:, fc * 512 + tt * 128: fc * 512 + (tt + 1) * 128],
                                     wd[:, fc * D + h * 512: fc * D + (h + 1) * 512],
                                     start=(fc == 0), stop=(fc == 7), reads=[at, wd], writes=[pp])
                            asl = acc[:, ti * D + h * 512: ti * D + (h + 1) * 512]
                            P.v(lambda e, pp=pp, asl=asl, ti=ti, e_=e_: e.scalar_tensor_tensor(
                                asl, pp[:, :], cws[:, ti * NE + e_: ti * NE + e_ + 1], asl, ALU.mult, ALU.add),
                                [pp, ("cws", ti), ("acc", ti)], [("acc", ti)])
        for ti in range(NTB):
            r0 = bt0 + ti * 128
            sidx = r0 - bidx * LS
            j = 2 if sidx < CTX else bidx
            xt = xts[0]
            at = acc[:, ti * D:(ti + 1) * D]
            P.dma("sync", xt[:], XR[r0:r0 + 128, :], reads=[("XR", r0 // 128)])
            P.g(lambda e, at=at, j=j: e.tensor_tensor(at, at, g2[0 if j < 2 else 1][:], ALU.mult), [("acc", ti), g2[0 if j < 2 else 1]], [("acc", ti)])
            P.v(lambda e, at=at, xt=xt: e.scalar_tensor_tensor(at, xt[:], float(DN_ALPHA), at, ALU.mult, ALU.add),
                [xt, ("acc", ti)], [("acc", ti)])

            class _T:
                def __init__(s, ap): s.ap = ap
                def __getitem__(s, idx): return s.ap[idx]
            ln_tile(kb, None, _T(at), gbc[:], bbc[:], _T(at), stats, mv, rstd, ("acc", ti), ("acc", ti))
            if kb.dbg.get("stop") == "m5":
                continue
            if last:
                if sidx >= CTX:
                    o0 = bidx * SEQ + sidx - CTX
                    P.dma("sync", out[o0:o0 + 128, :], at, reads=[("acc", ti)], writes=["out"])
            else:
                P.dma("sync", XR[r0:r0 + 128, :], at, reads=[("acc", ti)], writes=[("XR", r0 // 128)])


def _hy_consts(nm, L_, nch, N_):
    o = {}
    t = np.linspace(0.0, 1.0, L_, dtype=np.float32)[:, None]
    ang = (2.0 * math.pi * np.arange(L_, dtype=np.float32)[:, None] / L_).astype(np.float32)
    fb = np.linspace(1e-4, 15, 16, dtype=np.float32)[None, :]
    z = np.concatenate([t, np.cos(fb * ang), -np.sin(fb * ang)], axis=-1).astype(np.float32)
    o[f"zT_{nm}"] = np.ascontiguousarray(z.T)
    max_decay = math.log(1e-2) / 0.3
    min_decay = math.log(1e-2) / 1.5
    deltas = np.abs(np.linspace(min_decay, max_decay, 256, dtype=np.float32))
    o[f"win_{nm}"] = np.exp(-t * deltas[None, :]).astype(np.float32)
    tt = np.arange(L_, dtype=np.int64)
    ph = (np.outer(tt, tt) % N_).astype(np.float64) * (2.0 * math.pi / N_)
    o[f"ctab_{nm}"] = np.cos(ph).astype(ml_dtypes.bfloat16)
    o[f"stab_{nm}"] = np.sin(ph).astype(ml_dtypes.bfloat16)
    f_ = np.arange(L_).reshape(nch, 128).T
    o[f"wN_{nm}"] = np.where(f_ == 0, 1.0 / N_, 2.0 / N_).astype(np.float32)
    return o


def _prep_shared(inp):
    f = lambda a: np.ascontiguousarray(np.asarray(a, dtype=np.float32))
    sh = {}
    for k in ("w_ada", "b_ada", "w_in", "w_out", "w_router", "b_router", "w_gate", "w_up", "w_down", "b_down"):
        sh[k] = f(inp[k])
    sh["lnp"] = f(np.stack([inp["ln1_g"], inp["ln1_b"], inp["ln2_g"], inp["ln2_b"]], axis=1))
    bg = np.asarray(inp["b_gate"], np.float32).reshape(DEPTH, NE, 8, 128)
    bu = np.asarray(inp["b_up"], np.float32).reshape(DEPTH, NE, 8, 128)
    sh["bguT"] = f(np.stack([bg, bu], axis=1).transpose(0, 4, 1, 2, 3))
    sm = np.zeros((DEPTH, 128, 64), np.float32)

    def colT(a, nch):
        n = a.shape[1]
        return a.reshape(DEPTH, n, nch, 128).transpose(0, 3, 2, 1).reshape(DEPTH, 128, nch * n)
    sm[:, :, 0:18] = colT(np.asarray(inp["hy_conv_w"], np.float32), 6)
    sm[:, :, 18:24] = colT(np.asarray(inp["hy_conv_b"], np.float32)[:, None, :], 6)
    sm[:, :, 24:26] = colT(np.asarray(inp["hy_d"], np.float32)[:, None, :], 2)
    sm[:, :, 26:32] = colT(np.asarray(inp["sc_conv_w"], np.float32), 2)
    sm[:, :, 32:50] = colT(np.asarray(inp["gdn_conv_w"], np.float32), 6)
    sm[:, :, 50:52] = np.asarray(inp["gla_b_a"], np.float32).transpose(0, 2, 1)
    sh["smallT"] = sm
    sh["gla_w_a"] = f(inp["gla_w_a"])
    sh["gla_norm_w"] = f(inp["gla_norm_w"])
    jj = np.arange(128)[:, None]
    ii = np.arange(128)[None, :]
    tm = np.stack([np.tile((jj <= ii), (1, 4)), np.tile((jj >= ii), (1, 4))]).astype(np.float32)
    sh["trimask"] = tm
    sh["gdn_norm_w"] = f(inp["gdn_norm_w"])
    sm[:, 0:8, 52] = np.asarray(inp["gdn_dt_bias"], np.float32).reshape(DEPTH, 8)
    sm[:, 0:8, 53] = np.asarray(inp["gdn_a_log"], np.float32).reshape(DEPTH, 8)
    sh["bones"] = (np.arange(128)[:, None] // 64 == np.arange(128)[None, :] // 64).astype(np.float32)
    gs = np.zeros((16, 2), np.float32); gs[0:4, 0] = 1; gs[4:8, 1] = 1
    sh["gsel"] = gs
    sq_ = np.zeros((16, 4, 128), np.float32)
    for d_ in range(2):
        for hc in range(2):
            for m_ in range(128):
                sq_[4 * d_ + 2 * hc + m_ // 64, d_ * 2 + hc, m_] = 1
    sh["selq"] = sq_.reshape(16, 512)
    sr = np.zeros((16, 8, 128), np.float32)
    for r_ in range(8):
        sr[r_, r_, :] = 1
    sh["selr"] = sr.reshape(16, 1024)
    pp_ = np.arange(128)[:, None]; ff_ = np.arange(128)[None, :]
    BIG = 1.0e4
    sh["gmask"] = np.stack([np.where(ff_ < pp_, 0, -BIG), np.where(ff_ > pp_, 0, -BIG),
                            np.where(pp_ <= ff_, 0, BIG), np.where(pp_ >= ff_, 0, BIG)]).astype(np.float32)
    sms = [(pp_ // 8 == ff_ // 8).astype(np.float32)]
    for s_ in (8, 16, 32, 64):
        sms.append(-((pp_ // (2 * s_) == ff_ // (2 * s_)) & (pp_ // s_ != ff_ // s_)).astype(np.float32))
    sh["smask"] = np.stack(sms)
    for k_ in ("hy_w1", "hy_w2", "hy_w3", "hy_w4"):
        sh[k_] = f(inp[k_])
    sh["hyp"] = f(np.stack([inp["hy_b1"], inp["hy_b2"], inp["hy_b3"], inp["hy_freq"][:, 0], inp["hy_freq"][:, 1],
                            inp["hy_freq"][:, 2]], axis=-1))
    sh["altcol"] = ((-1.0) ** np.arange(128)).astype(np.float32)[:, None]
    sh["altrow"] = ((-1.0) ** np.arange(SEQ)).astype(np.float32)[None, :]
    for (nm, L_, nch, N_) in HYCFG:
        sh.update(_hy_consts(nm, L_, nch, N_))
    sh["hsel"] = (np.arange(128)[:, None] // 32 == np.arange(4)[None, :]).astype(np.float32)
    sh["bdmask"] = (np.arange(128)[:, None] // 32 == np.arange(256)[None, :] // 64).astype(np.float32)
    sh["ident"] = np.eye(128, dtype=np.float32)
    s3 = np.zeros((3, 3, 128), np.float32)
    for j in range(3):
        s3[j, j, :] = 1.0
    sh["sel3"] = s3
    return sh


def _prep_core(inp, i):
    b0 = i * NB
    xs = []
    for b in range(b0, b0 + NB):
        xs.append(np.asarray(inp["ctx"][b], np.float32))
        xs.append(np.asarray(inp["x"][b], np.float32))
    xin = np.ascontiguousarray(np.concatenate(xs, axis=0))
    c3 = np.stack([np.asarray(inp["c"][b0], np.float32), np.asarray(inp["c"][b0 + 1], np.float32),
                   np.asarray(inp["c_ctx"], np.float32)], axis=0)
    c3T = np.ascontiguousarray(c3.reshape(3, 8, 128).transpose(2, 1, 0))
    return {"xin": xin, "c3T": c3T}


def kernel(**inputs):
    n = 8
    kb = build()
    sh = _prep_shared(inputs)
    in_maps = []
    for i in range(n):
        m = dict(sh)
        m.update(_prep_core(inputs, i))
        in_maps.append(m)
    res = run_bass_kernel_spmd(kb.nc, in_maps, core_ids=list(range(n)))
    outs = [np.asarray(r["out"], dtype=np.float32).reshape(NB, SEQ, D) for r in res.results]
    return np.concatenate(outs, axis=0)
```
